# Optimizing a Trainium2 kernel written in Bass

```python
import math
import jax
import jax.numpy as jnp
from jax import lax
import numpy as np

D_MODEL = 4096
BATCH = 2
SEQ = 4096
DEPTH = 4

GRID_W = 64
CTX_LEN = 256

N_BRANCH = 3
BRANCH_WIDTH = D_MODEL // 4

HEAD_DIM = 128
N_Q_HEADS = BRANCH_WIDTH // HEAD_DIM
N_KV_HEADS = N_Q_HEADS // 4
Q_PER_KV = N_Q_HEADS // N_KV_HEADS
KV_WIDTH = N_KV_HEADS * HEAD_DIM
N_ROPE_FREQ = HEAD_DIM // 4
ROPE_THETA = 10000.0
Q_BLOCK = 128

RNN_WIDTH = BRANCH_WIDTH
RNN_BLOCKS = 8
RNN_BLOCK_DIM = RNN_WIDTH // RNN_BLOCKS
LRU_C = 8.0
CONV_W = 4

SSD_WIDTH = BRANCH_WIDTH
SSD_HEAD_DIM = 64
SSD_HEADS = SSD_WIDTH // SSD_HEAD_DIM
SSD_GROUPS = 2
SSD_STATE = 128
SSD_CHUNK = 128
SSD_BC = SSD_GROUPS * SSD_STATE
SSD_XBC = SSD_WIDTH + 2 * SSD_BC

Q_OFF = 0
K_OFF = Q_OFF + BRANCH_WIDTH
V_OFF = K_OFF + KV_WIDTH
RX_OFF = V_OFF + KV_WIDTH
RG_OFF = RX_OFF + RNN_WIDTH
SZ_OFF = RG_OFF + RNN_WIDTH
SX_OFF = SZ_OFF + SSD_WIDTH
SDT_OFF = SX_OFF + SSD_XBC
MIX_COLS = SDT_OFF + 2 * SSD_HEADS
IN_COLS = MIX_COLS + N_BRANCH * D_MODEL

MOD_RANK = 256
N_MOD = 6

N_EXPERTS = 16
N_EXPERT_GROUPS = 4
EXPERTS_PER_GROUP = N_EXPERTS // N_EXPERT_GROUPS
TOP_K = 2
EXPERT_FF = D_MODEL // 8

EPS = 1e-6

kernel_name = 'hybrid_rglru_gqa_ssd_moe_prefix_dit'


def rmsnorm(x, g):
    xf = x.astype(jnp.float32)
    y = xf * lax.rsqrt(jnp.mean(xf * xf, axis=-1, keepdims=True) + EPS)
    return (y * g.astype(jnp.float32)).astype(x.dtype)


def adaln(cond, w_a, w_b, b):
    m = (jax.nn.silu(cond) @ w_a) @ w_b + b
    return m.reshape(cond.shape[0], 1, N_MOD, D_MODEL)


def modulate(h, mod, k):
    return h * (1 + mod[..., k + 1, :]) + mod[..., k, :]


def axial_rope_tables(n_tokens, dtype):
    rows = n_tokens // GRID_W
    row = jnp.repeat(jnp.arange(rows, dtype=jnp.float32), GRID_W)
    col = jnp.tile(jnp.arange(GRID_W, dtype=jnp.float32), rows)
    inv = ROPE_THETA ** (-jnp.arange(N_ROPE_FREQ, dtype=jnp.float32) / N_ROPE_FREQ)
    ang = jnp.stack([row[:, None] * inv, col[:, None] * inv], axis=1)
    return jnp.cos(ang).astype(dtype), jnp.sin(ang).astype(dtype)


def apply_rope(t, cos, sin):
    shp = t.shape
    t = t.reshape(shp[:-1] + (2, 2, N_ROPE_FREQ))
    t1, t2 = t[..., 0, :], t[..., 1, :]
    cs, sn = cos[:, None], sin[:, None]
    out = jnp.stack([t1 * cs - t2 * sn, t2 * cs + t1 * sn], axis=-2)
    return out.reshape(shp)


def centred_dwconv(x, w, b):
    lo = (CONV_W - 1) // 2
    y = lax.conv_general_dilated(x, w[:, None, :].astype(x.dtype), window_strides=(1,),
                                 padding=[(lo, CONV_W - 1 - lo)],
                                 dimension_numbers=('NWC', 'WIO', 'NWC'),
                                 feature_group_count=x.shape[-1])
    return y + b


def linear_scan(a, u, h0, reverse):
    if h0 is not None:
        first = -1 if reverse else 0
        u = u.at[:, first].add(a[:, first] * h0)

    def comb(e1, e2):
        return e1[0] * e2[0], e2[0] * e1[1] + e2[1]

    _, h = lax.associative_scan(comb, (a, u), axis=1, reverse=reverse)
    return h


def attend(q, k, v):
    s = jnp.einsum('btkgd,bskd->bkgts', q, k, preferred_element_type=jnp.float32) * (HEAD_DIM ** -0.5)
    p = jax.nn.softmax(s, axis=-1).astype(v.dtype)
    return jnp.einsum('bkgts,bskd->btkgd', p, v)


def gqa_branch(pl, pc, q_norm, k_norm, cos, sin, ctx_out):
    def heads(p, off, n):
        return p[..., off:off + n * HEAD_DIM].reshape(p.shape[:2] + (n, HEAD_DIM))

    ql = apply_rope(rmsnorm(heads(pl, Q_OFF, N_Q_HEADS), q_norm), cos, sin)
    kl = apply_rope(rmsnorm(heads(pl, K_OFF, N_KV_HEADS), k_norm), cos, sin)
    vl = heads(pl, V_OFF, N_KV_HEADS)
    qc = rmsnorm(heads(pc, Q_OFF, N_Q_HEADS), q_norm)
    kc = rmsnorm(heads(pc, K_OFF, N_KV_HEADS), k_norm)
    vc = heads(pc, V_OFF, N_KV_HEADS)
    k_all = jnp.concatenate([kc, kl], axis=1)
    v_all = jnp.concatenate([vc, vl], axis=1)
    b, s = pl.shape[:2]
    qb = ql.reshape(b, s // Q_BLOCK, Q_BLOCK, N_KV_HEADS, Q_PER_KV, HEAD_DIM).swapaxes(0, 1)
    yl = lax.map(lambda q: attend(q, k_all, v_all), qb)
    yl = yl.swapaxes(0, 1).reshape(b, s, BRANCH_WIDTH)
    yc = None
    if ctx_out:
        n_ctx = pc.shape[1]
        yc = attend(qc.reshape(b, n_ctx, N_KV_HEADS, Q_PER_KV, HEAD_DIM), kc, vc).reshape(b, n_ctx, BRANCH_WIDTH)
    return yl, yc


def rglru_direction(xc, xl, lam, w_r, b_r, w_i, b_i, reverse, ctx_out):
    def gates_and_input(x):
        xf = x.astype(jnp.float32)
        xb = xf.reshape(x.shape[:2] + (RNN_BLOCKS, RNN_BLOCK_DIM))
        r = jax.nn.sigmoid(jnp.einsum('btnd,nde->btne', xb, w_r).reshape(x.shape) + b_r)
        i = jax.nn.sigmoid(jnp.einsum('btnd,nde->btne', xb, w_i).reshape(x.shape) + b_i)
        log_a = -LRU_C * r * jax.nn.softplus(-lam)
        return jnp.exp(log_a), jnp.sqrt(-jnp.expm1(2 * log_a)) * (i * xf)

    a_c, u_c = gates_and_input(xc)
    h_c = linear_scan(a_c, u_c, None, reverse)
    h_c_final = h_c[:, 0] if reverse else h_c[:, -1]
    a_l, u_l = gates_and_input(xl)
    h_l = linear_scan(a_l, u_l, h_c_final, reverse)
    return h_l, (h_c if ctx_out else None)


def rglru_branch(pl, pc, conv_w, conv_b, lam, w_r, b_r, w_i, b_i, ctx_out):
    xl = centred_dwconv(pl[..., RX_OFF:RX_OFF + RNN_WIDTH], conv_w, conv_b)
    xc = centred_dwconv(pc[..., RX_OFF:RX_OFF + RNN_WIDTH], conv_w, conv_b)
    hf_l, hf_c = rglru_direction(xc, xl, lam[0], w_r[0], b_r[0], w_i[0], b_i[0], False, ctx_out)
    hb_l, hb_c = rglru_direction(xc, xl, lam[1], w_r[1], b_r[1], w_i[1], b_i[1], True, ctx_out)
    yl = (jax.nn.gelu(pl[..., RG_OFF:RG_OFF + RNN_WIDTH]) * (hf_l + hb_l)).astype(pl.dtype)
    yc = None
    if ctx_out:
        yc = (jax.nn.gelu(pc[..., RG_OFF:RG_OFF + RNN_WIDTH]) * (hf_c + hb_c)).astype(pc.dtype)
    return yl, yc


def ssd_scan(x, dt, a_head, bm, cm, h0, reverse, with_y):
    if reverse:
        x, dt, bm, cm = (jnp.flip(t, axis=1) for t in (x, dt, bm, cm))
    bsz, t_len = x.shape[:2]
    nc = t_len // SSD_CHUNK
    hg = SSD_HEADS // SSD_GROUPS
    xdt = (x.astype(jnp.float32) * dt[..., None]).reshape(bsz, nc, SSD_CHUNK, SSD_GROUPS, hg, SSD_HEAD_DIM)
    a = (dt * a_head).reshape(bsz, nc, SSD_CHUNK, SSD_GROUPS, hg)
    bc = bm.astype(jnp.float32).reshape(bsz, nc, SSD_CHUNK, SSD_GROUPS, SSD_STATE)
    cc = cm.astype(jnp.float32).reshape(bsz, nc, SSD_CHUNK, SSD_GROUPS, SSD_STATE)
    a_cum = jnp.cumsum(a, axis=2)
    a_tot = a_cum[:, :, -1]
    states = jnp.einsum('bcqgn,bcqgh,bcqghp->bcghpn', bc, jnp.exp(a_tot[:, :, None] - a_cum), xdt)
    s_end = linear_scan(jnp.exp(a_tot)[..., None, None], states, h0, False)
    final = s_end[:, -1]
    if not with_y:
        return None, final
    init = h0 if h0 is not None else jnp.zeros_like(final)
    s_in = jnp.concatenate([init[:, None], s_end[:, :-1]], axis=1)
    diff = a_cum[:, :, :, None] - a_cum[:, :, None]
    lower = jnp.tril(jnp.ones((SSD_CHUNK, SSD_CHUNK), dtype=bool))[:, :, None, None]
    lmat = jnp.exp(jnp.where(lower, diff, -jnp.inf))
    cb = jnp.einsum('bcign,bcjgn->bcijg', cc, bc)
    y = jnp.einsum('bcijg,bcijgh,bcjghp->bcighp', cb, lmat, xdt)
    y = y + jnp.einsum('bcign,bcghpn,bcigh->bcighp', cc, s_in, jnp.exp(a_cum))
    y = y.reshape(bsz, t_len, SSD_HEADS, SSD_HEAD_DIM).astype(x.dtype)
    if reverse:
        y = jnp.flip(y, axis=1)
    return y, final


def ssd_branch(pl, pc, conv_w, conv_b, dt_bias, a_log, d_skip, norm_g, ctx_out):
    def prep(p):
        bsz, t_len = p.shape[:2]
        xbc = jax.nn.silu(centred_dwconv(p[..., SX_OFF:SX_OFF + SSD_XBC], conv_w, conv_b))
        xs = xbc[..., :SSD_WIDTH].reshape(bsz, t_len, SSD_HEADS, SSD_HEAD_DIM)
        bm = xbc[..., SSD_WIDTH:SSD_WIDTH + SSD_BC].reshape(bsz, t_len, SSD_GROUPS, SSD_STATE)
        cm = xbc[..., SSD_WIDTH + SSD_BC:].reshape(bsz, t_len, SSD_GROUPS, SSD_STATE)
        dt_raw = p[..., SDT_OFF:SDT_OFF + 2 * SSD_HEADS].astype(jnp.float32).reshape(bsz, t_len, 2, SSD_HEADS)
        return xs, bm, cm, dt_raw

    def gated_norm(y, p):
        z = p[..., SZ_OFF:SZ_OFF + SSD_WIDTH]
        g = (y.reshape(z.shape) * jax.nn.silu(z)).reshape(z.shape[:2] + (SSD_GROUPS, SSD_WIDTH // SSD_GROUPS))
        return rmsnorm(g, norm_g.reshape(SSD_GROUPS, SSD_WIDTH // SSD_GROUPS)).reshape(z.shape)

    xs_l, b_l, c_l, dt_l = prep(pl)
    xs_c, b_c, c_c, dt_c = prep(pc)
    y_l = d_skip[:, None] * xs_l
    y_c = d_skip[:, None] * xs_c if ctx_out else None
    for d, rev in enumerate((False, True)):
        a_head = -jnp.exp(a_log[d].astype(jnp.float32))
        yc_d, hc_final = ssd_scan(xs_c, jax.nn.softplus(dt_c[:, :, d] + dt_bias[d]), a_head, b_c, c_c,
                                  None, rev, ctx_out)
        yl_d, _ = ssd_scan(xs_l, jax.nn.softplus(dt_l[:, :, d] + dt_bias[d]), a_head, b_l, c_l,
                           hc_final, rev, True)
        y_l = y_l + yl_d
        if ctx_out:
            y_c = y_c + yc_d
    yl = gated_norm(y_l, pl)
    yc = gated_norm(y_c, pc) if ctx_out else None
    return yl, yc


def merge_branches(p, branches, w_up, w_o):
    gates = jax.nn.sigmoid(p[..., MIX_COLS:].reshape(p.shape[:2] + (N_BRANCH, D_MODEL)))
    up = jnp.einsum('btnw,nwd->btnd', jnp.stack(branches, axis=2), w_up)
    return jnp.sum(gates * up, axis=2) @ w_o


def moe_ffn(h, router_w, router_b, w_gate, w_up, w_down):
    shp = h.shape
    t = h.reshape(-1, D_MODEL)
    score = jax.nn.sigmoid((t @ router_w).astype(jnp.float32))
    sel = (score + router_b).reshape(-1, N_EXPERT_GROUPS, EXPERTS_PER_GROUP)
    group_score = jnp.sum(lax.top_k(sel, TOP_K)[0], axis=-1)
    in_group = jnp.argmax(group_score, axis=-1)[:, None] == jnp.arange(N_EXPERT_GROUPS)
    masked = jnp.where(in_group[..., None], sel, -jnp.inf).reshape(-1, N_EXPERTS)
    _, idx = lax.top_k(masked, TOP_K)
    w = jnp.take_along_axis(score, idx, axis=-1)
    w = w / jnp.sum(w, axis=-1, keepdims=True)
    combine = jnp.sum(jax.nn.one_hot(idx, N_EXPERTS, dtype=jnp.float32) * w[..., None], axis=1)
    hid = jax.nn.silu(jnp.einsum('td,edf->tef', t, w_gate)) * jnp.einsum('td,edf->tef', t, w_up)
    y = jnp.einsum('tef,efd->td', hid * combine[..., None].astype(hid.dtype), w_down)
    return y.reshape(shp)


def setup_inputs(seed: int = 0) -> dict:
    key = jax.random.key(seed)
    ks = iter(jax.random.split(key, 48))
    f32 = jnp.float32
    nl = DEPTH

    def normal(shape, scale):
        return jax.random.normal(next(ks), shape, f32) * scale

    def gain(shape):
        return 1.0 + normal(shape, 0.02)

    a_c = jax.random.uniform(next(ks), (nl, 2, RNN_WIDTH), f32, 0.9, 0.999)
    s_lam = a_c ** (1.0 / LRU_C)
    rnn_lambda = jnp.log(s_lam) - jnp.log1p(-s_lam)
    dt0 = jnp.exp(jax.random.uniform(next(ks), (nl, 2, SSD_HEADS), f32, math.log(1e-3), math.log(1e-1)))
    ssd_dt_bias = dt0 + jnp.log(-jnp.expm1(-dt0))
    ssd_a_log = jnp.log(jax.random.uniform(next(ks), (nl, 2, SSD_HEADS), f32, 1.0, 16.0))

    return {
        'x': normal((BATCH, SEQ, D_MODEL), 1.0),
        'c': normal((BATCH, D_MODEL), 1.0),
        'ctx': normal((BATCH, CTX_LEN, D_MODEL), 1.0),
        'c_ctx': normal((D_MODEL,), 1.0),
        'w_mod_a': normal((nl, D_MODEL, MOD_RANK), D_MODEL ** -0.5),
        'w_mod_b': normal((nl, MOD_RANK, N_MOD * D_MODEL), 0.3 * MOD_RANK ** -0.5),
        'b_mod': normal((nl, N_MOD * D_MODEL), 0.02),
        'g_mix': gain((nl, D_MODEL)),
        'g_ffn': gain((nl, D_MODEL)),
        'w_in': normal((nl, D_MODEL, IN_COLS), D_MODEL ** -0.5),
        'w_up': normal((nl, N_BRANCH, BRANCH_WIDTH, D_MODEL), BRANCH_WIDTH ** -0.5),
        'w_o': normal((nl, D_MODEL, D_MODEL), D_MODEL ** -0.5),
        'q_norm': gain((nl, HEAD_DIM)),
        'k_norm': gain((nl, HEAD_DIM)),
        'rnn_conv_w': normal((nl, CONV_W, RNN_WIDTH), CONV_W ** -0.5),
        'rnn_conv_b': normal((nl, RNN_WIDTH), 0.02),
        'rnn_lambda': rnn_lambda,
        'rnn_w_r': normal((nl, 2, RNN_BLOCKS, RNN_BLOCK_DIM, RNN_BLOCK_DIM), RNN_BLOCK_DIM ** -0.5),
        'rnn_b_r': normal((nl, 2, RNN_WIDTH), 0.1),
        'rnn_w_i': normal((nl, 2, RNN_BLOCKS, RNN_BLOCK_DIM, RNN_BLOCK_DIM), RNN_BLOCK_DIM ** -0.5),
        'rnn_b_i': normal((nl, 2, RNN_WIDTH), 0.1),
        'ssd_conv_w': normal((nl, CONV_W, SSD_XBC), CONV_W ** -0.5),
        'ssd_conv_b': normal((nl, SSD_XBC), 0.02),
        'ssd_dt_bias': ssd_dt_bias,
        'ssd_a_log': ssd_a_log,
        'ssd_d': 1.0 + normal((nl, SSD_HEADS), 0.1),
        'ssd_norm': gain((nl, SSD_WIDTH)),
        'router_w': normal((D_MODEL, N_EXPERTS), D_MODEL ** -0.5),
        'router_b': normal((N_EXPERTS,), 0.01),
        'moe_w_gate': normal((nl, N_EXPERTS, D_MODEL, EXPERT_FF), D_MODEL ** -0.5),
        'moe_w_up': normal((nl, N_EXPERTS, D_MODEL, EXPERT_FF), D_MODEL ** -0.5),
        'moe_w_down': normal((nl, N_EXPERTS, EXPERT_FF, D_MODEL), EXPERT_FF ** -0.5),
        'g_final': gain((D_MODEL,)),
    }


def reference(x, c, ctx, c_ctx, w_mod_a, w_mod_b, b_mod, g_mix, g_ffn, w_in, w_up, w_o, q_norm, k_norm,
              rnn_conv_w, rnn_conv_b, rnn_lambda, rnn_w_r, rnn_b_r, rnn_w_i, rnn_b_i,
              ssd_conv_w, ssd_conv_b, ssd_dt_bias, ssd_a_log, ssd_d, ssd_norm,
              router_w, router_b, moe_w_gate, moe_w_up, moe_w_down, g_final):
    cos, sin = axial_rope_tables(x.shape[1], x.dtype)
    xl, xc = x, ctx
    for l in range(DEPTH):
        ctx_out = l < DEPTH - 1
        mod_l = adaln(c, w_mod_a[l], w_mod_b[l], b_mod[l])
        mod_c = adaln(c_ctx[None], w_mod_a[l], w_mod_b[l], b_mod[l])

        hl = modulate(rmsnorm(xl, g_mix[l]), mod_l, 0)
        hc = modulate(rmsnorm(xc, g_mix[l]), mod_c, 0)
        pl = hl @ w_in[l]
        pc = hc @ (w_in[l] if ctx_out else w_in[l][:, :MIX_COLS])
        ya_l, ya_c = gqa_branch(pl, pc, q_norm[l], k_norm[l], cos, sin, ctx_out)
        yr_l, yr_c = rglru_branch(pl, pc, rnn_conv_w[l], rnn_conv_b[l], rnn_lambda[l], rnn_w_r[l],
                                  rnn_b_r[l], rnn_w_i[l], rnn_b_i[l], ctx_out)
        ys_l, ys_c = ssd_branch(pl, pc, ssd_conv_w[l], ssd_conv_b[l], ssd_dt_bias[l], ssd_a_log[l],
                                ssd_d[l], ssd_norm[l], ctx_out)
        xl = xl + mod_l[..., 2, :] * merge_branches(pl, (ya_l, yr_l, ys_l), w_up[l], w_o[l])

        hl2 = modulate(rmsnorm(xl, g_ffn[l]), mod_l, 3)
        if ctx_out:
            xc = xc + mod_c[..., 2, :] * merge_branches(pc, (ya_c, yr_c, ys_c), w_up[l], w_o[l])
            hc2 = modulate(rmsnorm(xc, g_ffn[l]), mod_c, 3)
            n_lat = hl2.shape[1]
            y2 = moe_ffn(jnp.concatenate([hl2, hc2], axis=1), router_w, router_b,
                         moe_w_gate[l], moe_w_up[l], moe_w_down[l])
            xl = xl + mod_l[..., 5, :] * y2[:, :n_lat]
            xc = xc + mod_c[..., 5, :] * y2[:, n_lat:]
        else:
            xl = xl + mod_l[..., 5, :] * moe_ffn(hl2, router_w, router_b,
                                                 moe_w_gate[l], moe_w_up[l], moe_w_down[l])
    return rmsnorm(xl, g_final)
```

```python
import numpy as np
import concourse.bass as bass
import concourse.mybir as mybir
from concourse.bass_utils import run_bass_kernel_spmd

F32 = mybir.dt.float32
BF16 = mybir.dt.bfloat16
I32 = mybir.dt.int32
AF = mybir.ActivationFunctionType
ALU = mybir.AluOpType
AX = mybir.AxisListType


class Buf:
    __slots__ = ("t", "w", "r", "name", "nt")

    def __init__(self, t, name=""):
        self.t = t
        self.w = None
        self.r = {}
        self.name = name
        self.nt = False

    def __getitem__(self, idx):
        return self.t[idx]


class Prog:
    ENG = ("pe", "dve", "act", "pool", "sp")

    def __init__(self, nc, n_dma_sems=48):
        self.nc = nc
        self.e = {"pe": nc.tensor, "dve": nc.vector, "act": nc.scalar, "pool": nc.gpsimd, "sp": nc.sync}
        self.sem = {}
        self.cnt = {}
        for k in self.ENG:
            self.sem[k] = nc.alloc_semaphore("s_" + k)
            self.cnt[k] = 0
        self.dsem = [nc.alloc_semaphore("d%d" % i) for i in range(n_dma_sems)]
        self.dcnt = [0] * n_dma_sems
        self.dnext = 0
        for i in range(n_dma_sems):
            self.sem[("d", i)] = self.dsem[i]
        self.seen = {k: {} for k in self.ENG}
        self.pend = {k: [] for k in self.ENG}
        self.ninst = 0

    def sb(self, name, shape, dt=F32):
        return Buf(self.nc.alloc_sbuf_tensor(name, list(shape), dt), name)

    def ps(self, name, shape, dt=F32):
        return Buf(self.nc.alloc_psum_tensor(name, list(shape), dt), name)

    def dram(self, name, shape, dt=F32, kind="Internal"):
        return Buf(self.nc.dram_tensor(name, list(shape), dt, kind=kind), name)

    def _wait(self, eng, ev):
        if ev is None:
            return
        key, val = ev
        if self.seen[eng].get(key, 0) >= val:
            return
        if key == eng and eng == "pe":
            return
        self.e[eng].wait_ge(self.sem[key], val)
        self.seen[eng][key] = val

    def _waitw(self, eng, w):
        if w is None:
            return
        if isinstance(w, list):
            for ev in w:
                self._wait(eng, ev)
        else:
            self._wait(eng, w)

    def _deps(self, eng, reads, writes):
        for b in reads:
            self._waitw(eng, b.w)
        for b in writes:
            self._waitw(eng, b.w)
            for k, v in b.r.items():
                self._wait(eng, (k, v))

    def _mark(self, ev, reads, writes):
        for b in reads:
            if not b.nt:
                b.r[ev[0]] = ev[1]
        for b in writes:
            if not b.nt:
                b.w = ev
                b.r = {}

    def op(self, eng, fn, r=(), w=(), inc=True):
        self._deps(eng, r, w)
        inst = fn()
        self.ninst += 1
        if not inc:
            self.pend[eng].append((r, w))
            return None
        self.cnt[eng] += 1
        inst.then_inc(self.sem[eng], 1)
        ev = (eng, self.cnt[eng])
        for pr, pw in self.pend[eng]:
            self._mark(ev, pr, pw)
        self.pend[eng] = []
        self._mark(ev, r, w)
        return ev

    def dma(self, out, in_, r=(), w=(), q="sp", **kw):
        i = self.dnext
        self.dnext = (self.dnext + 1) % len(self.dsem)
        key = ("d", i)
        if self.dcnt[i] > 0:
            self._wait(q, (key, self.dcnt[i]))
        self._deps(q, r, w)
        inst = self.e[q].dma_start(out=out, in_=in_, **kw)
        self.dcnt[i] += 16
        inst.then_inc(self.dsem[i], 16)
        ev = (key, self.dcnt[i])
        self._mark(ev, r, w)
        self.ninst += 1
        return ev

    def dma_multi(self, pairs, r=(), w=(), q="sp", **kw):
        self._deps(q, r, w)
        evs = []
        for (out, in_) in pairs:
            i = self.dnext
            self.dnext = (self.dnext + 1) % len(self.dsem)
            key = ("d", i)
            if self.dcnt[i] > 0:
                self._wait(q, (key, self.dcnt[i]))
            inst = self.e[q].dma_start(out=out, in_=in_, **kw)
            self.dcnt[i] += 16
            inst.then_inc(self.dsem[i], 16)
            evs.append((key, self.dcnt[i]))
            self.ninst += 1
        for b in r:
            if not b.nt:
                for ev in evs:
                    b.r[ev[0]] = ev[1]
        for b in w:
            if not b.nt:
                b.w = list(evs)
                b.r = {}
        return evs

    def barrier(self):
        for eng in self.ENG:
            for k in self.ENG:
                if k != eng and self.cnt[k] > 0:
                    self._wait(eng, (k, self.cnt[k]))
            for i in range(len(self.dsem)):
                if self.dcnt[i] > 0:
                    self._wait(eng, (("d", i), self.dcnt[i]))

    def finish(self, bufs):
        for b in bufs:
            self._waitw("sp", b.w)


import math

D = 4096
KD = 32
NB = 3
BW = 1024
MIXC = 6176
NCH_IN = 145
CH_Q, CH_K, CH_V, CH_RX, CH_RG, CH_SZ, CH_SX, CH_SB, CH_SC, CH_G, CH_DT = 0, 8, 10, 12, 20, 28, 36, 44, 46, 48, 144
EPS = 1e-6
WCAP = 16384
ACAP = 16384


class Cfg:
    def __init__(self, CTX, SEQ, DEPTH):
        self.CTX, self.SEQ, self.DEPTH = CTX, SEQ, DEPTH
        self.T = CTX + SEQ
        self.tiles = []
        for o in range(0, CTX, 512):
            self.tiles.append((o, min(512, CTX - o), 1))
        for o in range(0, SEQ, 512):
            self.tiles.append((CTX + o, min(512, SEQ - o), 0))
        self.tiles256 = []
        for (o, n, j) in self.tiles:
            for oo in range(0, n, 256):
                self.tiles256.append((o + oo, min(256, n - oo), j))


def wlay(W):
    K, N = W.shape
    KC, NC = K // 128, N // 128
    return np.ascontiguousarray(W.reshape(KC, 128, NC, 128).transpose(2, 1, 0, 3).reshape(NC, 128, KC * 128))


def vlay(v):
    return np.ascontiguousarray(v.reshape(-1, 128).T)


class K:
    def __init__(self, cfg, debug=()):
        self.cfg = cfg
        self.debug = set(debug)
        nc = bass.Bass("TRN2", target_bir_lowering=False)
        self.nc = nc
        self.P = Prog(nc)
        self.inputs = {}
        self.outputs = {}

    def inp(self, name, shape, dt=F32):
        b = self.P.dram(name, shape, dt, kind="ExternalInput")
        self.inputs[name] = b
        return b

    def outp(self, name, shape, dt=F32):
        b = self.P.dram(name, shape, dt, kind="ExternalOutput")
        self.outputs[name] = b
        return b

    def gemm(self, act, KC, wd, nch, tiles, evac, c0=0, tok_cap=512, split=1):
        P, nc = self.P, self.nc
        G = max(1, min(nch, WCAP // (KC * 128)))
        ntmax = min(tok_cap, ACAP // KC)
        tl = []
        for (o, n, j) in tiles:
            for oo in range(0, n, ntmax):
                tl.append((o + oo, min(ntmax, n - oo), j))
        actv = act.t.ap().rearrange("(kc p) t -> p kc t", p=128)
        gi = 0
        for g0 in range(0, nch, G):
            gn = min(G, nch - g0)
            wb = self.wbuf[gi % 2]
            gi += 1
            P.dma_multi([(wb.t.ap()[:, c * KC * 128:(c + 1) * KC * 128], wd.t.ap()[c0 + g0 + c, :, :]) for c in range(gn)],
                        r=[wd], w=[wb], q="pool", max_dma_last_dim=8192)
            for (o, n, j) in tl:
                ab = self.abuf[self.ai % 2]
                self.ai += 1
                adst = ab.t.ap()[:, 0:KC * n].rearrange("p (kc t) -> p kc t", kc=KC)
                P.dma(adst, actv[:, :, o:o + n], r=[act], w=[ab], q="sp")
                for c in range(gn):
                    if split > 1:
                        kg = KC // split
                        pss = [self.psum[(self.pi % 2) * 3 + s_] for s_ in range(split)]
                        self.pi += 1
                        for kc in range(KC):
                            ps = pss[kc // kg]
                            P.op("pe", lambda: nc.tensor.matmul(
                                out=ps.t.ap()[:, 0:n],
                                lhsT=wb.t.ap()[:, (c * KC + kc) * 128:(c * KC + kc + 1) * 128],
                                rhs=ab.t.ap()[:, kc * n:(kc + 1) * n],
                                start=(kc % kg == 0), stop=(kc % kg == kg - 1)),
                                r=[wb, ab], w=[ps], inc=(kc % kg == kg - 1))
                        evac(g0 + c, (o, n, j), pss)
                        continue
                    ps = self.psum[self.pi % 4]
                    self.pi += 1
                    for kc in range(KC):
                        P.op("pe", lambda: nc.tensor.matmul(
                            out=ps.t.ap()[:, 0:n],
                            lhsT=wb.t.ap()[:, (c * KC + kc) * 128:(c * KC + kc + 1) * 128],
                            rhs=ab.t.ap()[:, kc * n:(kc + 1) * n],
                            start=(kc == 0), stop=(kc == KC - 1)),
                            r=[wb, ab], w=[ps], inc=(kc == KC - 1))
                    evac(g0 + c, (o, n, j), ps)

    def setup(self):
        P, nc, cfg = self.P, self.nc, self.cfg
        self.wbuf = [P.sb("wbuf%d" % i, [128, WCAP], BF16) for i in range(2)]
        self.abuf = [P.sb("abuf%d" % i, [128, ACAP], BF16) for i in range(2)]
        self.psum = [P.ps("ps%d" % i, [128, 512], F32) for i in range(8)]
        self.ai = 0
        self.pi = 0
        self.stg = [P.sb("stg%d" % i, [128, 512], F32) for i in range(4)]
        self.si = 0
        self.ones_f = P.sb("ones_f", [128, 128], F32)
        P.op("dve", lambda: nc.vector.memset(self.ones_f.t.ap(), 1.0), w=[self.ones_f])
        self.ones_b = P.sb("ones_b", [128, 128], BF16)
        P.op("dve", lambda: nc.vector.memset(self.ones_b.t.ap(), 1.0), w=[self.ones_b])
        T = cfg.T
        self.xT = P.dram("xT", [D, T], F32)
        self.hT = P.dram("hT", [D, T], BF16)
        self.plA = P.dram("plA", [49 * 128, T], F32)
        self.plG = P.dram("plG", [96 * 128, T], F32)
        for b_ in (self.xT, self.hT, self.plA, self.plG):
            b_.nt = True
        self.x_in = self.inp("x_in", [D, T])
        self.cond = self.inp("cond", [128, KD * 2])
        L = cfg.DEPTH
        self.w_mod_a = self.inp("w_mod_a", [L, 128, KD * 256])
        self.w_mod_b = self.inp("w_mod_b", [L, 6, 128, 2 * D])
        self.b_mod = self.inp("b_mod", [L, 128, 6 * KD])
        self.g_mix = self.inp("g_mix", [L, 128, KD])
        self.g_ffn = self.inp("g_ffn", [L, 128, KD])
        self.w_in = self.inp("w_in", [L, NCH_IN, 128, D])
        self.modT = P.sb("modT", [128, 6 * KD * 2], F32)
        self.A1 = P.sb("A1", [128, KD * 2], F32)
        self.A2 = P.sb("A2", [128, KD * 2], F32)
        self.sc = P.sb("sc", [128, KD * 2], F32)
        self.t1 = P.sb("t1", [128, 4], F32)
        self.gvec = P.sb("gvec", [128, 2 * KD], F32)
        self.bmod = P.sb("bmod", [128, 6 * KD], F32)
        self.big = [P.sb("big%d" % i, [128, 8192], F32) for i in range(2)]

    def plv(self, r0, r1):
        c = r0 // 128
        if c < 48:
            return self.plA.t.ap()[r0:r1, :]
        if c == 144:
            return self.plA.t.ap()[r0 - 96 * 128:r1 - 96 * 128, :]
        return self.plG.t.ap()[r0 - 48 * 128:r1 - 48 * 128, :]

    def stage(self):
        b = self.stg[self.si % 4]
        self.si += 1
        return b

    def adaln(self, l):
        P, nc = self.P, self.nc
        ct = self.stage()
        P.dma(ct.t.ap()[:, 0:KD * 2], self.cond.t.ap()[:, :], r=[self.cond], w=[ct])
        P.op("act", lambda: nc.scalar.activation(out=self.sc.t.ap(), in_=ct.t.ap()[:, 0:KD * 2], func=AF.Silu),
             r=[ct], w=[self.sc])
        wa = self.big[0]
        P.dma(wa.t.ap()[:, 0:KD * 256], self.w_mod_a.t.ap()[l, :, :], r=[self.w_mod_a], w=[wa])
        ps = self.psum[4]
        for rc in range(2):
            for kc in range(KD):
                P.op("pe", lambda: nc.tensor.matmul(
                    out=ps.t.ap()[:, rc * 2:rc * 2 + 2],
                    lhsT=wa.t.ap()[:, kc * 256 + rc * 128: kc * 256 + rc * 128 + 128],
                    rhs=self.sc.t.ap()[:, kc * 2:kc * 2 + 2],
                    start=(kc == 0), stop=(kc == KD - 1)), r=[wa, self.sc], w=[ps], inc=(kc == KD - 1 and rc == 1))
        P.op("dve", lambda: nc.vector.tensor_copy(out=self.t1.t.ap(), in_=ps.t.ap()[:, 0:4]), r=[ps], w=[self.t1])
        P.dma(self.bmod.t.ap(), self.b_mod.t.ap()[l, :, :], r=[self.b_mod], w=[self.bmod])
        P.dma(self.gvec.t.ap()[:, 0:KD], self.g_mix.t.ap()[l, :, :], r=[self.g_mix], w=[self.gvec])
        P.dma(self.gvec.t.ap()[:, KD:2 * KD], self.g_ffn.t.ap()[l, :, :], r=[self.g_ffn], w=[self.gvec])
        for k in range(6):
            wbm = self.big[k % 2]
            P.dma(wbm.t.ap(), self.w_mod_b.t.ap()[l, k, :, :], r=[self.w_mod_b], w=[wbm])
            ps = self.psum[5 + (k % 2)]
            for dc in range(KD):
                for rc in range(2):
                    P.op("pe", lambda: nc.tensor.matmul(
                        out=ps.t.ap()[:, dc * 2:dc * 2 + 2],
                        lhsT=wbm.t.ap()[:, rc * D + dc * 128: rc * D + dc * 128 + 128],
                        rhs=self.t1.t.ap()[:, rc * 2:rc * 2 + 2],
                        start=(rc == 0), stop=(rc == 1)), r=[wbm, self.t1], w=[ps], inc=(rc == 1 and dc == KD - 1))
            P.op("dve", lambda: nc.vector.tensor_tensor(
                out=self.modT.t.ap()[:, k * 64:(k + 1) * 64].rearrange("p (d j) -> p d j", j=2),
                in0=ps.t.ap()[:, 0:64].rearrange("p (d j) -> p d j", j=2),
                in1=self.bmod.t.ap()[:, k * KD:(k + 1) * KD].unsqueeze(2).to_broadcast([128, KD, 2]),
                op=ALU.add), r=[ps, self.bmod], w=[self.modT])
        for (A, kk, go) in ((self.A1, 1, 0), (self.A2, 4, KD)):
            P.op("dve", lambda: nc.vector.scalar_tensor_tensor(
                out=A.t.ap().rearrange("p (d j) -> p d j", j=2),
                in0=self.modT.t.ap()[:, kk * 64:(kk + 1) * 64].rearrange("p (d j) -> p d j", j=2),
                scalar=1.0,
                in1=self.gvec.t.ap()[:, go:go + KD].unsqueeze(2).to_broadcast([128, KD, 2]),
                op0=ALU.add, op1=ALU.mult), r=[self.modT, self.gvec], w=[A])

    def mod(self, k, dc, j):
        return self.modT.t.ap()[:, k * 64 + dc * 2 + j: k * 64 + dc * 2 + j + 1]

    def norm_mod(self, A, kshift, src, dst):
        P, nc, cfg = self.P, self.nc, self.cfg
        for ti, (o, n, j) in enumerate(cfg.tiles256):
            xt = self.big[0]
            xv = xt.t.ap()[:, 0:KD * n].rearrange("p (kc t) -> p kc t", kc=KD)
            P.dma(xv, src.t.ap().rearrange("(kc p) t -> p kc t", p=128)[:, :, o:o + n], r=[src], w=[xt])
            sq = self.big[1]
            P.op("act", lambda: nc.scalar.activation(out=sq.t.ap()[:, 0:KD * n], in_=xt.t.ap()[:, 0:KD * n], func=AF.Square),
                 r=[xt], w=[sq])
            ps = self.psum[4 + (ti % 2)]
            for kc in range(KD):
                P.op("pe", lambda: nc.tensor.matmul(out=ps.t.ap()[:, 0:n], lhsT=self.ones_f.t.ap(),
                                                     rhs=sq.t.ap()[:, kc * n:(kc + 1) * n],
                                                     start=(kc == 0), stop=(kc == KD - 1)),
                     r=[self.ones_f, sq], w=[ps], inc=(kc == KD - 1))
            rs = self.stage()
            P.op("dve", lambda: nc.vector.tensor_scalar(out=rs.t.ap()[:, 0:n], in0=ps.t.ap()[:, 0:n],
                                                        scalar1=1.0 / D, scalar2=EPS, op0=ALU.mult, op1=ALU.add),
                 r=[ps], w=[rs])
            P.op("act", lambda: nc.scalar.activation(out=rs.t.ap()[:, 0:n], in_=rs.t.ap()[:, 0:n], func=AF.Sqrt),
                 r=[rs], w=[rs])
            P.op("dve", lambda: nc.vector.reciprocal(out=rs.t.ap()[:, 0:n], in_=rs.t.ap()[:, 0:n]), r=[rs], w=[rs])
            P.op("dve", lambda: nc.vector.tensor_tensor(
                out=sq.t.ap()[:, 0:KD * n].rearrange("p (kc t) -> p kc t", kc=KD), in0=xv,
                in1=rs.t.ap()[:, 0:n].unsqueeze(1).to_broadcast([128, KD, n]), op=ALU.mult),
                r=[xt, rs], w=[sq])
            hb = self.abuf[self.ai % 2]
            self.ai += 1
            for kc in range(KD):
                P.op("act", lambda: nc.scalar.activation(
                    out=hb.t.ap()[:, kc * n:(kc + 1) * n], in_=sq.t.ap()[:, kc * n:(kc + 1) * n], func=AF.Identity,
                    bias=self.mod(kshift, kc, j), scale=A.t.ap()[:, kc * 2 + j:kc * 2 + j + 1]),
                    r=[sq, self.modT, A], w=[hb])
            P.dma(dst.t.ap().rearrange("(kc p) t -> p kc t", p=128)[:, :, o:o + n],
                  hb.t.ap()[:, 0:KD * n].rearrange("p (kc t) -> p kc t", kc=KD), r=[hb], w=[dst])

    def p1(self, l):
        P, nc, cfg = self.P, self.nc, self.cfg
        wd = Buf(self.w_in.t.ap()[l], "w_in_l")
        wd.t = self.w_in.t
        def evac(c, tile, ps):
            o, n, j = tile
            st = self.stage()
            P.op("act", lambda: nc.scalar.copy(out=st.t.ap()[:, 0:n], in_=ps.t.ap()[:, 0:n]), r=[ps], w=[st])
            P.dma(self.plv(c * 128, (c + 1) * 128)[:, o:o + n], st.t.ap()[:, 0:n], r=[st], w=[self.plA, self.plG], q="act")
        wl = Buf(self.w_in.t.ap()[l], "w_in_layer")
        self.gemm(self.hT, KD, _LayerView(self.w_in, l), NCH_IN, cfg.tiles, evac)


class _LayerView:
    def __init__(self, buf, l):
        self.buf = buf
        self.l = l
        self.w, self.r = None, {}
        self.nt = True

    @property
    def t(self):
        return _TV(self.buf.t.ap()[self.l])


class _TV:
    def __init__(self, ap):
        self._ap = ap

    def ap(self):
        return self._ap


class _V:
    def __init__(self, ap):
        self._ap = ap

    def ap(self):
        return self._ap


def carve_buf(parent, e0, e1, dt=None, name=""):
    ap = parent.t.ap()[:, e0:e1]
    if dt is not None:
        ap = ap.bitcast(dt)
    return Buf(_V(ap), name)


def _attn_setup(self):
    T = self.cfg.T
    a = {}
    a["kT"] = carve_buf(self.wbuf[0], 0, T, None, "kT")
    a["vtok"] = carve_buf(self.wbuf[0], 8192, 8192 + T, None, "vtok")
    a["qT"] = [carve_buf(self.wbuf[1], i * 8192, i * 8192 + T, None, "qT%d" % i) for i in range(2)]
    a["PT"] = [carve_buf(self.abuf[0], i * 512, (i + 1) * 512, None, "PT%d" % i) for i in range(6)]
    a["yo"] = [carve_buf(self.abuf[0], 4096 + i * 512, 4096 + (i + 1) * 512, None, "yo%d" % i) for i in range(2)]
    f = lambda i, nm: carve_buf(self.big[0], i * 512, (i + 1) * 512, None, nm)
    a["raw"] = [f(0, "raw0"), f(1, "raw1")]
    a["sq"] = f(2, "sq")
    a["rs"] = f(3, "rs")
    a["xn"] = f(4, "xn")
    a["t1"] = f(5, "t1")
    a["t2"] = f(6, "t2")
    a["cos"] = [f(7, "cos0"), f(8, "cos1")]
    a["sin"] = [f(9, "sin0"), f(10, "sin1")]
    a["rz"] = f(11, "rz")
    return a


def attention(self, l):
    P, nc, cfg = self.P, self.nc, self.cfg
    T, CTX = cfg.T, cfg.CTX
    P.barrier()
    a = _attn_setup(self)
    ident = self.consts.t.ap()[:, 0:128]
    Rm = self.consts.t.ap()[:, 128:256]
    qk = self.stage()
    P.dma(qk.t.ap()[:, 0:2], self.qkn.t.ap()[l, :, :], r=[self.qkn], w=[qk])
    ri = [0]

    def prep(chunk_row, gcol, dst):
        for (o, n, j) in cfg.tiles:
            raw = a["raw"][ri[0] % 2]
            ri[0] += 1
            P.dma(raw.t.ap()[:, 0:n], self.plv(chunk_row * 128, (chunk_row + 1) * 128)[:, o:o + n], r=[self.plA, self.plG], w=[raw])
            P.op("act", lambda: nc.scalar.activation(out=a["sq"].t.ap()[:, 0:n], in_=raw.t.ap()[:, 0:n], func=AF.Square),
                 r=[raw], w=[a["sq"]])
            ps = self.psum[self.pi % 4]
            self.pi += 1
            P.op("pe", lambda: nc.tensor.matmul(out=ps.t.ap()[:, 0:n], lhsT=self.ones_f.t.ap(), rhs=a["sq"].t.ap()[:, 0:n],
                                                 start=True, stop=True), r=[self.ones_f, a["sq"]], w=[ps])
            rs = a["rs"]
            P.op("dve", lambda: nc.vector.tensor_scalar(out=rs.t.ap()[:, 0:n], in0=ps.t.ap()[:, 0:n], scalar1=1.0 / 128,
                                                        scalar2=EPS, op0=ALU.mult, op1=ALU.add), r=[ps], w=[rs])
            P.op("act", lambda: nc.scalar.activation(out=rs.t.ap()[:, 0:n], in_=rs.t.ap()[:, 0:n], func=AF.Sqrt), r=[rs], w=[rs])
            P.op("dve", lambda: nc.vector.reciprocal(out=rs.t.ap()[:, 0:n], in_=rs.t.ap()[:, 0:n]), r=[rs], w=[rs])
            if j == 1:
                P.op("dve", lambda: nc.vector.scalar_tensor_tensor(
                    out=dst.t.ap()[:, o:o + n], in0=raw.t.ap()[:, 0:n], scalar=qk.t.ap()[:, gcol:gcol + 1],
                    in1=rs.t.ap()[:, 0:n], op0=ALU.mult, op1=ALU.mult), r=[raw, qk, rs], w=[dst])
                continue
            xn = a["xn"]
            P.op("dve", lambda: nc.vector.scalar_tensor_tensor(
                out=xn.t.ap()[:, 0:n], in0=raw.t.ap()[:, 0:n], scalar=qk.t.ap()[:, gcol:gcol + 1],
                in1=rs.t.ap()[:, 0:n], op0=ALU.mult, op1=ALU.mult), r=[raw, qk, rs], w=[xn])
            ps2 = self.psum[self.pi % 4]
            self.pi += 1
            P.op("pe", lambda: nc.tensor.matmul(out=ps2.t.ap()[:, 0:n], lhsT=Rm, rhs=xn.t.ap()[:, 0:n], start=True, stop=True),
                 r=[self.consts, xn], w=[ps2])
            cs, sn = a["cos"][ri[0] % 2], a["sin"][ri[0] % 2]
            P.dma(cs.t.ap()[:, 0:n], self.rope.t.ap()[0, :, o - CTX:o - CTX + n], r=[self.rope], w=[cs])
            P.dma(sn.t.ap()[:, 0:n], self.rope.t.ap()[1, :, o - CTX:o - CTX + n], r=[self.rope], w=[sn])
            P.op("dve", lambda: nc.vector.tensor_tensor(out=a["t1"].t.ap()[:, 0:n], in0=xn.t.ap()[:, 0:n], in1=cs.t.ap()[:, 0:n],
                                                        op=ALU.mult), r=[xn, cs], w=[a["t1"]])
            P.op("dve", lambda: nc.vector.tensor_tensor(out=a["t2"].t.ap()[:, 0:n], in0=ps2.t.ap()[:, 0:n], in1=sn.t.ap()[:, 0:n],
                                                        op=ALU.mult), r=[ps2, sn], w=[a["t2"]])
            P.op("dve", lambda: nc.vector.tensor_tensor(out=dst.t.ap()[:, o:o + n], in0=a["t1"].t.ap()[:, 0:n],
                                                        in1=a["t2"].t.ap()[:, 0:n], op=ALU.add), r=[a["t1"], a["t2"]], w=[dst])

    scale = 128 ** -0.5
    for kv in range(2):
        prep(CH_K + kv, 1, a["kT"])
        for (o, n, j) in cfg.tiles:
            raw = a["raw"][ri[0] % 2]
            ri[0] += 1
            P.dma(raw.t.ap()[:, 0:n], self.plv((CH_V + kv) * 128, (CH_V + kv + 1) * 128)[:, o:o + n], r=[self.plA, self.plG], w=[raw])
            for s in range(n // 128):
                ps = self.psum[self.pi % 4]
                self.pi += 1
                P.op("pe", lambda: nc.tensor.transpose(out=ps.t.ap()[:, 0:128], in_=raw.t.ap()[:, s * 128:(s + 1) * 128], identity=ident),
                     r=[raw, self.consts], w=[ps])
                P.op("act", lambda: nc.scalar.copy(out=a["vtok"].t.ap()[:, o + s * 128:o + (s + 1) * 128], in_=ps.t.ap()[:, 0:128]),
                     r=[ps], w=[a["vtok"]])
        for hh in range(4):
            h = kv * 4 + hh
            qT = a["qT"][h % 2]
            prep(CH_Q + h, 0, qT)
            for (o, n, j) in cfg.tiles:
                kchunks = list(range(0, CTX // 128)) if j == 1 else list(range(0, T // 128))
                psO, psZ = self.psum[6], self.psum[7]
                nk = len(kchunks)

                def s_mm(ix):
                    kc = kchunks[ix]
                    psS = self.psum[4 + (ix % 2)]
                    P.op("pe", lambda: nc.tensor.matmul(out=psS.t.ap()[:, 0:n], lhsT=a["kT"].t.ap()[:, kc * 128:(kc + 1) * 128],
                                                         rhs=qT.t.ap()[:, o:o + n], start=True, stop=True),
                         r=[a["kT"], qT], w=[psS])
                s_mm(0)
                for ix in range(nk):
                    if ix + 1 < nk:
                        s_mm(ix + 1)
                    kc = kchunks[ix]
                    psS = self.psum[4 + (ix % 2)]
                    pt = a["PT"][ix % 6]
                    P.op("act", lambda: nc.scalar.activation(out=pt.t.ap()[:, 0:n], in_=psS.t.ap()[:, 0:n], func=AF.Exp, scale=scale),
                         r=[psS], w=[pt])
                    last = (ix == nk - 1)
                    P.op("pe", lambda: nc.tensor.matmul(out=psO.t.ap()[:, 0:n], lhsT=a["vtok"].t.ap()[:, kc * 128:(kc + 1) * 128],
                                                         rhs=pt.t.ap()[:, 0:n], start=(ix == 0), stop=last),
                         r=[a["vtok"], pt], w=[psO], inc=last)
                    P.op("pe", lambda: nc.tensor.matmul(out=psZ.t.ap()[:, 0:n], lhsT=self.ones_b.t.ap(),
                                                         rhs=pt.t.ap()[:, 0:n], start=(ix == 0), stop=last),
                         r=[self.ones_b, pt], w=[psZ], inc=last)
                rz = a["rz"]
                P.op("dve", lambda: nc.vector.reciprocal(out=rz.t.ap()[:, 0:n], in_=psZ.t.ap()[:, 0:n]), r=[psZ], w=[rz])
                yo = a["yo"][self.ai % 2]
                self.ai += 1
                P.op("dve", lambda: nc.vector.tensor_tensor(out=yo.t.ap()[:, 0:n], in0=psO.t.ap()[:, 0:n], in1=rz.t.ap()[:, 0:n],
                                                            op=ALU.mult), r=[psO, rz], w=[yo])
                P.dma(self.yT.t.ap()[h * 128:(h + 1) * 128, o:o + n], yo.t.ap()[:, 0:n], r=[yo], w=[self.yT])
    P.barrier()


K.attention = attention


def rglru(self, l):
    P, nc, cfg = self.P, self.nc, self.cfg
    T, CTX = cfg.T, cfg.CTX
    P.barrier()
    B = [carve_buf(self.wbuf[0], 0, 2 * T, F32, "rB0"), carve_buf(self.wbuf[1], 0, 2 * T, F32, "rB1"),
         carve_buf(self.abuf[0], 0, 2 * T, F32, "rB2"), carve_buf(self.abuf[1], 0, 2 * T, F32, "rB3"),
         carve_buf(self.big[0], 0, T, None, "rB4"), carve_buf(self.big[1], 0, T, None, "rB5")]
    yb = carve_buf(self.wbuf[0], 2 * T, 3 * T, None, "ryb")
    wm = carve_buf(self.wbuf[1], 2 * T, 2 * T + 1024, F32, "rwm")
    vec = carve_buf(self.abuf[0], 2 * T, 2 * T + 64, F32, "rvec")
    segs = [(0, CTX), (CTX, T)]
    for c in range(8):
        rx, rg, xc, Br, Bi, hs = B
        P.dma(rx.t.ap(), self.plv((CH_RX + c) * 128, (CH_RX + c + 1) * 128)[:, :], r=[self.plA, self.plG], w=[rx])
        P.dma(rg.t.ap(), self.plv((CH_RG + c) * 128, (CH_RG + c + 1) * 128)[:, :], r=[self.plA, self.plG], w=[rg])
        P.dma(vec.t.ap()[:, 0:11], self.rnn_vec.t.ap()[l, :, c, :], r=[self.rnn_vec], w=[vec])
        P.dma(wm.t.ap(), self.rnn_w.t.ap()[l, :, c, :], r=[self.rnn_w], w=[wm])
        V = lambda i: vec.t.ap()[:, i:i + 1]
        for d in range(2):
            P.op("act", lambda: nc.scalar.activation(out=V(15 + d), in_=V(5 + d), func=AF.Exp, scale=-1.0), r=[vec], w=[vec])
            P.op("act", lambda: nc.scalar.activation(out=V(15 + d), in_=V(15 + d), func=AF.Ln, bias=1.0, scale=1.0), r=[vec], w=[vec])
            P.op("dve", lambda: nc.vector.tensor_scalar(out=V(11 + d), in0=V(15 + d), scalar1=-8.0, scalar2=None, op0=ALU.mult), r=[vec], w=[vec])
            P.op("dve", lambda: nc.vector.tensor_scalar(out=V(13 + d), in0=V(15 + d), scalar1=-16.0, scalar2=None, op0=ALU.mult), r=[vec], w=[vec])
        for (s0, s1) in segs:
            X = lambda a0, a1: rx.t.ap()[:, a0:a1]
            Y = lambda a0, a1: xc.t.ap()[:, a0:a1]
            P.op("act", lambda: nc.scalar.activation(out=Y(s0, s1), in_=X(s0, s1), func=AF.Identity, bias=V(4), scale=V(1)),
                 r=[rx, vec], w=[xc])
            P.op("dve", lambda: nc.vector.scalar_tensor_tensor(out=Y(s0 + 1, s1), in0=X(s0, s1 - 1), scalar=V(0), in1=Y(s0 + 1, s1),
                                                               op0=ALU.mult, op1=ALU.add), r=[rx, vec, xc], w=[xc])
            P.op("dve", lambda: nc.vector.scalar_tensor_tensor(out=Y(s0, s1 - 1), in0=X(s0 + 1, s1), scalar=V(2), in1=Y(s0, s1 - 1),
                                                               op0=ALU.mult, op1=ALU.add), r=[rx, vec, xc], w=[xc])
            P.op("dve", lambda: nc.vector.scalar_tensor_tensor(out=Y(s0, s1 - 2), in0=X(s0 + 2, s1), scalar=V(3), in1=Y(s0, s1 - 2),
                                                               op0=ALU.mult, op1=ALU.add), r=[rx, vec, xc], w=[xc])
        for d in range(2):
            for (o, n, j) in cfg.tiles:
                for gi, (dstb, bcol) in enumerate(((Br, 7 + d), (Bi, 9 + d))):
                    ps = self.psum[self.pi % 4]
                    self.pi += 1
                    wsl = wm.t.ap()[:, (gi * 2 + d) * 128:(gi * 2 + d + 1) * 128]
                    P.op("pe", lambda: nc.tensor.matmul(out=ps.t.ap()[:, 0:n], lhsT=wsl, rhs=xc.t.ap()[:, o:o + n], start=True, stop=True),
                         r=[wm, xc], w=[ps])
                    P.op("act", lambda: nc.scalar.activation(out=dstb.t.ap()[:, o:o + n], in_=ps.t.ap()[:, 0:n], func=AF.Sigmoid,
                                                             bias=V(bcol), scale=1.0), r=[ps, vec], w=[dstb])
            tmp = rx
            P.op("act", lambda: nc.scalar.activation(out=tmp.t.ap(), in_=Br.t.ap(), func=AF.Exp, scale=V(13 + d)), r=[Br, vec], w=[tmp])
            P.op("act", lambda: nc.scalar.activation(out=tmp.t.ap(), in_=tmp.t.ap(), func=AF.Sqrt, bias=1.0, scale=-1.0), r=[tmp], w=[tmp])
            P.op("act", lambda: nc.scalar.activation(out=Br.t.ap(), in_=Br.t.ap(), func=AF.Exp, scale=V(11 + d)), r=[Br, vec], w=[Br])
            P.op("dve", lambda: nc.vector.tensor_tensor(out=Bi.t.ap(), in0=Bi.t.ap(), in1=tmp.t.ap(), op=ALU.mult), r=[Bi, tmp], w=[Bi])
            P.op("dve", lambda: nc.vector.tensor_tensor(out=Bi.t.ap(), in0=Bi.t.ap(), in1=xc.t.ap(), op=ALU.mult), r=[Bi, xc], w=[Bi])
            hd = hs if d == 0 else tmp
            if d == 0:
                P.op("dve", lambda: nc.vector.tensor_tensor_scan(out=hd.t.ap()[:, 0:CTX], data0=Br.t.ap()[:, 0:CTX], data1=Bi.t.ap()[:, 0:CTX],
                                                                 initial=0.0, op0=ALU.mult, op1=ALU.add), r=[Br, Bi], w=[hd])
                P.op("dve", lambda: nc.vector.tensor_tensor_scan(out=hd.t.ap()[:, CTX:T], data0=Br.t.ap()[:, CTX:T], data1=Bi.t.ap()[:, CTX:T],
                                                                 initial=hd.t.ap()[:, CTX - 1:CTX], op0=ALU.mult, op1=ALU.add), r=[Br, Bi, hd], w=[hd])
            else:
                rev = lambda b_, a0, a1: (b_.t.ap()[:, a1 - 1::-1] if a0 == 0 else b_.t.ap()[:, a1 - 1:a0 - 1:-1])
                P.op("dve", lambda: nc.vector.tensor_tensor_scan(out=rev(hd, 0, CTX), data0=rev(Br, 0, CTX), data1=rev(Bi, 0, CTX),
                                                                 initial=0.0, op0=ALU.mult, op1=ALU.add), r=[Br, Bi], w=[hd])
                P.op("dve", lambda: nc.vector.tensor_tensor_scan(out=rev(hd, CTX, T), data0=rev(Br, CTX, T), data1=rev(Bi, CTX, T),
                                                                 initial=hd.t.ap()[:, 0:1], op0=ALU.mult, op1=ALU.add), r=[Br, Bi, hd], w=[hd])
                P.op("dve", lambda: nc.vector.tensor_tensor(out=hs.t.ap(), in0=hs.t.ap(), in1=hd.t.ap(), op=ALU.add), r=[hs, hd], w=[hs])
        g1 = Br
        P.op("act", lambda: nc.scalar.activation(out=g1.t.ap(), in_=rg.t.ap(), func=AF.Square), r=[rg], w=[g1])
        P.op("dve", lambda: nc.vector.tensor_scalar(out=g1.t.ap(), in0=g1.t.ap(), scalar1=0.044715, scalar2=1.0, op0=ALU.mult, op1=ALU.add),
             r=[g1], w=[g1])
        P.op("dve", lambda: nc.vector.tensor_tensor(out=g1.t.ap(), in0=g1.t.ap(), in1=rg.t.ap(), op=ALU.mult), r=[g1, rg], w=[g1])
        P.op("act", lambda: nc.scalar.activation(out=g1.t.ap(), in_=g1.t.ap(), func=AF.Sigmoid, scale=2.0 * 0.7978845608028654), r=[g1], w=[g1])
        P.op("dve", lambda: nc.vector.tensor_tensor(out=g1.t.ap(), in0=g1.t.ap(), in1=rg.t.ap(), op=ALU.mult), r=[g1, rg], w=[g1])
        P.op("dve", lambda: nc.vector.tensor_tensor(out=yb.t.ap(), in0=g1.t.ap(), in1=hs.t.ap(), op=ALU.mult), r=[g1, hs], w=[yb])
        P.dma(self.yT.t.ap()[1024 + c * 128:1024 + (c + 1) * 128, :], yb.t.ap(), r=[yb], w=[self.yT])
    P.barrier()


K.rglru = rglru


def setup_mix(self):
    P, cfg = self.P, self.cfg
    L = cfg.DEPTH
    self.yT = P.dram("yT", [3 * BW, cfg.T], BF16)
    self.yT.nt = True
    self.consts_in = self.inp("consts", [128, 256])
    self.consts = P.sb("consts_sb", [128, 256], F32)
    P.dma(self.consts.t.ap(), self.consts_in.t.ap(), r=[self.consts_in], w=[self.consts])
    self.qkn = self.inp("qkn", [L, 128, 2])
    self.rope = self.inp("rope", [2, 128, cfg.SEQ])
    self.rnn_vec = self.inp("rnn_vec", [L, 128, 8, 11])
    self.rnn_w = self.inp("rnn_w", [L, 128, 8, 512])


K.setup_mix = setup_mix


def setup_ssd(self):
    P, cfg = self.P, self.cfg
    L = cfg.DEPTH
    self.gT = P.dram("gT", [BW, cfg.T], F32)
    self.gT.nt = True
    self.ssd_c_in = self.inp("ssd_consts", [128, 512])
    self.ssd_c = P.sb("ssd_c_sb", [128, 512], F32)
    P.dma(self.ssd_c.t.ap(), self.ssd_c_in.t.ap(), r=[self.ssd_c_in], w=[self.ssd_c])
    self.ssd_hvec = self.inp("ssd_hvec", [L, 16, 64, 8])
    self.ssd_gvec = self.inp("ssd_gvec", [L, 2, 128, 10])
    self.ssd_dvec = self.inp("ssd_dvec", [L, 32, 2])


K.setup_ssd = setup_ssd


def ssd(self, l):
    P, nc, cfg = self.P, self.nc, self.cfg
    T, CTX = cfg.T, cfg.CTX
    NC_, NCc = T // 128, CTX // 128
    P.barrier()
    ident = self.consts.t.ap()[:, 0:128]
    TRI = [self.ssd_c.t.ap()[:, 0:128], self.ssd_c.t.ap()[:, 128:256]]
    MNEG = [self.ssd_c.t.ap()[:, 256:384], self.ssd_c.t.ap()[:, 384:512]]
    segs = [(0, CTX), (CTX, T)]
    W = NC_ * 32
    f0 = lambda i, nm: carve_buf(self.big[0], i * W, (i + 1) * W, None, nm)
    dt_tok, a_tok, acum, atotB, Wt, EA = [f0(i, "s%d" % i) for i in range(6)]
    sm0 = 6 * W
    sm = lambda off, n, nm, dt=None: carve_buf(self.big[0], sm0 + off, sm0 + off + n, dt, nm)
    a_bc, Ex, LT = sm(0, 128, "a_bc"), sm(128, 128, "Ex"), sm(256, 128, "LT")
    Mb = sm(384, 64, "Mb", BF16)
    Csf = sm(448, 128, "Csf")
    Csb = sm(576, 64, "Csb", BF16)
    xw = sm(640, 32, "xw", BF16)
    S = sm(672, 64, "S")
    STb = sm(736, 32, "STb", BF16)
    dvec = sm(768, 4, "dvec")
    hvec = sm(772, 8, "hvec")
    gvec = sm(780, 10, "gvecs")
    dr = carve_buf(self.wbuf[0], 0, 2 * T, F32, "dr")
    d2 = carve_buf(self.wbuf[1], 0, 2 * T, F32, "d2")
    d3 = carve_buf(self.abuf[0], 0, 2 * T, F32, "d3")
    R32 = lambda b_: b_.t.ap()[0:32, :]
    P.dma(R32(dr), self.plv(CH_DT * 128, CH_DT * 128 + 32)[:, :], r=[self.plA, self.plG], w=[dr])
    P.dma(dvec.t.ap()[0:32, 0:2], self.ssd_dvec.t.ap()[l, :, :], r=[self.ssd_dvec], w=[dvec])
    DV = lambda i: dvec.t.ap()[0:32, i:i + 1]
    P.op("act", lambda: nc.scalar.activation(out=R32(dr), in_=R32(dr), func=AF.Identity, bias=DV(0), scale=1.0), r=[dr, dvec], w=[dr])
    P.op("act", lambda: nc.scalar.activation(out=R32(d2), in_=R32(dr), func=AF.Abs), r=[dr], w=[d2])
    P.op("act", lambda: nc.scalar.activation(out=R32(d2), in_=R32(d2), func=AF.Exp, scale=-1.0), r=[d2], w=[d2])
    P.op("act", lambda: nc.scalar.activation(out=R32(d2), in_=R32(d2), func=AF.Ln, bias=1.0, scale=1.0), r=[d2], w=[d2])
    P.op("dve", lambda: nc.vector.scalar_tensor_tensor(out=R32(dr), in0=R32(dr), scalar=0.0, in1=R32(d2), op0=ALU.max, op1=ALU.add),
         r=[dr, d2], w=[dr])
    P.op("act", lambda: nc.scalar.activation(out=DV(2), in_=DV(1), func=AF.Exp), r=[dvec], w=[dvec])
    P.op("dve", lambda: nc.vector.tensor_scalar(out=DV(3), in0=DV(2), scalar1=-1.0, scalar2=None, op0=ALU.mult), r=[dvec], w=[dvec])
    P.op("dve", lambda: nc.vector.tensor_scalar(out=R32(d3), in0=R32(dr), scalar1=DV(3), scalar2=None, op0=ALU.mult), r=[dr, dvec], w=[d3])
    for k in range(NC_):
        for (src, dst) in ((dr, dt_tok), (d3, a_tok)):
            ps = self.psum[self.pi % 4]
            self.pi += 1
            P.op("pe", lambda: nc.tensor.transpose(out=ps.t.ap()[:, 0:32], in_=src.t.ap()[0:32, k * 128:(k + 1) * 128], identity=ident[0:32, 0:32]),
                 r=[src, self.consts], w=[ps])
            P.op("act", lambda: nc.scalar.copy(out=dst.t.ap()[:, k * 32:(k + 1) * 32], in_=ps.t.ap()[:, 0:32]), r=[ps], w=[dst])
    for k in range(NC_):
        ps = self.psum[self.pi % 4]
        self.pi += 1
        for d in range(2):
            P.op("pe", lambda: nc.tensor.matmul(out=ps.t.ap()[:, d * 16:(d + 1) * 16], lhsT=TRI[d], rhs=a_tok.t.ap()[:, k * 32 + d * 16:k * 32 + (d + 1) * 16],
                                                 start=True, stop=True), r=[self.ssd_c, a_tok], w=[ps], inc=(d == 1))
        P.op("dve", lambda: nc.vector.tensor_copy(out=acum.t.ap()[:, k * 32:(k + 1) * 32], in_=ps.t.ap()[:, 0:32]), r=[ps], w=[acum])
        ps2 = self.psum[self.pi % 4]
        self.pi += 1
        P.op("pe", lambda: nc.tensor.matmul(out=ps2.t.ap()[:, 0:32], lhsT=self.ones_f.t.ap(), rhs=a_tok.t.ap()[:, k * 32:(k + 1) * 32],
                                             start=True, stop=True), r=[self.ones_f, a_tok], w=[ps2])
        P.op("act", lambda: nc.scalar.copy(out=atotB.t.ap()[:, k * 32:(k + 1) * 32], in_=ps2.t.ap()[:, 0:32]), r=[ps2], w=[atotB])
    P.op("dve", lambda: nc.vector.tensor_tensor(out=Wt.t.ap(), in0=atotB.t.ap(), in1=acum.t.ap(), op=ALU.subtract), r=[atotB, acum], w=[Wt])
    P.op("act", lambda: nc.scalar.activation(out=Wt.t.ap(), in_=Wt.t.ap(), func=AF.Exp), r=[Wt], w=[Wt])
    P.op("dve", lambda: nc.vector.tensor_tensor(out=Wt.t.ap(), in0=Wt.t.ap(), in1=dt_tok.t.ap(), op=ALU.mult), r=[Wt, dt_tok], w=[Wt])
    P.op("act", lambda: nc.scalar.activation(out=EA.t.ap(), in_=atotB.t.ap(), func=AF.Exp), r=[atotB], w=[EA])
    P.barrier()

    def conv_silu(src, dst, vec_ap, np_, tmp):
        Vv = lambda i: vec_ap[0:np_, i:i + 1]
        for (s0, s1) in segs:
            X = lambda a0, a1: src.t.ap()[0:np_, a0:a1]
            Y = lambda a0, a1: tmp.t.ap()[0:np_, a0:a1]
            P.op("act", lambda: nc.scalar.activation(out=Y(s0, s1), in_=X(s0, s1), func=AF.Identity, bias=Vv(4), scale=Vv(1)), r=[src], w=[tmp])
            P.op("dve", lambda: nc.vector.scalar_tensor_tensor(out=Y(s0 + 1, s1), in0=X(s0, s1 - 1), scalar=Vv(0), in1=Y(s0 + 1, s1), op0=ALU.mult, op1=ALU.add), r=[src, tmp], w=[tmp])
            P.op("dve", lambda: nc.vector.scalar_tensor_tensor(out=Y(s0, s1 - 1), in0=X(s0 + 1, s1), scalar=Vv(2), in1=Y(s0, s1 - 1), op0=ALU.mult, op1=ALU.add), r=[src, tmp], w=[tmp])
            P.op("dve", lambda: nc.vector.scalar_tensor_tensor(out=Y(s0, s1 - 2), in0=X(s0 + 2, s1), scalar=Vv(3), in1=Y(s0, s1 - 2), op0=ALU.mult, op1=ALU.add), r=[src, tmp], w=[tmp])
        P.op("act", lambda: nc.scalar.activation(out=dst.t.ap()[0:np_, :], in_=tmp.t.ap()[0:np_, :], func=AF.Silu), r=[tmp], w=[dst])

    for g in range(2):
        Bf = carve_buf(self.wbuf[0], 0, T, None, "Bf")
        Cf = carve_buf(self.wbuf[0], T, 2 * T, None, "Cf")
        Btok = carve_buf(self.wbuf[0], 2 * T, 3 * T, None, "Btok")
        CBT = carve_buf(self.wbuf[1], 0, 2 * T, F32, "CBT")
        xtok = carve_buf(self.wbuf[1], 2 * T, 2 * T + T // 2, None, "xtok")
        raw = carve_buf(self.abuf[0], 0, 2 * T, F32, "sraw")
        cv = carve_buf(self.abuf[1], 0, 2 * T, F32, "scv")
        zt = carve_buf(self.big[1], 0, T, None, "zt")
        cvt = carve_buf(self.big[1], T, T + 2048, None, "cvt")
        P.dma(gvec.t.ap(), self.ssd_gvec.t.ap()[l, g, :, :], r=[self.ssd_gvec], w=[gvec])
        for bi, (chrow, dstb) in enumerate(((CH_SB + g, Bf), (CH_SC + g, Cf))):
            P.dma(raw.t.ap(), self.plv(chrow * 128, (chrow + 1) * 128)[:, :], r=[self.plA, self.plG], w=[raw])
            conv_silu(raw, zt, gvec.t.ap()[:, bi * 5:(bi + 1) * 5], 128, cv)
            P.op("dve", lambda: nc.vector.tensor_copy(out=dstb.t.ap(), in_=zt.t.ap()), r=[zt], w=[dstb])
            if bi == 0:
                for k in range(NC_):
                    ps = self.psum[self.pi % 4]
                    self.pi += 1
                    P.op("pe", lambda: nc.tensor.transpose(out=ps.t.ap()[:, 0:128], in_=zt.t.ap()[:, k * 128:(k + 1) * 128], identity=ident),
                         r=[zt, self.consts], w=[ps])
                    P.op("act", lambda: nc.scalar.copy(out=Btok.t.ap()[:, k * 128:(k + 1) * 128], in_=ps.t.ap()[:, 0:128]), r=[ps], w=[Btok])
        for k in range(NC_):
            ps = self.psum[self.pi % 4]
            self.pi += 1
            P.op("pe", lambda: nc.tensor.matmul(out=ps.t.ap()[:, 0:128], lhsT=Bf.t.ap()[:, k * 128:(k + 1) * 128], rhs=Cf.t.ap()[:, k * 128:(k + 1) * 128],
                                                 start=True, stop=True), r=[Bf, Cf], w=[ps])
            P.op("act", lambda: nc.scalar.copy(out=CBT.t.ap()[:, k * 128:(k + 1) * 128], in_=ps.t.ap()[:, 0:128]), r=[ps], w=[CBT])
        for hh in range(8):
            h = g * 8 + hh
            xs, ysum = raw, cv
            P.dma(hvec.t.ap()[0:64, :], self.ssd_hvec.t.ap()[l, h, :, :], r=[self.ssd_hvec], w=[hvec])
            HV = lambda i: hvec.t.ap()[0:64, i:i + 1]
            P.dma(zt.t.ap()[0:64, :], self.plv(CH_SX * 128 + h * 64, CH_SX * 128 + (h + 1) * 64)[:, :], r=[self.plA, self.plG], w=[zt])
            conv_silu(zt, xs, hvec.t.ap()[:, 0:5], 64, ysum)
            for k in range(NC_):
                ps = self.psum[self.pi % 4]
                self.pi += 1
                P.op("pe", lambda: nc.tensor.transpose(out=ps.t.ap()[:, 0:64], in_=xs.t.ap()[0:64, k * 128:(k + 1) * 128], identity=ident[0:64, 0:64]),
                     r=[xs, self.consts], w=[ps])
                P.op("act", lambda: nc.scalar.copy(out=xtok.t.ap()[:, k * 64:(k + 1) * 64], in_=ps.t.ap()[:, 0:64]), r=[ps], w=[xtok])
            P.op("dve", lambda: nc.vector.tensor_scalar(out=ysum.t.ap()[0:64, :], in0=xs.t.ap()[0:64, :], scalar1=HV(5), scalar2=None, op0=ALU.mult),
                 r=[xs, hvec], w=[ysum])
            for d in range(2):
                col = d * 16 + h
                P.op("dve", lambda: nc.vector.memset(S.t.ap(), 0.0), w=[S])
                P.op("dve", lambda: nc.vector.memset(STb.t.ap(), 0.0), w=[STb])
                order = list(range(NC_)) if d == 0 else (list(range(NCc - 1, -1, -1)) + list(range(NC_ - 1, NCc - 1, -1)))
                for k in order:
                    cc = k * 32 + col
                    P.op("dve", lambda: nc.vector.tensor_copy(out=a_bc.t.ap(), in_=a_tok.t.ap()[:, cc:cc + 1].to_broadcast([128, 128])), r=[a_tok], w=[a_bc])
                    psA = self.psum[4]
                    P.op("pe", lambda: nc.tensor.matmul(out=psA.t.ap()[:, 0:128], lhsT=a_bc.t.ap(), rhs=TRI[d], start=True, stop=True),
                         r=[a_bc, self.ssd_c], w=[psA])
                    P.op("dve", lambda: nc.vector.scalar_tensor_tensor(out=Ex.t.ap(), in0=psA.t.ap()[:, 0:128], scalar=acum.t.ap()[:, cc:cc + 1],
                                                                       in1=MNEG[d], op0=ALU.subtract, op1=ALU.add), r=[psA, acum, self.ssd_c], w=[Ex])
                    P.op("act", lambda: nc.scalar.activation(out=LT.t.ap(), in_=Ex.t.ap(), func=AF.Exp), r=[Ex], w=[LT])
                    P.op("dve", lambda: nc.vector.scalar_tensor_tensor(out=Mb.t.ap(), in0=LT.t.ap(), scalar=dt_tok.t.ap()[:, cc:cc + 1],
                                                                       in1=CBT.t.ap()[:, k * 128:(k + 1) * 128], op0=ALU.mult, op1=ALU.mult),
                         r=[LT, dt_tok, CBT], w=[Mb])
                    P.op("act", lambda: nc.scalar.activation(out=Csf.t.ap(), in_=psA.t.ap()[:, 0:128], func=AF.Exp), r=[psA], w=[Csf])
                    P.op("dve", lambda: nc.vector.tensor_tensor(out=Csb.t.ap(), in0=Csf.t.ap(), in1=Cf.t.ap()[:, k * 128:(k + 1) * 128], op=ALU.mult),
                         r=[Csf, Cf], w=[Csb])
                    psY = self.psum[5]
                    P.op("pe", lambda: nc.tensor.matmul(out=psY.t.ap()[0:64, 0:128], lhsT=xtok.t.ap()[:, k * 64:(k + 1) * 64], rhs=Mb.t.ap(),
                                                         start=True, stop=False), r=[xtok, Mb], w=[psY], inc=False)
                    P.op("pe", lambda: nc.tensor.matmul(out=psY.t.ap()[0:64, 0:128], lhsT=STb.t.ap(), rhs=Csb.t.ap(),
                                                         start=False, stop=True), r=[STb, Csb], w=[psY])
                    P.op("dve", lambda: nc.vector.tensor_tensor(out=ysum.t.ap()[0:64, k * 128:(k + 1) * 128], in0=ysum.t.ap()[0:64, k * 128:(k + 1) * 128],
                                                                in1=psY.t.ap()[0:64, 0:128], op=ALU.add), r=[ysum, psY], w=[ysum])
                    P.op("dve", lambda: nc.vector.tensor_scalar(out=xw.t.ap(), in0=xtok.t.ap()[:, k * 64:(k + 1) * 64], scalar1=Wt.t.ap()[:, cc:cc + 1],
                                                                scalar2=None, op0=ALU.mult), r=[xtok, Wt], w=[xw])
                    psS = self.psum[6]
                    P.op("pe", lambda: nc.tensor.matmul(out=psS.t.ap()[:, 0:64], lhsT=Btok.t.ap()[:, k * 128:(k + 1) * 128], rhs=xw.t.ap(),
                                                         start=True, stop=True), r=[Btok, xw], w=[psS])
                    P.op("dve", lambda: nc.vector.scalar_tensor_tensor(out=S.t.ap(), in0=S.t.ap(), scalar=EA.t.ap()[:, cc:cc + 1], in1=psS.t.ap()[:, 0:64],
                                                                       op0=ALU.mult, op1=ALU.add), r=[S, EA, psS], w=[S])
                    P.op("act", lambda: nc.scalar.copy(out=STb.t.ap(), in_=S.t.ap()), r=[S], w=[STb])
            P.dma(zt.t.ap()[0:64, :], self.plv(CH_SZ * 128 + h * 64, CH_SZ * 128 + (h + 1) * 64)[:, :], r=[self.plA, self.plG], w=[zt])
            P.op("act", lambda: nc.scalar.activation(out=zt.t.ap()[0:64, :], in_=zt.t.ap()[0:64, :], func=AF.Silu), r=[zt], w=[zt])
            P.op("dve", lambda: nc.vector.tensor_tensor(out=ysum.t.ap()[0:64, :], in0=ysum.t.ap()[0:64, :], in1=zt.t.ap()[0:64, :], op=ALU.mult),
                 r=[ysum, zt], w=[ysum])
            P.dma(self.gT.t.ap()[h * 64:(h + 1) * 64, :], ysum.t.ap()[0:64, :], r=[ysum], w=[self.gT])
    P.barrier()
    gt = [carve_buf(self.big[1], i * 1024, (i + 1) * 1024, None, "gt%d" % i) for i in range(8)]
    sq = carve_buf(self.big[0], 0, 512, None, "gsq")
    rs = carve_buf(self.big[0], 512, 1024, None, "grs")
    nv = carve_buf(self.big[0], 1024, 1024 + 128, None, "gnv")
    yb = [carve_buf(self.abuf[0], i * 512, (i + 1) * 512, None, "gyb%d" % i) for i in range(4)]
    P.dma(nv.t.ap()[0:64, :].rearrange("p (h e) -> p h e", e=8), self.ssd_hvec.t.ap()[l].rearrange("h p e -> p h e"), r=[self.ssd_hvec], w=[nv])
    yi = 0
    for g in range(2):
        for (o, n, j) in cfg.tiles:
            ps = self.psum[self.pi % 4]
            self.pi += 1
            for hh in range(8):
                h = g * 8 + hh
                P.dma(gt[hh].t.ap()[0:64, 0:n], self.gT.t.ap()[h * 64:(h + 1) * 64, o:o + n], r=[self.gT], w=[gt[hh]])
                P.op("act", lambda: nc.scalar.activation(out=sq.t.ap()[0:64, 0:n], in_=gt[hh].t.ap()[0:64, 0:n], func=AF.Square), r=[gt[hh]], w=[sq])
                P.op("pe", lambda: nc.tensor.matmul(out=ps.t.ap()[:, 0:n], lhsT=self.ones_f.t.ap()[0:64, :], rhs=sq.t.ap()[0:64, 0:n],
                                                     start=(hh == 0), stop=(hh == 7)), r=[self.ones_f, sq], w=[ps])
            P.op("dve", lambda: nc.vector.tensor_scalar(out=rs.t.ap()[:, 0:n], in0=ps.t.ap()[:, 0:n], scalar1=1.0 / 512, scalar2=EPS, op0=ALU.mult, op1=ALU.add),
                 r=[ps], w=[rs])
            P.op("act", lambda: nc.scalar.activation(out=rs.t.ap()[:, 0:n], in_=rs.t.ap()[:, 0:n], func=AF.Sqrt), r=[rs], w=[rs])
            P.op("dve", lambda: nc.vector.reciprocal(out=rs.t.ap()[:, 0:n], in_=rs.t.ap()[:, 0:n]), r=[rs], w=[rs])
            for hh in range(8):
                h = g * 8 + hh
                y_ = yb[yi % 4]
                yi += 1
                P.op("dve", lambda: nc.vector.scalar_tensor_tensor(out=y_.t.ap()[0:64, 0:n], in0=gt[hh].t.ap()[0:64, 0:n], scalar=nv.t.ap()[0:64, h * 8 + 6:h * 8 + 7],
                                                                   in1=rs.t.ap()[0:64, 0:n], op0=ALU.mult, op1=ALU.mult), r=[gt[hh], nv, rs], w=[y_])
                P.dma(self.yT.t.ap()[2048 + h * 64:2048 + (h + 1) * 64, o:o + n], y_.t.ap()[0:64, 0:n], r=[y_], w=[self.yT])
    P.barrier()


K.ssd = ssd


def setup_ffn(self):
    P, cfg = self.P, self.cfg
    L, T = cfg.DEPTH, cfg.T
    self.mT = P.dram("mT", [D, T], BF16)
    self.hidT = P.dram("hidT", [2 * D, T], BF16)
    self.mT.nt = True
    self.hidT.nt = True
    self.w_up = self.inp("w_up", [L, 32, 128, 3 * BW])
    self.w_o = self.inp("w_o", [L, 32, 128, D])
    self.w_gu = self.inp("w_gu", [L, 128, 128, D])
    self.w_dn = self.inp("w_dn", [L, 32, 128, 2 * D])
    self.router_w = self.inp("router_w", [128, KD * 16])
    self.router_b = self.inp("router_b", [1, 16])
    self.esel_in = self.inp("esel", [16, 16 * 128])
    self.g_final = self.inp("g_final", [128, KD])
    self.o_out = self.outp("o_out", [D, cfg.SEQ])
    self.mg = [carve_buf(self.big[0], i * 512, (i + 1) * 512, None, "mg%d" % i) for i in range(3)]
    self.mbf = [carve_buf(self.big[0], 1536 + i * 256, 1536 + (i + 1) * 256, BF16, "mbf%d" % i) for i in range(2)]
    self.mi = 0


K.setup_ffn = setup_ffn


def merge(self, l):
    P, nc, cfg = self.P, self.nc, self.cfg

    def evac(dc, tile, pss):
        o, n, j = tile
        for nb in range(3):
            gt = self.mg[nb]
            row = (CH_G + nb * 32 + dc) * 128
            P.dma(gt.t.ap()[:, 0:n], self.plv(row, row + 128)[:, o:o + n], r=[self.plA, self.plG], w=[gt])
            P.op("act", lambda: nc.scalar.activation(out=gt.t.ap()[:, 0:n], in_=gt.t.ap()[:, 0:n], func=AF.Sigmoid), r=[gt], w=[gt])
            P.op("dve", lambda: nc.vector.tensor_tensor(out=gt.t.ap()[:, 0:n], in0=gt.t.ap()[:, 0:n], in1=pss[nb].t.ap()[:, 0:n], op=ALU.mult),
                 r=[gt, pss[nb]], w=[gt])
        P.op("dve", lambda: nc.vector.tensor_tensor(out=self.mg[0].t.ap()[:, 0:n], in0=self.mg[0].t.ap()[:, 0:n], in1=self.mg[1].t.ap()[:, 0:n], op=ALU.add),
             r=[self.mg[0], self.mg[1]], w=[self.mg[0]])
        mb = self.mbf[self.mi % 2]
        self.mi += 1
        P.op("dve", lambda: nc.vector.tensor_tensor(out=mb.t.ap()[:, 0:n], in0=self.mg[0].t.ap()[:, 0:n], in1=self.mg[2].t.ap()[:, 0:n], op=ALU.add),
             r=[self.mg[0], self.mg[2]], w=[mb])
        P.dma(self.mT.t.ap()[dc * 128:(dc + 1) * 128, o:o + n], mb.t.ap()[:, 0:n], r=[mb], w=[self.mT], q="act")
    P.barrier()
    self.gemm(self.yT, 24, _LayerView(self.w_up, l), 32, cfg.tiles, evac, split=3)
    P.barrier()


K.merge = merge


def resid_gemm(self, l, act, KC, wbuf_in, kmod):
    P, nc, cfg = self.P, self.nc, self.cfg

    def evac(dc, tile, ps):
        o, n, j = tile
        xt = self.stage()
        P.dma(xt.t.ap()[:, 0:n], self.xT.t.ap()[dc * 128:(dc + 1) * 128, o:o + n], r=[self.xT], w=[xt])
        P.op("dve", lambda: nc.vector.scalar_tensor_tensor(out=xt.t.ap()[:, 0:n], in0=ps.t.ap()[:, 0:n], scalar=self.mod(kmod, dc, j),
                                                           in1=xt.t.ap()[:, 0:n], op0=ALU.mult, op1=ALU.add), r=[ps, self.modT, xt], w=[xt])
        P.dma(self.xT.t.ap()[dc * 128:(dc + 1) * 128, o:o + n], xt.t.ap()[:, 0:n], r=[xt], w=[self.xT], q="act")
    self.gemm(act, KC, _LayerView(wbuf_in, l), 32, cfg.tiles, evac)


K.resid_gemm = resid_gemm


def router(self, l):
    P, nc, cfg = self.P, self.nc, self.cfg
    T = cfg.T
    NC_ = T // 128
    P.barrier()
    W16 = NC_ * 16
    self.combT = carve_buf(self.big[1], 0, T, None, "combT")
    self.esel = carve_buf(self.big[1], T, T + 2048, None, "esel")
    self.combB = carve_buf(self.big[1], T + 2048, T + 2048 + 512, None, "combB")
    self.hsil = carve_buf(self.big[1], T + 2560, T + 2560 + 512, None, "hsil")
    self.hbf = [carve_buf(self.big[1], T + 3072 + i * 256, T + 3072 + (i + 1) * 256, BF16, "hbf%d" % i) for i in range(2)]
    P.dma(self.esel.t.ap()[0:16, :], self.esel_in.t.ap(), r=[self.esel_in], w=[self.esel])
    f = lambda i, nm: carve_buf(self.big[0], i * W16, (i + 1) * W16, None, nm)
    score, sel, msk, oh, tmp = f(0, "score"), f(1, "sel"), f(2, "msk"), f(3, "oh"), f(4, "rtmp")
    o2 = 5 * W16
    pairs = carve_buf(self.big[0], o2, o2 + NC_ * 24, None, "pairs")
    o2 += NC_ * 24
    gs = carve_buf(self.big[0], o2, o2 + NC_ * 4, None, "gs"); o2 += NC_ * 4
    ing = carve_buf(self.big[0], o2, o2 + NC_ * 4, None, "ing"); o2 += NC_ * 4
    v1 = carve_buf(self.big[0], o2, o2 + NC_, None, "v1"); o2 += NC_
    rb = carve_buf(self.big[0], o2, o2 + 16, None, "rb"); o2 += 16
    rw = carve_buf(self.big[0], o2, o2 + 256, BF16, "rw"); o2 += 256
    P.dma(rw.t.ap(), self.router_w.t.ap(), r=[self.router_w], w=[rw], q="pool")
    P.dma(rb.t.ap(), self.router_b.t.ap().partition_broadcast(128), r=[self.router_b], w=[rb])
    actv = self.hT.t.ap().rearrange("(kc p) t -> p kc t", p=128)
    for (o, n, j) in cfg.tiles:
        ab = self.abuf[self.ai % 2]
        self.ai += 1
        P.dma(ab.t.ap()[:, 0:KD * n].rearrange("p (kc t) -> p kc t", kc=KD), actv[:, :, o:o + n], r=[self.hT], w=[ab])
        for s_ in range(n // 128):
            ps = self.psum[self.pi % 4]
            self.pi += 1
            for kc in range(KD):
                P.op("pe", lambda: nc.tensor.matmul(out=ps.t.ap()[:, 0:16], lhsT=ab.t.ap()[:, kc * n + s_ * 128:kc * n + (s_ + 1) * 128],
                                                     rhs=rw.t.ap()[:, kc * 16:(kc + 1) * 16], start=(kc == 0), stop=(kc == KD - 1)),
                     r=[ab, rw], w=[ps], inc=(kc == KD - 1))
            ck = (o + s_ * 128) // 128
            P.op("act", lambda: nc.scalar.activation(out=score.t.ap()[:, ck * 16:(ck + 1) * 16], in_=ps.t.ap()[:, 0:16], func=AF.Sigmoid), r=[ps], w=[score])
    v3 = lambda b_, e: b_.t.ap().rearrange("p (c e) -> p c e", e=e)
    P.op("dve", lambda: nc.vector.tensor_tensor(out=v3(sel, 16), in0=v3(score, 16), in1=rb.t.ap().unsqueeze(1).to_broadcast([128, NC_, 16]), op=ALU.add),
         r=[score, rb], w=[sel])
    s4 = sel.t.ap().rearrange("p (c e) -> p c e", e=4)
    p6 = pairs.t.ap().rearrange("p (c e) -> p c e", e=6)
    for (po, pn, a0, b0) in ((0, 3, 0, 1), (3, 2, 0, 2), (5, 1, 0, 3)):
        P.op("dve", lambda: nc.vector.tensor_tensor(out=p6[:, :, po:po + pn], in0=s4[:, :, a0:a0 + pn], in1=s4[:, :, b0:b0 + pn], op=ALU.add),
             r=[sel], w=[pairs])
    P.op("dve", lambda: nc.vector.tensor_reduce(out=gs.t.ap(), in_=p6, axis=AX.X, op=ALU.max), r=[pairs], w=[gs])
    P.op("dve", lambda: nc.vector.tensor_reduce(out=v1.t.ap(), in_=v3(gs, 4), axis=AX.X, op=ALU.max), r=[gs], w=[v1])
    P.op("dve", lambda: nc.vector.tensor_tensor(out=v3(ing, 4), in0=v3(gs, 4), in1=v1.t.ap().unsqueeze(2).to_broadcast([128, NC_, 4]), op=ALU.is_equal),
         r=[gs, v1], w=[ing])
    P.op("dve", lambda: nc.vector.tensor_scalar(out=ing.t.ap(), in0=ing.t.ap(), scalar1=1e9, scalar2=-1e9, op0=ALU.mult, op1=ALU.add), r=[ing], w=[ing])
    P.op("dve", lambda: nc.vector.tensor_tensor(out=msk.t.ap().rearrange("p (c e) -> p c e", e=4), in0=s4,
                                                in1=ing.t.ap().unsqueeze(2).to_broadcast([128, NC_ * 4, 4]), op=ALU.add), r=[sel, ing], w=[msk])
    P.op("dve", lambda: nc.vector.tensor_reduce(out=v1.t.ap(), in_=v3(msk, 16), axis=AX.X, op=ALU.max), r=[msk], w=[v1])
    P.op("dve", lambda: nc.vector.tensor_tensor(out=v3(oh, 16), in0=v3(msk, 16), in1=v1.t.ap().unsqueeze(2).to_broadcast([128, NC_, 16]), op=ALU.is_equal),
         r=[msk, v1], w=[oh])
    P.op("dve", lambda: nc.vector.scalar_tensor_tensor(out=msk.t.ap(), in0=oh.t.ap(), scalar=-1e9, in1=msk.t.ap(), op0=ALU.mult, op1=ALU.add),
         r=[oh, msk], w=[msk])
    P.op("dve", lambda: nc.vector.tensor_reduce(out=v1.t.ap(), in_=v3(msk, 16), axis=AX.X, op=ALU.max), r=[msk], w=[v1])
    P.op("dve", lambda: nc.vector.tensor_tensor(out=v3(tmp, 16), in0=v3(msk, 16), in1=v1.t.ap().unsqueeze(2).to_broadcast([128, NC_, 16]), op=ALU.is_equal),
         r=[msk, v1], w=[tmp])
    P.op("dve", lambda: nc.vector.tensor_tensor(out=oh.t.ap(), in0=oh.t.ap(), in1=tmp.t.ap(), op=ALU.add), r=[oh, tmp], w=[oh])
    P.op("dve", lambda: nc.vector.tensor_tensor(out=oh.t.ap(), in0=oh.t.ap(), in1=score.t.ap(), op=ALU.mult), r=[oh, score], w=[oh])
    P.op("dve", lambda: nc.vector.tensor_reduce(out=v1.t.ap(), in_=v3(oh, 16), axis=AX.X, op=ALU.add), r=[oh], w=[v1])
    P.op("dve", lambda: nc.vector.reciprocal(out=v1.t.ap(), in_=v1.t.ap()), r=[v1], w=[v1])
    P.op("dve", lambda: nc.vector.tensor_tensor(out=v3(oh, 16), in0=v3(oh, 16), in1=v1.t.ap().unsqueeze(2).to_broadcast([128, NC_, 16]), op=ALU.mult),
         r=[oh, v1], w=[oh])
    ident = self.consts.t.ap()[:, 0:128]
    for ck in range(NC_):
        ps = self.psum[self.pi % 4]
        self.pi += 1
        P.op("pe", lambda: nc.tensor.transpose(out=ps.t.ap()[0:16, 0:128], in_=oh.t.ap()[:, ck * 16:(ck + 1) * 16], identity=ident),
             r=[oh, self.consts], w=[ps])
        P.op("act", lambda: nc.scalar.copy(out=self.combT.t.ap()[0:16, ck * 128:(ck + 1) * 128], in_=ps.t.ap()[0:16, 0:128]), r=[ps], w=[self.combT])
    P.barrier()


K.router = router


def moe(self, l):
    P, nc, cfg = self.P, self.nc, self.cfg
    held = [None]

    def evac(c, tile, ps):
        o, n, j = tile
        e, f, gu = c // 8, (c % 8) // 2, c % 2
        if c % 4 == 0:
            pc = self.psum[7]
            P.op("pe", lambda: nc.tensor.matmul(out=pc.t.ap()[:, 0:n], lhsT=self.esel.t.ap()[0:16, e * 128:(e + 1) * 128],
                                                 rhs=self.combT.t.ap()[0:16, o:o + n], start=True, stop=True), r=[self.esel, self.combT], w=[pc])
            P.op("act", lambda: nc.scalar.copy(out=self.combB.t.ap()[:, 0:n], in_=pc.t.ap()[:, 0:n]), r=[pc], w=[self.combB])
        if gu == 0:
            held[0] = ps
            return
        pg = held[0]
        P.op("act", lambda: nc.scalar.activation(out=self.hsil.t.ap()[:, 0:n], in_=pg.t.ap()[:, 0:n], func=AF.Silu), r=[pg], w=[self.hsil])
        P.op("dve", lambda: nc.vector.tensor_tensor(out=self.hsil.t.ap()[:, 0:n], in0=self.hsil.t.ap()[:, 0:n], in1=ps.t.ap()[:, 0:n], op=ALU.mult),
             r=[self.hsil, ps], w=[self.hsil])
        hb = self.hbf[self.mi % 2]
        self.mi += 1
        P.op("dve", lambda: nc.vector.tensor_tensor(out=hb.t.ap()[:, 0:n], in0=self.hsil.t.ap()[:, 0:n], in1=self.combB.t.ap()[:, 0:n], op=ALU.mult),
             r=[self.hsil, self.combB], w=[hb])
        row = e * 512 + f * 128
        P.dma(self.hidT.t.ap()[row:row + 128, o:o + n], hb.t.ap()[:, 0:n], r=[hb], w=[self.hidT], q="act")
    self.gemm(self.hT, KD, _LayerView(self.w_gu, l), 128, cfg.tiles, evac)
    P.barrier()
    self.resid_gemm(l, self.hidT, 64, self.w_dn, 5)


K.moe = moe


def final_norm(self):
    P, nc, cfg = self.P, self.nc, self.cfg
    P.barrier()
    gf = self.gvec
    P.dma(gf.t.ap()[:, 0:KD], self.g_final.t.ap(), r=[self.g_final], w=[gf])
    for ti, (o, n, j) in enumerate(cfg.tiles256):
        if j == 1:
            continue
        xt, sq = self.big[0], self.big[1]
        xv = xt.t.ap()[:, 0:KD * n].rearrange("p (kc t) -> p kc t", kc=KD)
        P.dma(xv, self.xT.t.ap().rearrange("(kc p) t -> p kc t", p=128)[:, :, o:o + n], r=[self.xT], w=[xt])
        P.op("act", lambda: nc.scalar.activation(out=sq.t.ap()[:, 0:KD * n], in_=xt.t.ap()[:, 0:KD * n], func=AF.Square), r=[xt], w=[sq])
        ps = self.psum[4 + (ti % 2)]
        for kc in range(KD):
            P.op("pe", lambda: nc.tensor.matmul(out=ps.t.ap()[:, 0:n], lhsT=self.ones_f.t.ap(), rhs=sq.t.ap()[:, kc * n:(kc + 1) * n],
                                                 start=(kc == 0), stop=(kc == KD - 1)), r=[self.ones_f, sq], w=[ps], inc=(kc == KD - 1))
        rs = self.stage()
        P.op("dve", lambda: nc.vector.tensor_scalar(out=rs.t.ap()[:, 0:n], in0=ps.t.ap()[:, 0:n], scalar1=1.0 / D, scalar2=EPS, op0=ALU.mult, op1=ALU.add),
             r=[ps], w=[rs])
        P.op("act", lambda: nc.scalar.activation(out=rs.t.ap()[:, 0:n], in_=rs.t.ap()[:, 0:n], func=AF.Sqrt), r=[rs], w=[rs])
        P.op("dve", lambda: nc.vector.reciprocal(out=rs.t.ap()[:, 0:n], in_=rs.t.ap()[:, 0:n]), r=[rs], w=[rs])
        P.op("dve", lambda: nc.vector.tensor_tensor(out=sq.t.ap()[:, 0:KD * n].rearrange("p (kc t) -> p kc t", kc=KD), in0=xv,
                                                    in1=rs.t.ap()[:, 0:n].unsqueeze(1).to_broadcast([128, KD, n]), op=ALU.mult), r=[xt, rs], w=[sq])
        for kc in range(KD):
            P.op("act", lambda: nc.scalar.activation(out=xt.t.ap()[:, kc * n:(kc + 1) * n], in_=sq.t.ap()[:, kc * n:(kc + 1) * n], func=AF.Identity,
                                                     scale=gf.t.ap()[:, kc:kc + 1]), r=[sq, gf], w=[xt])
        P.dma(self.o_out.t.ap().rearrange("(kc p) t -> p kc t", p=128)[:, :, o - cfg.CTX:o - cfg.CTX + n], xv, r=[xt], w=[self.o_out])


K.final_norm = final_norm


def build_all(cfg):
    k = K(cfg)
    k.setup(); k.setup_mix(); k.setup_ssd(); k.setup_ffn()
    P = k.P
    P.dma(k.xT.t.ap()[:, :], k.x_in.t.ap()[:, :], r=[k.x_in], w=[k.xT])
    P.barrier()
    for l in range(cfg.DEPTH):
        k.adaln(l)
        k.norm_mod(k.A1, 0, k.xT, k.hT)
        P.barrier()
        k.p1(l)
        k.attention(l)
        k.rglru(l)
        k.ssd(l)
        k.merge(l)
        k.resid_gemm(l, k.mT, KD, k.w_o, 2)
        P.barrier()
        k.norm_mod(k.A2, 3, k.xT, k.hT)
        k.router(l)
        k.moe(l)
        P.barrier()
    k.final_norm()
    P.finish([k.o_out])
    return k


def rope_tables(SEQ):
    GRID_W, NF = 64, 32
    t = np.arange(SEQ)
    row = (t // GRID_W).astype(np.float32); col = (t % GRID_W).astype(np.float32)
    inv = (np.float32(10000.0) ** (-np.arange(NF, dtype=np.float32) / NF)).astype(np.float32)
    ang = np.stack([row[:, None] * inv, col[:, None] * inv], 1)
    cos = np.cos(ang).astype(np.float32); sin = np.sin(ang).astype(np.float32)
    cosT = np.zeros((128, SEQ), np.float32); sinT = np.zeros((128, SEQ), np.float32)
    for d in range(128):
        ax, f = d // 64, d % 32
        cosT[d] = cos[:, ax, f]; sinT[d] = sin[:, ax, f]
    return np.stack([cosT, sinT])

def consts():
    c = np.zeros((128, 256), np.float32)
    c[:, :128] = np.eye(128, dtype=np.float32)
    R = np.zeros((128, 128), np.float32)
    for m in range(128):
        if (m % 64) < 32: R[m + 32, m] = -1.0
        else: R[m - 32, m] = 1.0
    c[:, 128:] = R
    return c

def prep(I, b, L, cfg):
    m = {}
    f32 = lambda a: np.ascontiguousarray(a, dtype=np.float32)
    m["x_in"] = f32(np.concatenate([I["ctx"][b], I["x"][b]], 0).T)
    cond = np.stack([I["c"][b], I["c_ctx"]], -1)
    m["cond"] = f32(cond.reshape(KD, 128, 2).transpose(1, 0, 2).reshape(128, KD * 2))
    m["w_mod_a"] = f32(np.stack([I["w_mod_a"][l].reshape(KD, 128, 256).transpose(1, 0, 2).reshape(128, KD * 256) for l in range(L)]))
    m["w_mod_b"] = f32(np.stack([np.stack([I["w_mod_b"][l][:, kk * D:(kk + 1) * D].reshape(2, 128, D).transpose(1, 0, 2).reshape(128, 2 * D) for kk in range(6)]) for l in range(L)]))
    m["b_mod"] = f32(np.stack([I["b_mod"][l].reshape(6, KD, 128).transpose(2, 0, 1).reshape(128, 6 * KD) for l in range(L)]))
    m["g_mix"] = f32(np.stack([vlay(I["g_mix"][l]) for l in range(L)]))
    m["g_ffn"] = f32(np.stack([vlay(I["g_ffn"][l]) for l in range(L)]))
    ws = []
    for l in range(L):
        W = I["w_in"][l]
        W2 = np.concatenate([W[:, :6144], W[:, MIXC:], W[:, 6144:MIXC], np.zeros((D, 96), np.float32)], 1)
        ws.append(wlay(W2))
    m["w_in"] = f32(np.stack(ws))
    m["consts"] = consts()
    m["qkn"] = f32(np.stack([np.stack([I["q_norm"][l], I["k_norm"][l]], -1) for l in range(L)]))
    m["rope"] = rope_tables(cfg.SEQ)
    rv = np.zeros((L, 128, 8, 11), np.float32)
    rw = np.zeros((L, 128, 8, 512), np.float32)
    for l in range(L):
        for c in range(8):
            sl = slice(c * 128, (c + 1) * 128)
            rv[l, :, c, 0:4] = I["rnn_conv_w"][l][:, sl].T
            rv[l, :, c, 4] = I["rnn_conv_b"][l][sl]
            for d in range(2):
                rv[l, :, c, 5 + d] = I["rnn_lambda"][l][d][sl]
                rv[l, :, c, 7 + d] = I["rnn_b_r"][l][d][sl]
                rv[l, :, c, 9 + d] = I["rnn_b_i"][l][d][sl]
                rw[l, :, c, (0 * 2 + d) * 128:(0 * 2 + d + 1) * 128] = I["rnn_w_r"][l][d][c]
                rw[l, :, c, (1 * 2 + d) * 128:(1 * 2 + d + 1) * 128] = I["rnn_w_i"][l][d][c]
    m["rnn_vec"] = rv; m["rnn_w"] = rw
    return m

def ssd_consts():
    c = np.zeros((128, 512), np.float32)
    j = np.arange(128)[:, None]; i = np.arange(128)[None, :]
    c[:, 0:128] = (j <= i); c[:, 128:256] = (j >= i)
    c[:, 256:384] = np.where(j <= i, 0.0, -30000.0); c[:, 384:512] = np.where(j >= i, 0.0, -30000.0)
    return c

def prep_ssd(I, L):
    m = {}
    m["ssd_consts"] = ssd_consts()
    hv = np.zeros((L, 16, 64, 8), np.float32); gv = np.zeros((L, 2, 128, 10), np.float32); dv = np.zeros((L, 32, 2), np.float32)
    for l in range(L):
        cw, cb = I["ssd_conv_w"][l], I["ssd_conv_b"][l]
        for h in range(16):
            sl = slice(h * 64, (h + 1) * 64)
            hv[l, h, :, 0:4] = cw[:, sl].T; hv[l, h, :, 4] = cb[sl]
            hv[l, h, :, 5] = I["ssd_d"][l][h]; hv[l, h, :, 6] = I["ssd_norm"][l][sl]
        for g in range(2):
            for bi in range(2):
                sl = slice(1024 + bi * 256 + g * 128, 1024 + bi * 256 + (g + 1) * 128)
                gv[l, g, :, bi * 5:bi * 5 + 4] = cw[:, sl].T; gv[l, g, :, bi * 5 + 4] = cb[sl]
        for d in range(2):
            dv[l, d * 16:(d + 1) * 16, 0] = I["ssd_dt_bias"][l][d]; dv[l, d * 16:(d + 1) * 16, 1] = I["ssd_a_log"][l][d]
    m["ssd_hvec"] = hv; m["ssd_gvec"] = gv; m["ssd_dvec"] = dv
    return m

def prep_ffn(I, L):
    m = {}
    f32 = lambda a: np.ascontiguousarray(a, dtype=np.float32)
    m["w_up"] = f32(np.stack([wlay(I["w_up"][l].reshape(3 * 1024, D)) for l in range(L)]))
    m["w_o"] = f32(np.stack([wlay(I["w_o"][l]) for l in range(L)]))
    gus = []
    for l in range(L):
        wg, wu = I["moe_w_gate"][l], I["moe_w_up"][l]
        cols = []
        for e in range(16):
            for f in range(4):
                cols.append(wg[e][:, f * 128:(f + 1) * 128]); cols.append(wu[e][:, f * 128:(f + 1) * 128])
        gus.append(wlay(np.concatenate(cols, 1)))
    m["w_gu"] = f32(np.stack(gus))
    m["w_dn"] = f32(np.stack([wlay(I["moe_w_down"][l].reshape(16 * 512, D)) for l in range(L)]))
    m["router_w"] = f32(I["router_w"].reshape(KD, 128, 16).transpose(1, 0, 2).reshape(128, KD * 16))
    m["router_b"] = f32(I["router_b"].reshape(1, 16))
    es = np.zeros((16, 16 * 128), np.float32)
    for e in range(16): es[e, e * 128:(e + 1) * 128] = 1.0
    m["esel"] = es
    m["g_final"] = f32(vlay(I["g_final"]))
    return m


PER_LAYER = ("w_mod_a", "w_mod_b", "b_mod", "g_mix", "g_ffn", "w_in", "w_up", "w_o", "q_norm", "k_norm",
             "rnn_conv_w", "rnn_conv_b", "rnn_lambda", "rnn_w_r", "rnn_b_r", "rnn_w_i", "rnn_b_i",
             "ssd_conv_w", "ssd_conv_b", "ssd_dt_bias", "ssd_a_log", "ssd_d", "ssd_norm",
             "moe_w_gate", "moe_w_up", "moe_w_down")


def build_layer(cfg):
    k = K(cfg)
    k.setup(); k.setup_mix(); k.setup_ssd(); k.setup_ffn()
    P = k.P
    P.dma(k.xT.t.ap()[:, :], k.x_in.t.ap()[:, :], r=[k.x_in], w=[k.xT])
    P.barrier()
    k.adaln(0)
    k.norm_mod(k.A1, 0, k.xT, k.hT)
    P.barrier()
    k.p1(0)
    k.attention(0)
    k.rglru(0)
    k.ssd(0)
    k.merge(0)
    k.resid_gemm(0, k.mT, KD, k.w_o, 2)
    P.barrier()
    k.norm_mod(k.A2, 3, k.xT, k.hT)
    k.router(0)
    k.moe(0)
    P.barrier()
    o_x = k.outp("o_x", [D, cfg.T])
    P.dma(o_x.t.ap()[:, :], k.xT.t.ap()[:, :], r=[k.xT], w=[o_x])
    k.final_norm()
    P.finish([k.o_out, o_x])
    return k


def kernel(**inputs):
    I = {k_: np.asarray(v) for k_, v in inputs.items()}
    B, SEQ, _ = I["x"].shape
    CTX = I["ctx"].shape[1]
    L = I["w_in"].shape[0]
    cfg = Cfg(CTX, SEQ, 1)
    k = build_layer(cfg)
    xs = [np.ascontiguousarray(np.concatenate([I["ctx"][b], I["x"][b]], 0).T, dtype=np.float32) for b in range(B)]
    conds = []
    for b in range(B):
        cond = np.stack([I["c"][b], I["c_ctx"]], -1)
        conds.append(np.ascontiguousarray(cond.reshape(KD, 128, 2).transpose(1, 0, 2).reshape(128, KD * 2), dtype=np.float32))
    res = None
    for l in range(L):
        Il = {k_: (v[l:l + 1] if k_ in PER_LAYER else v) for k_, v in I.items()}
        shared = prep(Il, 0, 1, cfg)
        shared.update(prep_ssd(Il, 1))
        shared.update(prep_ffn(Il, 1))
        in_maps = []
        for b in range(B):
            m = dict(shared)
            m["x_in"] = xs[b]
            m["cond"] = conds[b]
            in_maps.append({k_: v for k_, v in m.items() if k_ in k.inputs})
        res = run_bass_kernel_spmd(k.nc, in_maps, core_ids=list(range(B)))
        xs = [np.ascontiguousarray(res.results[b]["o_x"], dtype=np.float32) for b in range(B)]
        del shared, in_maps
    out = np.stack([np.ascontiguousarray(res.results[b]["o_out"].T) for b in range(B)]).astype(np.float32)
    return out
```

```python
import numpy as np
import concourse.bass as bass
import concourse.mybir as mybir
from concourse.bass_utils import run_bass_kernel_spmd

F32 = mybir.dt.float32
BF16 = mybir.dt.bfloat16
I32 = mybir.dt.int32
AF = mybir.ActivationFunctionType
ALU = mybir.AluOpType
AX = mybir.AxisListType


class Buf:
    __slots__ = ("t", "w", "r", "name", "nt")

    def __init__(self, t, name=""):
        self.t = t
        self.w = None
        self.r = {}
        self.name = name
        self.nt = False

    def __getitem__(self, idx):
        return self.t[idx]


class Prog:
    ENG = ("pe", "dve", "act", "pool", "sp")

    def __init__(self, nc, n_dma_sems=48):
        self.nc = nc
        self.e = {"pe": nc.tensor, "dve": nc.vector, "act": nc.scalar, "pool": nc.gpsimd, "sp": nc.sync}
        self.sem = {}
        self.cnt = {}
        for k in self.ENG:
            self.sem[k] = nc.alloc_semaphore("s_" + k)
            self.cnt[k] = 0
        self.dsem = [nc.alloc_semaphore("d%d" % i) for i in range(n_dma_sems)]
        self.dcnt = [0] * n_dma_sems
        self.dnext = 0
        for i in range(n_dma_sems):
            self.sem[("d", i)] = self.dsem[i]
        self.seen = {k: {} for k in self.ENG}
        self.pend = {k: [] for k in self.ENG}
        self.ninst = 0

    def sb(self, name, shape, dt=F32):
        return Buf(self.nc.alloc_sbuf_tensor(name, list(shape), dt), name)

    def ps(self, name, shape, dt=F32):
        return Buf(self.nc.alloc_psum_tensor(name, list(shape), dt), name)

    def dram(self, name, shape, dt=F32, kind="Internal"):
        return Buf(self.nc.dram_tensor(name, list(shape), dt, kind=kind), name)

    def _wait(self, eng, ev):
        if ev is None:
            return
        key, val = ev
        if self.seen[eng].get(key, 0) >= val:
            return
        if key == eng and eng == "pe":
            return
        self.e[eng].wait_ge(self.sem[key], val)
        self.seen[eng][key] = val

    def _waitw(self, eng, w):
        if w is None:
            return
        if isinstance(w, list):
            for ev in w:
                self._wait(eng, ev)
        else:
            self._wait(eng, w)

    def _deps(self, eng, reads, writes):
        for b in reads:
            self._waitw(eng, b.w)
        for b in writes:
            self._waitw(eng, b.w)
            for k, v in b.r.items():
                self._wait(eng, (k, v))

    def _mark(self, ev, reads, writes):
        for b in reads:
            if not b.nt:
                b.r[ev[0]] = ev[1]
        for b in writes:
            if not b.nt:
                b.w = ev
                b.r = {}

    def op(self, eng, fn, r=(), w=(), inc=True):
        self._deps(eng, r, w)
        inst = fn()
        self.ninst += 1
        if not inc:
            self.pend[eng].append((r, w))
            return None
        self.cnt[eng] += 1
        inst.then_inc(self.sem[eng], 1)
        ev = (eng, self.cnt[eng])
        for pr, pw in self.pend[eng]:
            self._mark(ev, pr, pw)
        self.pend[eng] = []
        self._mark(ev, r, w)
        return ev

    def dma(self, out, in_, r=(), w=(), q="sp", **kw):
        i = self.dnext
        self.dnext = (self.dnext + 1) % len(self.dsem)
        key = ("d", i)
        if self.dcnt[i] > 0:
            self._wait(q, (key, self.dcnt[i]))
        self._deps(q, r, w)
        inst = self.e[q].dma_start(out=out, in_=in_, **kw)
        self.dcnt[i] += 16
        inst.then_inc(self.dsem[i], 16)
        ev = (key, self.dcnt[i])
        self._mark(ev, r, w)
        self.ninst += 1
        return ev

    def dma_multi(self, pairs, r=(), w=(), q="sp", **kw):
        self._deps(q, r, w)
        evs = []
        for (out, in_) in pairs:
            i = self.dnext
            self.dnext = (self.dnext + 1) % len(self.dsem)
            key = ("d", i)
            if self.dcnt[i] > 0:
                self._wait(q, (key, self.dcnt[i]))
            inst = self.e[q].dma_start(out=out, in_=in_, **kw)
            self.dcnt[i] += 16
            inst.then_inc(self.dsem[i], 16)
            evs.append((key, self.dcnt[i]))
            self.ninst += 1
        for b in r:
            if not b.nt:
                for ev in evs:
                    b.r[ev[0]] = ev[1]
        for b in w:
            if not b.nt:
                b.w = list(evs)
                b.r = {}
        return evs

    def barrier(self):
        for eng in self.ENG:
            for k in self.ENG:
                if k != eng and self.cnt[k] > 0:
                    self._wait(eng, (k, self.cnt[k]))
            for i in range(len(self.dsem)):
                if self.dcnt[i] > 0:
                    self._wait(eng, (("d", i), self.dcnt[i]))

    def finish(self, bufs):
        for b in bufs:
            self._waitw("sp", b.w)


import math

D = 4096
KD = 32
NB = 3
BW = 1024
MIXC = 6176
NCH_IN = 145
CH_Q, CH_K, CH_V, CH_RX, CH_RG, CH_SZ, CH_SX, CH_SB, CH_SC, CH_G, CH_DT = 0, 8, 10, 12, 20, 28, 36, 44, 46, 48, 144
EPS = 1e-6
WCAP = 16384
ACAP = 16384


class Cfg:
    def __init__(self, CTX, SEQ, DEPTH):
        self.CTX, self.SEQ, self.DEPTH = CTX, SEQ, DEPTH
        self.T = CTX + SEQ
        self.tiles = []
        for o in range(0, CTX, 512):
            self.tiles.append((o, min(512, CTX - o), 1))
        for o in range(0, SEQ, 512):
            self.tiles.append((CTX + o, min(512, SEQ - o), 0))
        self.tiles256 = []
        for (o, n, j) in self.tiles:
            for oo in range(0, n, 256):
                self.tiles256.append((o + oo, min(256, n - oo), j))


def wlay(W):
    K, N = W.shape
    KC, NC = K // 128, N // 128
    return np.ascontiguousarray(W.reshape(KC, 128, NC, 128).transpose(2, 1, 0, 3).reshape(NC, 128, KC * 128))


def vlay(v):
    return np.ascontiguousarray(v.reshape(-1, 128).T)


class K:
    def __init__(self, cfg, debug=()):
        self.cfg = cfg
        self.debug = set(debug)
        nc = bass.Bass("TRN2", target_bir_lowering=False)
        self.nc = nc
        self.P = Prog(nc)
        self.inputs = {}
        self.outputs = {}

    def inp(self, name, shape, dt=F32):
        b = self.P.dram(name, shape, dt, kind="ExternalInput")
        self.inputs[name] = b
        return b

    def outp(self, name, shape, dt=F32):
        b = self.P.dram(name, shape, dt, kind="ExternalOutput")
        self.outputs[name] = b
        return b

    def gemm(self, act, KC, wd, nch, tiles, evac, c0=0, tok_cap=512, split=1):
        P, nc = self.P, self.nc
        G = max(1, min(nch, WCAP // (KC * 128)))
        ntmax = min(tok_cap, ACAP // KC)
        tl = []
        for (o, n, j) in tiles:
            for oo in range(0, n, ntmax):
                tl.append((o + oo, min(ntmax, n - oo), j))
        actv = act.t.ap().rearrange("(kc p) t -> p kc t", p=128)
        gi = 0
        for g0 in range(0, nch, G):
            gn = min(G, nch - g0)
            wb = self.wbuf[gi % 2]
            gi += 1
            P.dma_multi([(wb.t.ap()[:, c * KC * 128:(c + 1) * KC * 128], wd.t.ap()[c0 + g0 + c, :, :]) for c in range(gn)],
                        r=[wd], w=[wb], q="pool", max_dma_last_dim=8192)
            for (o, n, j) in tl:
                ab = self.abuf[self.ai % 2]
                self.ai += 1
                adst = ab.t.ap()[:, 0:KC * n].rearrange("p (kc t) -> p kc t", kc=KC)
                P.dma(adst, actv[:, :, o:o + n], r=[act], w=[ab], q="sp")
                for c in range(gn):
                    if split > 1:
                        kg = KC // split
                        pss = [self.psum[(self.pi % 2) * 3 + s_] for s_ in range(split)]
                        self.pi += 1
                        for kc in range(KC):
                            ps = pss[kc // kg]
                            P.op("pe", lambda: nc.tensor.matmul(
                                out=ps.t.ap()[:, 0:n],
                                lhsT=wb.t.ap()[:, (c * KC + kc) * 128:(c * KC + kc + 1) * 128],
                                rhs=ab.t.ap()[:, kc * n:(kc + 1) * n],
                                start=(kc % kg == 0), stop=(kc % kg == kg - 1)),
                                r=[wb, ab], w=[ps], inc=(kc % kg == kg - 1))
                        evac(g0 + c, (o, n, j), pss)
                        continue
                    ps = self.psum[self.pi % 4]
                    self.pi += 1
                    for kc in range(KC):
                        P.op("pe", lambda: nc.tensor.matmul(
                            out=ps.t.ap()[:, 0:n],
                            lhsT=wb.t.ap()[:, (c * KC + kc) * 128:(c * KC + kc + 1) * 128],
                            rhs=ab.t.ap()[:, kc * n:(kc + 1) * n],
                            start=(kc == 0), stop=(kc == KC - 1)),
                            r=[wb, ab], w=[ps], inc=(kc == KC - 1))
                    evac(g0 + c, (o, n, j), ps)

    def setup(self):
        P, nc, cfg = self.P, self.nc, self.cfg
        self.wbuf = [P.sb("wbuf%d" % i, [128, WCAP], BF16) for i in range(2)]
        self.abuf = [P.sb("abuf%d" % i, [128, ACAP], BF16) for i in range(2)]
        self.psum = [P.ps("ps%d" % i, [128, 512], F32) for i in range(8)]
        self.ai = 0
        self.pi = 0
        self.stg = [P.sb("stg%d" % i, [128, 512], F32) for i in range(4)]
        self.si = 0
        self.ones_f = P.sb("ones_f", [128, 128], F32)
        P.op("dve", lambda: nc.vector.memset(self.ones_f.t.ap(), 1.0), w=[self.ones_f])
        self.ones_b = P.sb("ones_b", [128, 128], BF16)
        P.op("dve", lambda: nc.vector.memset(self.ones_b.t.ap(), 1.0), w=[self.ones_b])
        T = cfg.T
        self.xT = P.dram("xT", [D, T], F32)
        self.hT = P.dram("hT", [D, T], BF16)
        self.plA = P.dram("plA", [49 * 128, T], F32)
        self.plG = P.dram("plG", [96 * 128, T], F32)
        for b_ in (self.xT, self.hT, self.plA, self.plG):
            b_.nt = True
        self.x_in = self.inp("x_in", [D, T])
        self.cond = self.inp("cond", [128, KD * 2])
        L = cfg.DEPTH
        self.w_mod_a = self.inp("w_mod_a", [L, 128, KD * 256])
        self.w_mod_b = self.inp("w_mod_b", [L, 6, 128, 2 * D])
        self.b_mod = self.inp("b_mod", [L, 128, 6 * KD])
        self.g_mix = self.inp("g_mix", [L, 128, KD])
        self.g_ffn = self.inp("g_ffn", [L, 128, KD])
        self.w_in = self.inp("w_in", [L, NCH_IN, 128, D])
        self.modT = P.sb("modT", [128, 6 * KD * 2], F32)
        self.A1 = P.sb("A1", [128, KD * 2], F32)
        self.A2 = P.sb("A2", [128, KD * 2], F32)
        self.sc = P.sb("sc", [128, KD * 2], F32)
        self.t1 = P.sb("t1", [128, 4], F32)
        self.gvec = P.sb("gvec", [128, 2 * KD], F32)
        self.bmod = P.sb("bmod", [128, 6 * KD], F32)
        self.big = [P.sb("big%d" % i, [128, 8192], F32) for i in range(2)]

    def plv(self, r0, r1):
        c = r0 // 128
        if c < 48:
            return self.plA.t.ap()[r0:r1, :]
        if c == 144:
            return self.plA.t.ap()[r0 - 96 * 128:r1 - 96 * 128, :]
        return self.plG.t.ap()[r0 - 48 * 128:r1 - 48 * 128, :]

    def stage(self):
        b = self.stg[self.si % 4]
        self.si += 1
        return b

    def adaln(self, l):
        P, nc = self.P, self.nc
        ct = self.stage()
        P.dma(ct.t.ap()[:, 0:KD * 2], self.cond.t.ap()[:, :], r=[self.cond], w=[ct])
        P.op("act", lambda: nc.scalar.activation(out=self.sc.t.ap(), in_=ct.t.ap()[:, 0:KD * 2], func=AF.Silu),
             r=[ct], w=[self.sc])
        wa = self.big[0]
        P.dma(wa.t.ap()[:, 0:KD * 256], self.w_mod_a.t.ap()[l, :, :], r=[self.w_mod_a], w=[wa])
        ps = self.psum[4]
        for rc in range(2):
            for kc in range(KD):
                P.op("pe", lambda: nc.tensor.matmul(
                    out=ps.t.ap()[:, rc * 2:rc * 2 + 2],
                    lhsT=wa.t.ap()[:, kc * 256 + rc * 128: kc * 256 + rc * 128 + 128],
                    rhs=self.sc.t.ap()[:, kc * 2:kc * 2 + 2],
                    start=(kc == 0), stop=(kc == KD - 1)), r=[wa, self.sc], w=[ps], inc=(kc == KD - 1 and rc == 1))
        P.op("dve", lambda: nc.vector.tensor_copy(out=self.t1.t.ap(), in_=ps.t.ap()[:, 0:4]), r=[ps], w=[self.t1])
        P.dma(self.bmod.t.ap(), self.b_mod.t.ap()[l, :, :], r=[self.b_mod], w=[self.bmod])
        P.dma(self.gvec.t.ap()[:, 0:KD], self.g_mix.t.ap()[l, :, :], r=[self.g_mix], w=[self.gvec])
        P.dma(self.gvec.t.ap()[:, KD:2 * KD], self.g_ffn.t.ap()[l, :, :], r=[self.g_ffn], w=[self.gvec])
        for k in range(6):
            wbm = self.big[k % 2]
            P.dma(wbm.t.ap(), self.w_mod_b.t.ap()[l, k, :, :], r=[self.w_mod_b], w=[wbm])
            ps = self.psum[5 + (k % 2)]
            for dc in range(KD):
                for rc in range(2):
                    P.op("pe", lambda: nc.tensor.matmul(
                        out=ps.t.ap()[:, dc * 2:dc * 2 + 2],
                        lhsT=wbm.t.ap()[:, rc * D + dc * 128: rc * D + dc * 128 + 128],
                        rhs=self.t1.t.ap()[:, rc * 2:rc * 2 + 2],
                        start=(rc == 0), stop=(rc == 1)), r=[wbm, self.t1], w=[ps], inc=(rc == 1 and dc == KD - 1))
            P.op("dve", lambda: nc.vector.tensor_tensor(
                out=self.modT.t.ap()[:, k * 64:(k + 1) * 64].rearrange("p (d j) -> p d j", j=2),
                in0=ps.t.ap()[:, 0:64].rearrange("p (d j) -> p d j", j=2),
                in1=self.bmod.t.ap()[:, k * KD:(k + 1) * KD].unsqueeze(2).to_broadcast([128, KD, 2]),
                op=ALU.add), r=[ps, self.bmod], w=[self.modT])
        for (A, kk, go) in ((self.A1, 1, 0), (self.A2, 4, KD)):
            P.op("dve", lambda: nc.vector.scalar_tensor_tensor(
                out=A.t.ap().rearrange("p (d j) -> p d j", j=2),
                in0=self.modT.t.ap()[:, kk * 64:(kk + 1) * 64].rearrange("p (d j) -> p d j", j=2),
                scalar=1.0,
                in1=self.gvec.t.ap()[:, go:go + KD].unsqueeze(2).to_broadcast([128, KD, 2]),
                op0=ALU.add, op1=ALU.mult), r=[self.modT, self.gvec], w=[A])

    def mod(self, k, dc, j):
        return self.modT.t.ap()[:, k * 64 + dc * 2 + j: k * 64 + dc * 2 + j + 1]

    def norm_mod(self, A, kshift, src, dst):
        P, nc, cfg = self.P, self.nc, self.cfg
        for ti, (o, n, j) in enumerate(cfg.tiles256):
            xt = self.big[0]
            xv = xt.t.ap()[:, 0:KD * n].rearrange("p (kc t) -> p kc t", kc=KD)
            P.dma(xv, src.t.ap().rearrange("(kc p) t -> p kc t", p=128)[:, :, o:o + n], r=[src], w=[xt])
            sq = self.big[1]
            P.op("act", lambda: nc.scalar.activation(out=sq.t.ap()[:, 0:KD * n], in_=xt.t.ap()[:, 0:KD * n], func=AF.Square),
                 r=[xt], w=[sq])
            ps = self.psum[4 + (ti % 2)]
            for kc in range(KD):
                P.op("pe", lambda: nc.tensor.matmul(out=ps.t.ap()[:, 0:n], lhsT=self.ones_f.t.ap(),
                                                     rhs=sq.t.ap()[:, kc * n:(kc + 1) * n],
                                                     start=(kc == 0), stop=(kc == KD - 1)),
                     r=[self.ones_f, sq], w=[ps], inc=(kc == KD - 1))
            rs = self.stage()
            P.op("dve", lambda: nc.vector.tensor_scalar(out=rs.t.ap()[:, 0:n], in0=ps.t.ap()[:, 0:n],
                                                        scalar1=1.0 / D, scalar2=EPS, op0=ALU.mult, op1=ALU.add),
                 r=[ps], w=[rs])
            P.op("act", lambda: nc.scalar.activation(out=rs.t.ap()[:, 0:n], in_=rs.t.ap()[:, 0:n], func=AF.Sqrt),
                 r=[rs], w=[rs])
            P.op("dve", lambda: nc.vector.reciprocal(out=rs.t.ap()[:, 0:n], in_=rs.t.ap()[:, 0:n]), r=[rs], w=[rs])
            P.op("dve", lambda: nc.vector.tensor_tensor(
                out=sq.t.ap()[:, 0:KD * n].rearrange("p (kc t) -> p kc t", kc=KD), in0=xv,
                in1=rs.t.ap()[:, 0:n].unsqueeze(1).to_broadcast([128, KD, n]), op=ALU.mult),
                r=[xt, rs], w=[sq])
            hb = self.abuf[self.ai % 2]
            self.ai += 1
            for kc in range(KD):
                P.op("act", lambda: nc.scalar.activation(
                    out=hb.t.ap()[:, kc * n:(kc + 1) * n], in_=sq.t.ap()[:, kc * n:(kc + 1) * n], func=AF.Identity,
                    bias=self.mod(kshift, kc, j), scale=A.t.ap()[:, kc * 2 + j:kc * 2 + j + 1]),
                    r=[sq, self.modT, A], w=[hb])
            P.dma(dst.t.ap().rearrange("(kc p) t -> p kc t", p=128)[:, :, o:o + n],
                  hb.t.ap()[:, 0:KD * n].rearrange("p (kc t) -> p kc t", kc=KD), r=[hb], w=[dst])

    def p1(self, l):
        P, nc, cfg = self.P, self.nc, self.cfg
        wd = Buf(self.w_in.t.ap()[l], "w_in_l")
        wd.t = self.w_in.t
        def evac(c, tile, ps):
            o, n, j = tile
            st = self.stage()
            P.op("act", lambda: nc.scalar.copy(out=st.t.ap()[:, 0:n], in_=ps.t.ap()[:, 0:n]), r=[ps], w=[st])
            P.dma(self.plv(c * 128, (c + 1) * 128)[:, o:o + n], st.t.ap()[:, 0:n], r=[st], w=[self.plA, self.plG], q="act")
        wl = Buf(self.w_in.t.ap()[l], "w_in_layer")
        self.gemm(self.hT, KD, _LayerView(self.w_in, l), NCH_IN, cfg.tiles, evac)


class _LayerView:
    def __init__(self, buf, l):
        self.buf = buf
        self.l = l
        self.w, self.r = None, {}
        self.nt = True

    @property
    def t(self):
        return _TV(self.buf.t.ap()[self.l])


class _TV:
    def __init__(self, ap):
        self._ap = ap

    def ap(self):
        return self._ap


class _V:
    def __init__(self, ap):
        self._ap = ap

    def ap(self):
        return self._ap


def carve_buf(parent, e0, e1, dt=None, name=""):
    ap = parent.t.ap()[:, e0:e1]
    if dt is not None:
        ap = ap.bitcast(dt)
    return Buf(_V(ap), name)


def _attn_setup(self):
    T = self.cfg.T
    a = {}
    a["kT"] = carve_buf(self.wbuf[0], 0, T, None, "kT")
    a["vtok"] = carve_buf(self.wbuf[0], 8192, 8192 + T, None, "vtok")
    a["qT"] = [carve_buf(self.wbuf[1], i * 8192, i * 8192 + T, None, "qT%d" % i) for i in range(2)]
    a["PT"] = [carve_buf(self.abuf[0], i * 512, (i + 1) * 512, None, "PT%d" % i) for i in range(6)]
    a["yo"] = [carve_buf(self.abuf[0], 4096 + i * 512, 4096 + (i + 1) * 512, None, "yo%d" % i) for i in range(2)]
    f = lambda i, nm: carve_buf(self.big[0], i * 512, (i + 1) * 512, None, nm)
    a["raw"] = [f(0, "raw0"), f(1, "raw1")]
    a["sq"] = f(2, "sq")
    a["rs"] = f(3, "rs")
    a["xn"] = f(4, "xn")
    a["t1"] = f(5, "t1")
    a["t2"] = f(6, "t2")
    a["cos"] = [f(7, "cos0"), f(8, "cos1")]
    a["sin"] = [f(9, "sin0"), f(10, "sin1")]
    a["rz"] = f(11, "rz")
    return a


def attention(self, l):
    P, nc, cfg = self.P, self.nc, self.cfg
    T, CTX = cfg.T, cfg.CTX
    P.barrier()
    a = _attn_setup(self)
    ident = self.consts.t.ap()[:, 0:128]
    Rm = self.consts.t.ap()[:, 128:256]
    qk = self.stage()
    P.dma(qk.t.ap()[:, 0:2], self.qkn.t.ap()[l, :, :], r=[self.qkn], w=[qk])
    ri = [0]

    def prep(chunk_row, gcol, dst):
        for (o, n, j) in cfg.tiles:
            raw = a["raw"][ri[0] % 2]
            ri[0] += 1
            P.dma(raw.t.ap()[:, 0:n], self.plv(chunk_row * 128, (chunk_row + 1) * 128)[:, o:o + n], r=[self.plA, self.plG], w=[raw])
            P.op("act", lambda: nc.scalar.activation(out=a["sq"].t.ap()[:, 0:n], in_=raw.t.ap()[:, 0:n], func=AF.Square),
                 r=[raw], w=[a["sq"]])
            ps = self.psum[self.pi % 4]
            self.pi += 1
            P.op("pe", lambda: nc.tensor.matmul(out=ps.t.ap()[:, 0:n], lhsT=self.ones_f.t.ap(), rhs=a["sq"].t.ap()[:, 0:n],
                                                 start=True, stop=True), r=[self.ones_f, a["sq"]], w=[ps])
            rs = a["rs"]
            P.op("dve", lambda: nc.vector.tensor_scalar(out=rs.t.ap()[:, 0:n], in0=ps.t.ap()[:, 0:n], scalar1=1.0 / 128,
                                                        scalar2=EPS, op0=ALU.mult, op1=ALU.add), r=[ps], w=[rs])
            P.op("act", lambda: nc.scalar.activation(out=rs.t.ap()[:, 0:n], in_=rs.t.ap()[:, 0:n], func=AF.Sqrt), r=[rs], w=[rs])
            P.op("dve", lambda: nc.vector.reciprocal(out=rs.t.ap()[:, 0:n], in_=rs.t.ap()[:, 0:n]), r=[rs], w=[rs])
            if j == 1:
                P.op("dve", lambda: nc.vector.scalar_tensor_tensor(
                    out=dst.t.ap()[:, o:o + n], in0=raw.t.ap()[:, 0:n], scalar=qk.t.ap()[:, gcol:gcol + 1],
                    in1=rs.t.ap()[:, 0:n], op0=ALU.mult, op1=ALU.mult), r=[raw, qk, rs], w=[dst])
                continue
            xn = a["xn"]
            P.op("dve", lambda: nc.vector.scalar_tensor_tensor(
                out=xn.t.ap()[:, 0:n], in0=raw.t.ap()[:, 0:n], scalar=qk.t.ap()[:, gcol:gcol + 1],
                in1=rs.t.ap()[:, 0:n], op0=ALU.mult, op1=ALU.mult), r=[raw, qk, rs], w=[xn])
            ps2 = self.psum[self.pi % 4]
            self.pi += 1
            P.op("pe", lambda: nc.tensor.matmul(out=ps2.t.ap()[:, 0:n], lhsT=Rm, rhs=xn.t.ap()[:, 0:n], start=True, stop=True),
                 r=[self.consts, xn], w=[ps2])
            cs, sn = a["cos"][ri[0] % 2], a["sin"][ri[0] % 2]
            P.dma(cs.t.ap()[:, 0:n], self.rope.t.ap()[0, :, o - CTX:o - CTX + n], r=[self.rope], w=[cs])
            P.dma(sn.t.ap()[:, 0:n], self.rope.t.ap()[1, :, o - CTX:o - CTX + n], r=[self.rope], w=[sn])
            P.op("dve", lambda: nc.vector.tensor_tensor(out=a["t1"].t.ap()[:, 0:n], in0=xn.t.ap()[:, 0:n], in1=cs.t.ap()[:, 0:n],
                                                        op=ALU.mult), r=[xn, cs], w=[a["t1"]])
            P.op("dve", lambda: nc.vector.tensor_tensor(out=a["t2"].t.ap()[:, 0:n], in0=ps2.t.ap()[:, 0:n], in1=sn.t.ap()[:, 0:n],
                                                        op=ALU.mult), r=[ps2, sn], w=[a["t2"]])
            P.op("dve", lambda: nc.vector.tensor_tensor(out=dst.t.ap()[:, o:o + n], in0=a["t1"].t.ap()[:, 0:n],
                                                        in1=a["t2"].t.ap()[:, 0:n], op=ALU.add), r=[a["t1"], a["t2"]], w=[dst])

    scale = 128 ** -0.5
    for kv in range(2):
        prep(CH_K + kv, 1, a["kT"])
        for (o, n, j) in cfg.tiles:
            raw = a["raw"][ri[0] % 2]
            ri[0] += 1
            P.dma(raw.t.ap()[:, 0:n], self.plv((CH_V + kv) * 128, (CH_V + kv + 1) * 128)[:, o:o + n], r=[self.plA, self.plG], w=[raw])
            for s in range(n // 128):
                ps = self.psum[self.pi % 4]
                self.pi += 1
                P.op("pe", lambda: nc.tensor.transpose(out=ps.t.ap()[:, 0:128], in_=raw.t.ap()[:, s * 128:(s + 1) * 128], identity=ident),
                     r=[raw, self.consts], w=[ps])
                P.op("act", lambda: nc.scalar.copy(out=a["vtok"].t.ap()[:, o + s * 128:o + (s + 1) * 128], in_=ps.t.ap()[:, 0:128]),
                     r=[ps], w=[a["vtok"]])
        for hh in range(4):
            h = kv * 4 + hh
            qT = a["qT"][h % 2]
            prep(CH_Q + h, 0, qT)
            for (o, n, j) in cfg.tiles:
                kchunks = list(range(0, CTX // 128)) if j == 1 else list(range(0, T // 128))
                psO, psZ = self.psum[6], self.psum[7]
                nk = len(kchunks)

                def s_mm(ix):
                    kc = kchunks[ix]
                    psS = self.psum[4 + (ix % 2)]
                    P.op("pe", lambda: nc.tensor.matmul(out=psS.t.ap()[:, 0:n], lhsT=a["kT"].t.ap()[:, kc * 128:(kc + 1) * 128],
                                                         rhs=qT.t.ap()[:, o:o + n], start=True, stop=True),
                         r=[a["kT"], qT], w=[psS])
                s_mm(0)
                for ix in range(nk):
                    if ix + 1 < nk:
                        s_mm(ix + 1)
                    kc = kchunks[ix]
                    psS = self.psum[4 + (ix % 2)]
                    pt = a["PT"][ix % 6]
                    P.op("act", lambda: nc.scalar.activation(out=pt.t.ap()[:, 0:n], in_=psS.t.ap()[:, 0:n], func=AF.Exp, scale=scale),
                         r=[psS], w=[pt])
                    last = (ix == nk - 1)
                    P.op("pe", lambda: nc.tensor.matmul(out=psO.t.ap()[:, 0:n], lhsT=a["vtok"].t.ap()[:, kc * 128:(kc + 1) * 128],
                                                         rhs=pt.t.ap()[:, 0:n], start=(ix == 0), stop=last),
                         r=[a["vtok"], pt], w=[psO], inc=last)
                    P.op("pe", lambda: nc.tensor.matmul(out=psZ.t.ap()[:, 0:n], lhsT=self.ones_b.t.ap(),
                                                         rhs=pt.t.ap()[:, 0:n], start=(ix == 0), stop=last),
                         r=[self.ones_b, pt], w=[psZ], inc=last)
                rz = a["rz"]
                P.op("dve", lambda: nc.vector.reciprocal(out=rz.t.ap()[:, 0:n], in_=psZ.t.ap()[:, 0:n]), r=[psZ], w=[rz])
                yo = a["yo"][self.ai % 2]
                self.ai += 1
                P.op("dve", lambda: nc.vector.tensor_tensor(out=yo.t.ap()[:, 0:n], in0=psO.t.ap()[:, 0:n], in1=rz.t.ap()[:, 0:n],
                                                            op=ALU.mult), r=[psO, rz], w=[yo])
                P.dma(self.yT.t.ap()[h * 128:(h + 1) * 128, o:o + n], yo.t.ap()[:, 0:n], r=[yo], w=[self.yT])
    P.barrier()


K.attention = attention


def rglru(self, l):
    P, nc, cfg = self.P, self.nc, self.cfg
    T, CTX = cfg.T, cfg.CTX
    P.barrier()
    B = [carve_buf(self.wbuf[0], 0, 2 * T, F32, "rB0"), carve_buf(self.wbuf[1], 0, 2 * T, F32, "rB1"),
         carve_buf(self.abuf[0], 0, 2 * T, F32, "rB2"), carve_buf(self.abuf[1], 0, 2 * T, F32, "rB3"),
         carve_buf(self.big[0], 0, T, None, "rB4"), carve_buf(self.big[1], 0, T, None, "rB5")]
    yb = carve_buf(self.wbuf[0], 2 * T, 3 * T, None, "ryb")
    wm = carve_buf(self.wbuf[1], 2 * T, 2 * T + 1024, F32, "rwm")
    vec = carve_buf(self.abuf[0], 2 * T, 2 * T + 64, F32, "rvec")
    segs = [(0, CTX), (CTX, T)]
    for c in range(8):
        rx, rg, xc, Br, Bi, hs = B
        P.dma(rx.t.ap(), self.plv((CH_RX + c) * 128, (CH_RX + c + 1) * 128)[:, :], r=[self.plA, self.plG], w=[rx])
        P.dma(rg.t.ap(), self.plv((CH_RG + c) * 128, (CH_RG + c + 1) * 128)[:, :], r=[self.plA, self.plG], w=[rg])
        P.dma(vec.t.ap()[:, 0:11], self.rnn_vec.t.ap()[l, :, c, :], r=[self.rnn_vec], w=[vec])
        P.dma(wm.t.ap(), self.rnn_w.t.ap()[l, :, c, :], r=[self.rnn_w], w=[wm])
        V = lambda i: vec.t.ap()[:, i:i + 1]
        for d in range(2):
            P.op("act", lambda: nc.scalar.activation(out=V(15 + d), in_=V(5 + d), func=AF.Exp, scale=-1.0), r=[vec], w=[vec])
            P.op("act", lambda: nc.scalar.activation(out=V(15 + d), in_=V(15 + d), func=AF.Ln, bias=1.0, scale=1.0), r=[vec], w=[vec])
            P.op("dve", lambda: nc.vector.tensor_scalar(out=V(11 + d), in0=V(15 + d), scalar1=-8.0, scalar2=None, op0=ALU.mult), r=[vec], w=[vec])
            P.op("dve", lambda: nc.vector.tensor_scalar(out=V(13 + d), in0=V(15 + d), scalar1=-16.0, scalar2=None, op0=ALU.mult), r=[vec], w=[vec])
        for (s0, s1) in segs:
            X = lambda a0, a1: rx.t.ap()[:, a0:a1]
            Y = lambda a0, a1: xc.t.ap()[:, a0:a1]
            P.op("act", lambda: nc.scalar.activation(out=Y(s0, s1), in_=X(s0, s1), func=AF.Identity, bias=V(4), scale=V(1)),
                 r=[rx, vec], w=[xc])
            P.op("dve", lambda: nc.vector.scalar_tensor_tensor(out=Y(s0 + 1, s1), in0=X(s0, s1 - 1), scalar=V(0), in1=Y(s0 + 1, s1),
                                                               op0=ALU.mult, op1=ALU.add), r=[rx, vec, xc], w=[xc])
            P.op("dve", lambda: nc.vector.scalar_tensor_tensor(out=Y(s0, s1 - 1), in0=X(s0 + 1, s1), scalar=V(2), in1=Y(s0, s1 - 1),
                                                               op0=ALU.mult, op1=ALU.add), r=[rx, vec, xc], w=[xc])
            P.op("dve", lambda: nc.vector.scalar_tensor_tensor(out=Y(s0, s1 - 2), in0=X(s0 + 2, s1), scalar=V(3), in1=Y(s0, s1 - 2),
                                                               op0=ALU.mult, op1=ALU.add), r=[rx, vec, xc], w=[xc])
        for d in range(2):
            for (o, n, j) in cfg.tiles:
                for gi, (dstb, bcol) in enumerate(((Br, 7 + d), (Bi, 9 + d))):
                    ps = self.psum[self.pi % 4]
                    self.pi += 1
                    wsl = wm.t.ap()[:, (gi * 2 + d) * 128:(gi * 2 + d + 1) * 128]
                    P.op("pe", lambda: nc.tensor.matmul(out=ps.t.ap()[:, 0:n], lhsT=wsl, rhs=xc.t.ap()[:, o:o + n], start=True, stop=True),
                         r=[wm, xc], w=[ps])
                    P.op("act", lambda: nc.scalar.activation(out=dstb.t.ap()[:, o:o + n], in_=ps.t.ap()[:, 0:n], func=AF.Sigmoid,
                                                             bias=V(bcol), scale=1.0), r=[ps, vec], w=[dstb])
            tmp = rx
            P.op("act", lambda: nc.scalar.activation(out=tmp.t.ap(), in_=Br.t.ap(), func=AF.Exp, scale=V(13 + d)), r=[Br, vec], w=[tmp])
            P.op("act", lambda: nc.scalar.activation(out=tmp.t.ap(), in_=tmp.t.ap(), func=AF.Sqrt, bias=1.0, scale=-1.0), r=[tmp], w=[tmp])
            P.op("act", lambda: nc.scalar.activation(out=Br.t.ap(), in_=Br.t.ap(), func=AF.Exp, scale=V(11 + d)), r=[Br, vec], w=[Br])
            P.op("dve", lambda: nc.vector.tensor_tensor(out=Bi.t.ap(), in0=Bi.t.ap(), in1=tmp.t.ap(), op=ALU.mult), r=[Bi, tmp], w=[Bi])
            P.op("dve", lambda: nc.vector.tensor_tensor(out=Bi.t.ap(), in0=Bi.t.ap(), in1=xc.t.ap(), op=ALU.mult), r=[Bi, xc], w=[Bi])
            hd = hs if d == 0 else tmp
            if d == 0:
                P.op("dve", lambda: nc.vector.tensor_tensor_scan(out=hd.t.ap()[:, 0:CTX], data0=Br.t.ap()[:, 0:CTX], data1=Bi.t.ap()[:, 0:CTX],
                                                                 initial=0.0, op0=ALU.mult, op1=ALU.add), r=[Br, Bi], w=[hd])
                P.op("dve", lambda: nc.vector.tensor_tensor_scan(out=hd.t.ap()[:, CTX:T], data0=Br.t.ap()[:, CTX:T], data1=Bi.t.ap()[:, CTX:T],
                                                                 initial=hd.t.ap()[:, CTX - 1:CTX], op0=ALU.mult, op1=ALU.add), r=[Br, Bi, hd], w=[hd])
            else:
                rev = lambda b_, a0, a1: (b_.t.ap()[:, a1 - 1::-1] if a0 == 0 else b_.t.ap()[:, a1 - 1:a0 - 1:-1])
                P.op("dve", lambda: nc.vector.tensor_tensor_scan(out=rev(hd, 0, CTX), data0=rev(Br, 0, CTX), data1=rev(Bi, 0, CTX),
                                                                 initial=0.0, op0=ALU.mult, op1=ALU.add), r=[Br, Bi], w=[hd])
                P.op("dve", lambda: nc.vector.tensor_tensor_scan(out=rev(hd, CTX, T), data0=rev(Br, CTX, T), data1=rev(Bi, CTX, T),
                                                                 initial=hd.t.ap()[:, 0:1], op0=ALU.mult, op1=ALU.add), r=[Br, Bi, hd], w=[hd])
                P.op("dve", lambda: nc.vector.tensor_tensor(out=hs.t.ap(), in0=hs.t.ap(), in1=hd.t.ap(), op=ALU.add), r=[hs, hd], w=[hs])
        g1 = Br
        P.op("act", lambda: nc.scalar.activation(out=g1.t.ap(), in_=rg.t.ap(), func=AF.Square), r=[rg], w=[g1])
        P.op("dve", lambda: nc.vector.tensor_scalar(out=g1.t.ap(), in0=g1.t.ap(), scalar1=0.044715, scalar2=1.0, op0=ALU.mult, op1=ALU.add),
             r=[g1], w=[g1])
        P.op("dve", lambda: nc.vector.tensor_tensor(out=g1.t.ap(), in0=g1.t.ap(), in1=rg.t.ap(), op=ALU.mult), r=[g1, rg], w=[g1])
        P.op("act", lambda: nc.scalar.activation(out=g1.t.ap(), in_=g1.t.ap(), func=AF.Sigmoid, scale=2.0 * 0.7978845608028654), r=[g1], w=[g1])
        P.op("dve", lambda: nc.vector.tensor_tensor(out=g1.t.ap(), in0=g1.t.ap(), in1=rg.t.ap(), op=ALU.mult), r=[g1, rg], w=[g1])
        P.op("dve", lambda: nc.vector.tensor_tensor(out=yb.t.ap(), in0=g1.t.ap(), in1=hs.t.ap(), op=ALU.mult), r=[g1, hs], w=[yb])
        P.dma(self.yT.t.ap()[1024 + c * 128:1024 + (c + 1) * 128, :], yb.t.ap(), r=[yb], w=[self.yT])
    P.barrier()


K.rglru = rglru


def setup_mix(self):
    P, cfg = self.P, self.cfg
    L = cfg.DEPTH
    self.yT = P.dram("yT", [3 * BW, cfg.T], BF16)
    self.yT.nt = True
    self.consts_in = self.inp("consts", [128, 256])
    self.consts = P.sb("consts_sb", [128, 256], F32)
    P.dma(self.consts.t.ap(), self.consts_in.t.ap(), r=[self.consts_in], w=[self.consts])
    self.qkn = self.inp("qkn", [L, 128, 2])
    self.rope = self.inp("rope", [2, 128, cfg.SEQ])
    self.rnn_vec = self.inp("rnn_vec", [L, 128, 8, 11])
    self.rnn_w = self.inp("rnn_w", [L, 128, 8, 512])


K.setup_mix = setup_mix


def setup_ssd(self):
    P, cfg = self.P, self.cfg
    L = cfg.DEPTH
    self.gT = P.dram("gT", [BW, cfg.T], F32)
    self.gT.nt = True
    self.ssd_c_in = self.inp("ssd_consts", [128, 512])
    self.ssd_c = P.sb("ssd_c_sb", [128, 512], F32)
    P.dma(self.ssd_c.t.ap(), self.ssd_c_in.t.ap(), r=[self.ssd_c_in], w=[self.ssd_c])
    self.ssd_hvec = self.inp("ssd_hvec", [L, 16, 64, 8])
    self.ssd_gvec = self.inp("ssd_gvec", [L, 2, 128, 10])
    self.ssd_dvec = self.inp("ssd_dvec", [L, 32, 2])


K.setup_ssd = setup_ssd


def ssd(self, l):
    P, nc, cfg = self.P, self.nc, self.cfg
    T, CTX = cfg.T, cfg.CTX
    NC_, NCc = T // 128, CTX // 128
    P.barrier()
    ident = self.consts.t.ap()[:, 0:128]
    TRI = [self.ssd_c.t.ap()[:, 0:128], self.ssd_c.t.ap()[:, 128:256]]
    MNEG = [self.ssd_c.t.ap()[:, 256:384], self.ssd_c.t.ap()[:, 384:512]]
    segs = [(0, CTX), (CTX, T)]
    W = NC_ * 32
    f0 = lambda i, nm: carve_buf(self.big[0], i * W, (i + 1) * W, None, nm)
    dt_tok, a_tok, acum, atotB, Wt, EA = [f0(i, "s%d" % i) for i in range(6)]
    sm0 = 6 * W
    sm = lambda off, n, nm, dt=None: carve_buf(self.big[0], sm0 + off, sm0 + off + n, dt, nm)
    a_bc, Ex, LT = sm(0, 128, "a_bc"), sm(128, 128, "Ex"), sm(256, 128, "LT")
    Mb = sm(384, 64, "Mb", BF16)
    Csf = sm(448, 128, "Csf")
    Csb = sm(576, 64, "Csb", BF16)
    xw = sm(640, 32, "xw", BF16)
    S = sm(672, 64, "S")
    STb = sm(736, 32, "STb", BF16)
    dvec = sm(768, 4, "dvec")
    hvec = sm(772, 8, "hvec")
    gvec = sm(780, 10, "gvecs")
    dr = carve_buf(self.wbuf[0], 0, 2 * T, F32, "dr")
    d2 = carve_buf(self.wbuf[1], 0, 2 * T, F32, "d2")
    d3 = carve_buf(self.abuf[0], 0, 2 * T, F32, "d3")
    R32 = lambda b_: b_.t.ap()[0:32, :]
    P.dma(R32(dr), self.plv(CH_DT * 128, CH_DT * 128 + 32)[:, :], r=[self.plA, self.plG], w=[dr])
    P.dma(dvec.t.ap()[0:32, 0:2], self.ssd_dvec.t.ap()[l, :, :], r=[self.ssd_dvec], w=[dvec])
    DV = lambda i: dvec.t.ap()[0:32, i:i + 1]
    P.op("act", lambda: nc.scalar.activation(out=R32(dr), in_=R32(dr), func=AF.Identity, bias=DV(0), scale=1.0), r=[dr, dvec], w=[dr])
    P.op("act", lambda: nc.scalar.activation(out=R32(d2), in_=R32(dr), func=AF.Abs), r=[dr], w=[d2])
    P.op("act", lambda: nc.scalar.activation(out=R32(d2), in_=R32(d2), func=AF.Exp, scale=-1.0), r=[d2], w=[d2])
    P.op("act", lambda: nc.scalar.activation(out=R32(d2), in_=R32(d2), func=AF.Ln, bias=1.0, scale=1.0), r=[d2], w=[d2])
    P.op("dve", lambda: nc.vector.scalar_tensor_tensor(out=R32(dr), in0=R32(dr), scalar=0.0, in1=R32(d2), op0=ALU.max, op1=ALU.add),
         r=[dr, d2], w=[dr])
    P.op("act", lambda: nc.scalar.activation(out=DV(2), in_=DV(1), func=AF.Exp), r=[dvec], w=[dvec])
    P.op("dve", lambda: nc.vector.tensor_scalar(out=DV(3), in0=DV(2), scalar1=-1.0, scalar2=None, op0=ALU.mult), r=[dvec], w=[dvec])
    P.op("dve", lambda: nc.vector.tensor_scalar(out=R32(d3), in0=R32(dr), scalar1=DV(3), scalar2=None, op0=ALU.mult), r=[dr, dvec], w=[d3])
    for k in range(NC_):
        for (src, dst) in ((dr, dt_tok), (d3, a_tok)):
            ps = self.psum[self.pi % 4]
            self.pi += 1
            P.op("pe", lambda: nc.tensor.transpose(out=ps.t.ap()[:, 0:32], in_=src.t.ap()[0:32, k * 128:(k + 1) * 128], identity=ident[0:32, 0:32]),
                 r=[src, self.consts], w=[ps])
            P.op("act", lambda: nc.scalar.copy(out=dst.t.ap()[:, k * 32:(k + 1) * 32], in_=ps.t.ap()[:, 0:32]), r=[ps], w=[dst])
    for k in range(NC_):
        ps = self.psum[self.pi % 4]
        self.pi += 1
        for d in range(2):
            P.op("pe", lambda: nc.tensor.matmul(out=ps.t.ap()[:, d * 16:(d + 1) * 16], lhsT=TRI[d], rhs=a_tok.t.ap()[:, k * 32 + d * 16:k * 32 + (d + 1) * 16],
                                                 start=True, stop=True), r=[self.ssd_c, a_tok], w=[ps], inc=(d == 1))
        P.op("dve", lambda: nc.vector.tensor_copy(out=acum.t.ap()[:, k * 32:(k + 1) * 32], in_=ps.t.ap()[:, 0:32]), r=[ps], w=[acum])
        ps2 = self.psum[self.pi % 4]
        self.pi += 1
        P.op("pe", lambda: nc.tensor.matmul(out=ps2.t.ap()[:, 0:32], lhsT=self.ones_f.t.ap(), rhs=a_tok.t.ap()[:, k * 32:(k + 1) * 32],
                                             start=True, stop=True), r=[self.ones_f, a_tok], w=[ps2])
        P.op("act", lambda: nc.scalar.copy(out=atotB.t.ap()[:, k * 32:(k + 1) * 32], in_=ps2.t.ap()[:, 0:32]), r=[ps2], w=[atotB])
    P.op("dve", lambda: nc.vector.tensor_tensor(out=Wt.t.ap(), in0=atotB.t.ap(), in1=acum.t.ap(), op=ALU.subtract), r=[atotB, acum], w=[Wt])
    P.op("act", lambda: nc.scalar.activation(out=Wt.t.ap(), in_=Wt.t.ap(), func=AF.Exp), r=[Wt], w=[Wt])
    P.op("dve", lambda: nc.vector.tensor_tensor(out=Wt.t.ap(), in0=Wt.t.ap(), in1=dt_tok.t.ap(), op=ALU.mult), r=[Wt, dt_tok], w=[Wt])
    P.op("act", lambda: nc.scalar.activation(out=EA.t.ap(), in_=atotB.t.ap(), func=AF.Exp), r=[atotB], w=[EA])
    P.barrier()

    def conv_silu(src, dst, vec_ap, np_, tmp):
        Vv = lambda i: vec_ap[0:np_, i:i + 1]
        for (s0, s1) in segs:
            X = lambda a0, a1: src.t.ap()[0:np_, a0:a1]
            Y = lambda a0, a1: tmp.t.ap()[0:np_, a0:a1]
            P.op("act", lambda: nc.scalar.activation(out=Y(s0, s1), in_=X(s0, s1), func=AF.Identity, bias=Vv(4), scale=Vv(1)), r=[src], w=[tmp])
            P.op("dve", lambda: nc.vector.scalar_tensor_tensor(out=Y(s0 + 1, s1), in0=X(s0, s1 - 1), scalar=Vv(0), in1=Y(s0 + 1, s1), op0=ALU.mult, op1=ALU.add), r=[src, tmp], w=[tmp])
            P.op("dve", lambda: nc.vector.scalar_tensor_tensor(out=Y(s0, s1 - 1), in0=X(s0 + 1, s1), scalar=Vv(2), in1=Y(s0, s1 - 1), op0=ALU.mult, op1=ALU.add), r=[src, tmp], w=[tmp])
            P.op("dve", lambda: nc.vector.scalar_tensor_tensor(out=Y(s0, s1 - 2), in0=X(s0 + 2, s1), scalar=Vv(3), in1=Y(s0, s1 - 2), op0=ALU.mult, op1=ALU.add), r=[src, tmp], w=[tmp])
        P.op("act", lambda: nc.scalar.activation(out=dst.t.ap()[0:np_, :], in_=tmp.t.ap()[0:np_, :], func=AF.Silu), r=[tmp], w=[dst])

    for g in range(2):
        Bf = carve_buf(self.wbuf[0], 0, T, None, "Bf")
        Cf = carve_buf(self.wbuf[0], T, 2 * T, None, "Cf")
        Btok = carve_buf(self.wbuf[0], 2 * T, 3 * T, None, "Btok")
        CBT = carve_buf(self.wbuf[1], 0, 2 * T, F32, "CBT")
        xtok = carve_buf(self.wbuf[1], 2 * T, 2 * T + T // 2, None, "xtok")
        raw = carve_buf(self.abuf[0], 0, 2 * T, F32, "sraw")
        tsets = []
        tb = 2 * T
        for si_ in range(4):
            def cv_(n_f32, dt_, nm):
                nonlocal tb
                b_ = carve_buf(self.abuf[0], tb, tb + 2 * n_f32, dt_, "%s_%d" % (nm, si_))
                tb += 2 * n_f32
                return b_
            tsets.append((cv_(128, F32, "a_bc"), cv_(128, F32, "Ex"), cv_(128, F32, "LT"), cv_(64, None, "Mb"),
                          cv_(128, F32, "Csf"), cv_(64, None, "Csb"), cv_(32, None, "xw")))
        Sd, STd = [], []
        for d_ in range(2):
            Sd.append(carve_buf(self.abuf[0], tb, tb + 128, F32, "S%d" % d_)); tb += 128
            STd.append(carve_buf(self.abuf[0], tb, tb + 64, None, "STb%d" % d_)); tb += 64
        assert tb <= ACAP, tb
        cv = carve_buf(self.abuf[1], 0, 2 * T, F32, "scv")
        zt = carve_buf(self.big[1], 0, T, None, "zt")
        cvt = carve_buf(self.big[1], T, T + 2048, None, "cvt")
        P.dma(gvec.t.ap(), self.ssd_gvec.t.ap()[l, g, :, :], r=[self.ssd_gvec], w=[gvec])
        for bi, (chrow, dstb) in enumerate(((CH_SB + g, Bf), (CH_SC + g, Cf))):
            P.dma(raw.t.ap(), self.plv(chrow * 128, (chrow + 1) * 128)[:, :], r=[self.plA, self.plG], w=[raw])
            conv_silu(raw, zt, gvec.t.ap()[:, bi * 5:(bi + 1) * 5], 128, cv)
            P.op("dve", lambda: nc.vector.tensor_copy(out=dstb.t.ap(), in_=zt.t.ap()), r=[zt], w=[dstb])
            if bi == 0:
                for k in range(NC_):
                    ps = self.psum[self.pi % 4]
                    self.pi += 1
                    P.op("pe", lambda: nc.tensor.transpose(out=ps.t.ap()[:, 0:128], in_=zt.t.ap()[:, k * 128:(k + 1) * 128], identity=ident),
                         r=[zt, self.consts], w=[ps])
                    P.op("act", lambda: nc.scalar.copy(out=Btok.t.ap()[:, k * 128:(k + 1) * 128], in_=ps.t.ap()[:, 0:128]), r=[ps], w=[Btok])
        for k in range(NC_):
            ps = self.psum[self.pi % 4]
            self.pi += 1
            P.op("pe", lambda: nc.tensor.matmul(out=ps.t.ap()[:, 0:128], lhsT=Bf.t.ap()[:, k * 128:(k + 1) * 128], rhs=Cf.t.ap()[:, k * 128:(k + 1) * 128],
                                                 start=True, stop=True), r=[Bf, Cf], w=[ps])
            P.op("act", lambda: nc.scalar.copy(out=CBT.t.ap()[:, k * 128:(k + 1) * 128], in_=ps.t.ap()[:, 0:128]), r=[ps], w=[CBT])
        for hh in range(8):
            h = g * 8 + hh
            xs, ysum = raw, cv
            P.dma(hvec.t.ap()[0:64, :], self.ssd_hvec.t.ap()[l, h, :, :], r=[self.ssd_hvec], w=[hvec])
            HV = lambda i: hvec.t.ap()[0:64, i:i + 1]
            P.dma(zt.t.ap()[0:64, :], self.plv(CH_SX * 128 + h * 64, CH_SX * 128 + (h + 1) * 64)[:, :], r=[self.plA, self.plG], w=[zt])
            conv_silu(zt, xs, hvec.t.ap()[:, 0:5], 64, ysum)
            for k in range(NC_):
                ps = self.psum[self.pi % 4]
                self.pi += 1
                P.op("pe", lambda: nc.tensor.transpose(out=ps.t.ap()[:, 0:64], in_=xs.t.ap()[0:64, k * 128:(k + 1) * 128], identity=ident[0:64, 0:64]),
                     r=[xs, self.consts], w=[ps])
                P.op("act", lambda: nc.scalar.copy(out=xtok.t.ap()[:, k * 64:(k + 1) * 64], in_=ps.t.ap()[:, 0:64]), r=[ps], w=[xtok])
            P.op("dve", lambda: nc.vector.tensor_scalar(out=ysum.t.ap()[0:64, :], in0=xs.t.ap()[0:64, :], scalar1=HV(5), scalar2=None, op0=ALU.mult),
                 r=[xs, hvec], w=[ysum])
            orders = [list(range(NC_)), list(range(NCc - 1, -1, -1)) + list(range(NC_ - 1, NCc - 1, -1))]
            for d in range(2):
                P.op("dve", lambda: nc.vector.memset(Sd[d].t.ap(), 0.0), w=[Sd[d]])
                P.op("dve", lambda: nc.vector.memset(STd[d].t.ap(), 0.0), w=[STd[d]])
            for i in range(NC_):
                for d in range(2):
                    col = d * 16 + h
                    k = orders[d][i]
                    a_bc, Ex, LT, Mb, Csf, Csb, xw = tsets[d * 2 + (i % 2)]
                    S, STb = Sd[d], STd[d]
                    cc = k * 32 + col
                    P.op("dve", lambda: nc.vector.tensor_copy(out=a_bc.t.ap(), in_=a_tok.t.ap()[:, cc:cc + 1].to_broadcast([128, 128])), r=[a_tok], w=[a_bc])
                    psA = self.psum[4 + d]
                    P.op("pe", lambda: nc.tensor.matmul(out=psA.t.ap()[:, 0:128], lhsT=a_bc.t.ap(), rhs=TRI[d], start=True, stop=True),
                         r=[a_bc, self.ssd_c], w=[psA])
                    P.op("dve", lambda: nc.vector.scalar_tensor_tensor(out=Ex.t.ap(), in0=psA.t.ap()[:, 0:128], scalar=acum.t.ap()[:, cc:cc + 1],
                                                                       in1=MNEG[d], op0=ALU.subtract, op1=ALU.add), r=[psA, acum, self.ssd_c], w=[Ex])
                    P.op("act", lambda: nc.scalar.activation(out=LT.t.ap(), in_=Ex.t.ap(), func=AF.Exp), r=[Ex], w=[LT])
                    P.op("dve", lambda: nc.vector.scalar_tensor_tensor(out=Mb.t.ap(), in0=LT.t.ap(), scalar=dt_tok.t.ap()[:, cc:cc + 1],
                                                                       in1=CBT.t.ap()[:, k * 128:(k + 1) * 128], op0=ALU.mult, op1=ALU.mult),
                         r=[LT, dt_tok, CBT], w=[Mb])
                    P.op("act", lambda: nc.scalar.activation(out=Csf.t.ap(), in_=psA.t.ap()[:, 0:128], func=AF.Exp), r=[psA], w=[Csf])
                    P.op("dve", lambda: nc.vector.tensor_tensor(out=Csb.t.ap(), in0=Csf.t.ap(), in1=Cf.t.ap()[:, k * 128:(k + 1) * 128], op=ALU.mult),
                         r=[Csf, Cf], w=[Csb])
                    P.op("dve", lambda: nc.vector.tensor_scalar(out=xw.t.ap(), in0=xtok.t.ap()[:, k * 64:(k + 1) * 64], scalar1=Wt.t.ap()[:, cc:cc + 1],
                                                                scalar2=None, op0=ALU.mult), r=[xtok, Wt], w=[xw])
                    psS = self.psum[self.pi % 4]
                    self.pi += 1
                    P.op("pe", lambda: nc.tensor.matmul(out=psS.t.ap()[:, 0:64], lhsT=Btok.t.ap()[:, k * 128:(k + 1) * 128], rhs=xw.t.ap(),
                                                         start=True, stop=True), r=[Btok, xw], w=[psS])
                    psY = self.psum[6 + d]
                    P.op("pe", lambda: nc.tensor.matmul(out=psY.t.ap()[0:64, 0:128], lhsT=xtok.t.ap()[:, k * 64:(k + 1) * 64], rhs=Mb.t.ap(),
                                                         start=True, stop=False), r=[xtok, Mb], w=[psY], inc=False)
                    P.op("pe", lambda: nc.tensor.matmul(out=psY.t.ap()[0:64, 0:128], lhsT=STb.t.ap(), rhs=Csb.t.ap(),
                                                         start=False, stop=True), r=[STb, Csb], w=[psY])
                    P.op("dve", lambda: nc.vector.scalar_tensor_tensor(out=S.t.ap(), in0=S.t.ap(), scalar=EA.t.ap()[:, cc:cc + 1], in1=psS.t.ap()[:, 0:64],
                                                                       op0=ALU.mult, op1=ALU.add), r=[S, EA, psS], w=[S])
                    P.op("act", lambda: nc.scalar.copy(out=STb.t.ap(), in_=S.t.ap()), r=[S], w=[STb])
                    P.op("dve", lambda: nc.vector.tensor_tensor(out=ysum.t.ap()[0:64, k * 128:(k + 1) * 128], in0=ysum.t.ap()[0:64, k * 128:(k + 1) * 128],
                                                                in1=psY.t.ap()[0:64, 0:128], op=ALU.add), r=[ysum, psY], w=[ysum])
            P.dma(zt.t.ap()[0:64, :], self.plv(CH_SZ * 128 + h * 64, CH_SZ * 128 + (h + 1) * 64)[:, :], r=[self.plA, self.plG], w=[zt])
            P.op("act", lambda: nc.scalar.activation(out=zt.t.ap()[0:64, :], in_=zt.t.ap()[0:64, :], func=AF.Silu), r=[zt], w=[zt])
            P.op("dve", lambda: nc.vector.tensor_tensor(out=ysum.t.ap()[0:64, :], in0=ysum.t.ap()[0:64, :], in1=zt.t.ap()[0:64, :], op=ALU.mult),
                 r=[ysum, zt], w=[ysum])
            P.dma(self.gT.t.ap()[h * 64:(h + 1) * 64, :], ysum.t.ap()[0:64, :], r=[ysum], w=[self.gT])
    P.barrier()
    gt = [carve_buf(self.big[1], i * 1024, (i + 1) * 1024, None, "gt%d" % i) for i in range(8)]
    sq = carve_buf(self.big[0], 0, 512, None, "gsq")
    rs = carve_buf(self.big[0], 512, 1024, None, "grs")
    nv = carve_buf(self.big[0], 1024, 1024 + 128, None, "gnv")
    yb = [carve_buf(self.abuf[0], i * 512, (i + 1) * 512, None, "gyb%d" % i) for i in range(4)]
    P.dma(nv.t.ap()[0:64, :].rearrange("p (h e) -> p h e", e=8), self.ssd_hvec.t.ap()[l].rearrange("h p e -> p h e"), r=[self.ssd_hvec], w=[nv])
    yi = 0
    for g in range(2):
        for (o, n, j) in cfg.tiles:
            ps = self.psum[self.pi % 4]
            self.pi += 1
            for hh in range(8):
                h = g * 8 + hh
                P.dma(gt[hh].t.ap()[0:64, 0:n], self.gT.t.ap()[h * 64:(h + 1) * 64, o:o + n], r=[self.gT], w=[gt[hh]])
                P.op("act", lambda: nc.scalar.activation(out=sq.t.ap()[0:64, 0:n], in_=gt[hh].t.ap()[0:64, 0:n], func=AF.Square), r=[gt[hh]], w=[sq])
                P.op("pe", lambda: nc.tensor.matmul(out=ps.t.ap()[:, 0:n], lhsT=self.ones_f.t.ap()[0:64, :], rhs=sq.t.ap()[0:64, 0:n],
                                                     start=(hh == 0), stop=(hh == 7)), r=[self.ones_f, sq], w=[ps])
            P.op("dve", lambda: nc.vector.tensor_scalar(out=rs.t.ap()[:, 0:n], in0=ps.t.ap()[:, 0:n], scalar1=1.0 / 512, scalar2=EPS, op0=ALU.mult, op1=ALU.add),
                 r=[ps], w=[rs])
            P.op("act", lambda: nc.scalar.activation(out=rs.t.ap()[:, 0:n], in_=rs.t.ap()[:, 0:n], func=AF.Sqrt), r=[rs], w=[rs])
            P.op("dve", lambda: nc.vector.reciprocal(out=rs.t.ap()[:, 0:n], in_=rs.t.ap()[:, 0:n]), r=[rs], w=[rs])
            for hh in range(8):
                h = g * 8 + hh
                y_ = yb[yi % 4]
                yi += 1
                P.op("dve", lambda: nc.vector.scalar_tensor_tensor(out=y_.t.ap()[0:64, 0:n], in0=gt[hh].t.ap()[0:64, 0:n], scalar=nv.t.ap()[0:64, h * 8 + 6:h * 8 + 7],
                                                                   in1=rs.t.ap()[0:64, 0:n], op0=ALU.mult, op1=ALU.mult), r=[gt[hh], nv, rs], w=[y_])
                P.dma(self.yT.t.ap()[2048 + h * 64:2048 + (h + 1) * 64, o:o + n], y_.t.ap()[0:64, 0:n], r=[y_], w=[self.yT])
    P.barrier()


K.ssd = ssd


def setup_ffn(self):
    P, cfg = self.P, self.cfg
    L, T = cfg.DEPTH, cfg.T
    self.mT = P.dram("mT", [D, T], BF16)
    self.hidT = P.dram("hidT", [2 * D, T], BF16)
    self.mT.nt = True
    self.hidT.nt = True
    self.w_up = self.inp("w_up", [L, 32, 128, 3 * BW])
    self.w_o = self.inp("w_o", [L, 32, 128, D])
    self.w_gu = self.inp("w_gu", [L, 128, 128, D])
    self.w_dn = self.inp("w_dn", [L, 32, 128, 2 * D])
    self.router_w = self.inp("router_w", [128, KD * 16])
    self.router_b = self.inp("router_b", [1, 16])
    self.esel_in = self.inp("esel", [16, 16 * 128])
    self.g_final = self.inp("g_final", [128, KD])
    self.o_out = self.outp("o_out", [D, cfg.SEQ])
    self.mg = [carve_buf(self.big[0], i * 512, (i + 1) * 512, None, "mg%d" % i) for i in range(3)]
    self.mbf = [carve_buf(self.big[0], 1536 + i * 256, 1536 + (i + 1) * 256, BF16, "mbf%d" % i) for i in range(2)]
    self.mi = 0


K.setup_ffn = setup_ffn


def merge(self, l):
    P, nc, cfg = self.P, self.nc, self.cfg

    def evac(dc, tile, pss):
        o, n, j = tile
        for nb in range(3):
            gt = self.mg[nb]
            row = (CH_G + nb * 32 + dc) * 128
            P.dma(gt.t.ap()[:, 0:n], self.plv(row, row + 128)[:, o:o + n], r=[self.plA, self.plG], w=[gt])
            P.op("act", lambda: nc.scalar.activation(out=gt.t.ap()[:, 0:n], in_=gt.t.ap()[:, 0:n], func=AF.Sigmoid), r=[gt], w=[gt])
            P.op("dve", lambda: nc.vector.tensor_tensor(out=gt.t.ap()[:, 0:n], in0=gt.t.ap()[:, 0:n], in1=pss[nb].t.ap()[:, 0:n], op=ALU.mult),
                 r=[gt, pss[nb]], w=[gt])
        P.op("dve", lambda: nc.vector.tensor_tensor(out=self.mg[0].t.ap()[:, 0:n], in0=self.mg[0].t.ap()[:, 0:n], in1=self.mg[1].t.ap()[:, 0:n], op=ALU.add),
             r=[self.mg[0], self.mg[1]], w=[self.mg[0]])
        mb = self.mbf[self.mi % 2]
        self.mi += 1
        P.op("dve", lambda: nc.vector.tensor_tensor(out=mb.t.ap()[:, 0:n], in0=self.mg[0].t.ap()[:, 0:n], in1=self.mg[2].t.ap()[:, 0:n], op=ALU.add),
             r=[self.mg[0], self.mg[2]], w=[mb])
        P.dma(self.mT.t.ap()[dc * 128:(dc + 1) * 128, o:o + n], mb.t.ap()[:, 0:n], r=[mb], w=[self.mT], q="act")
    P.barrier()
    self.gemm(self.yT, 24, _LayerView(self.w_up, l), 32, cfg.tiles, evac, split=3)
    P.barrier()


K.merge = merge


def resid_gemm(self, l, act, KC, wbuf_in, kmod):
    P, nc, cfg = self.P, self.nc, self.cfg

    def evac(dc, tile, ps):
        o, n, j = tile
        xt = self.stage()
        P.dma(xt.t.ap()[:, 0:n], self.xT.t.ap()[dc * 128:(dc + 1) * 128, o:o + n], r=[self.xT], w=[xt])
        P.op("dve", lambda: nc.vector.scalar_tensor_tensor(out=xt.t.ap()[:, 0:n], in0=ps.t.ap()[:, 0:n], scalar=self.mod(kmod, dc, j),
                                                           in1=xt.t.ap()[:, 0:n], op0=ALU.mult, op1=ALU.add), r=[ps, self.modT, xt], w=[xt])
        P.dma(self.xT.t.ap()[dc * 128:(dc + 1) * 128, o:o + n], xt.t.ap()[:, 0:n], r=[xt], w=[self.xT], q="act")
    self.gemm(act, KC, _LayerView(wbuf_in, l), 32, cfg.tiles, evac)


K.resid_gemm = resid_gemm


def router(self, l):
    P, nc, cfg = self.P, self.nc, self.cfg
    T = cfg.T
    NC_ = T // 128
    P.barrier()
    W16 = NC_ * 16
    self.combT = carve_buf(self.big[1], 0, T, None, "combT")
    self.esel = carve_buf(self.big[1], T, T + 2048, None, "esel")
    self.combB = carve_buf(self.big[1], T + 2048, T + 2048 + 512, None, "combB")
    self.hsil = carve_buf(self.big[1], T + 2560, T + 2560 + 512, None, "hsil")
    self.hbf = [carve_buf(self.big[1], T + 3072 + i * 256, T + 3072 + (i + 1) * 256, BF16, "hbf%d" % i) for i in range(2)]
    P.dma(self.esel.t.ap()[0:16, :], self.esel_in.t.ap(), r=[self.esel_in], w=[self.esel])
    f = lambda i, nm: carve_buf(self.big[0], i * W16, (i + 1) * W16, None, nm)
    score, sel, msk, oh, tmp = f(0, "score"), f(1, "sel"), f(2, "msk"), f(3, "oh"), f(4, "rtmp")
    o2 = 5 * W16
    pairs = carve_buf(self.big[0], o2, o2 + NC_ * 24, None, "pairs")
    o2 += NC_ * 24
    gs = carve_buf(self.big[0], o2, o2 + NC_ * 4, None, "gs"); o2 += NC_ * 4
    ing = carve_buf(self.big[0], o2, o2 + NC_ * 4, None, "ing"); o2 += NC_ * 4
    v1 = carve_buf(self.big[0], o2, o2 + NC_, None, "v1"); o2 += NC_
    rb = carve_buf(self.big[0], o2, o2 + 16, None, "rb"); o2 += 16
    rw = carve_buf(self.big[0], o2, o2 + 256, BF16, "rw"); o2 += 256
    P.dma(rw.t.ap(), self.router_w.t.ap(), r=[self.router_w], w=[rw], q="pool")
    P.dma(rb.t.ap(), self.router_b.t.ap().partition_broadcast(128), r=[self.router_b], w=[rb])
    actv = self.hT.t.ap().rearrange("(kc p) t -> p kc t", p=128)
    for (o, n, j) in cfg.tiles:
        ab = self.abuf[self.ai % 2]
        self.ai += 1
        P.dma(ab.t.ap()[:, 0:KD * n].rearrange("p (kc t) -> p kc t", kc=KD), actv[:, :, o:o + n], r=[self.hT], w=[ab])
        for s_ in range(n // 128):
            ps = self.psum[self.pi % 4]
            self.pi += 1
            for kc in range(KD):
                P.op("pe", lambda: nc.tensor.matmul(out=ps.t.ap()[:, 0:16], lhsT=ab.t.ap()[:, kc * n + s_ * 128:kc * n + (s_ + 1) * 128],
                                                     rhs=rw.t.ap()[:, kc * 16:(kc + 1) * 16], start=(kc == 0), stop=(kc == KD - 1)),
                     r=[ab, rw], w=[ps], inc=(kc == KD - 1))
            ck = (o + s_ * 128) // 128
            P.op("act", lambda: nc.scalar.activation(out=score.t.ap()[:, ck * 16:(ck + 1) * 16], in_=ps.t.ap()[:, 0:16], func=AF.Sigmoid), r=[ps], w=[score])
    v3 = lambda b_, e: b_.t.ap().rearrange("p (c e) -> p c e", e=e)
    P.op("dve", lambda: nc.vector.tensor_tensor(out=v3(sel, 16), in0=v3(score, 16), in1=rb.t.ap().unsqueeze(1).to_broadcast([128, NC_, 16]), op=ALU.add),
         r=[score, rb], w=[sel])
    s4 = sel.t.ap().rearrange("p (c e) -> p c e", e=4)
    p6 = pairs.t.ap().rearrange("p (c e) -> p c e", e=6)
    for (po, pn, a0, b0) in ((0, 3, 0, 1), (3, 2, 0, 2), (5, 1, 0, 3)):
        P.op("dve", lambda: nc.vector.tensor_tensor(out=p6[:, :, po:po + pn], in0=s4[:, :, a0:a0 + pn], in1=s4[:, :, b0:b0 + pn], op=ALU.add),
             r=[sel], w=[pairs])
    P.op("dve", lambda: nc.vector.tensor_reduce(out=gs.t.ap(), in_=p6, axis=AX.X, op=ALU.max), r=[pairs], w=[gs])
    P.op("dve", lambda: nc.vector.tensor_reduce(out=v1.t.ap(), in_=v3(gs, 4), axis=AX.X, op=ALU.max), r=[gs], w=[v1])
    P.op("dve", lambda: nc.vector.tensor_tensor(out=v3(ing, 4), in0=v3(gs, 4), in1=v1.t.ap().unsqueeze(2).to_broadcast([128, NC_, 4]), op=ALU.is_equal),
         r=[gs, v1], w=[ing])
    P.op("dve", lambda: nc.vector.tensor_scalar(out=ing.t.ap(), in0=ing.t.ap(), scalar1=1e9, scalar2=-1e9, op0=ALU.mult, op1=ALU.add), r=[ing], w=[ing])
    P.op("dve", lambda: nc.vector.tensor_tensor(out=msk.t.ap().rearrange("p (c e) -> p c e", e=4), in0=s4,
                                                in1=ing.t.ap().unsqueeze(2).to_broadcast([128, NC_ * 4, 4]), op=ALU.add), r=[sel, ing], w=[msk])
    P.op("dve", lambda: nc.vector.tensor_reduce(out=v1.t.ap(), in_=v3(msk, 16), axis=AX.X, op=ALU.max), r=[msk], w=[v1])
    P.op("dve", lambda: nc.vector.tensor_tensor(out=v3(oh, 16), in0=v3(msk, 16), in1=v1.t.ap().unsqueeze(2).to_broadcast([128, NC_, 16]), op=ALU.is_equal),
         r=[msk, v1], w=[oh])
    P.op("dve", lambda: nc.vector.scalar_tensor_tensor(out=msk.t.ap(), in0=oh.t.ap(), scalar=-1e9, in1=msk.t.ap(), op0=ALU.mult, op1=ALU.add),
         r=[oh, msk], w=[msk])
    P.op("dve", lambda: nc.vector.tensor_reduce(out=v1.t.ap(), in_=v3(msk, 16), axis=AX.X, op=ALU.max), r=[msk], w=[v1])
    P.op("dve", lambda: nc.vector.tensor_tensor(out=v3(tmp, 16), in0=v3(msk, 16), in1=v1.t.ap().unsqueeze(2).to_broadcast([128, NC_, 16]), op=ALU.is_equal),
         r=[msk, v1], w=[tmp])
    P.op("dve", lambda: nc.vector.tensor_tensor(out=oh.t.ap(), in0=oh.t.ap(), in1=tmp.t.ap(), op=ALU.add), r=[oh, tmp], w=[oh])
    P.op("dve", lambda: nc.vector.tensor_tensor(out=oh.t.ap(), in0=oh.t.ap(), in1=score.t.ap(), op=ALU.mult), r=[oh, score], w=[oh])
    P.op("dve", lambda: nc.vector.tensor_reduce(out=v1.t.ap(), in_=v3(oh, 16), axis=AX.X, op=ALU.add), r=[oh], w=[v1])
    P.op("dve", lambda: nc.vector.reciprocal(out=v1.t.ap(), in_=v1.t.ap()), r=[v1], w=[v1])
    P.op("dve", lambda: nc.vector.tensor_tensor(out=v3(oh, 16), in0=v3(oh, 16), in1=v1.t.ap().unsqueeze(2).to_broadcast([128, NC_, 16]), op=ALU.mult),
         r=[oh, v1], w=[oh])
    ident = self.consts.t.ap()[:, 0:128]
    for ck in range(NC_):
        ps = self.psum[self.pi % 4]
        self.pi += 1
        P.op("pe", lambda: nc.tensor.transpose(out=ps.t.ap()[0:16, 0:128], in_=oh.t.ap()[:, ck * 16:(ck + 1) * 16], identity=ident),
             r=[oh, self.consts], w=[ps])
        P.op("act", lambda: nc.scalar.copy(out=self.combT.t.ap()[0:16, ck * 128:(ck + 1) * 128], in_=ps.t.ap()[0:16, 0:128]), r=[ps], w=[self.combT])
    P.barrier()


K.router = router


def moe(self, l):
    P, nc, cfg = self.P, self.nc, self.cfg
    held = [None]

    def evac(c, tile, ps):
        o, n, j = tile
        e, f, gu = c // 8, (c % 8) // 2, c % 2
        if c % 4 == 0:
            pc = self.psum[7]
            P.op("pe", lambda: nc.tensor.matmul(out=pc.t.ap()[:, 0:n], lhsT=self.esel.t.ap()[0:16, e * 128:(e + 1) * 128],
                                                 rhs=self.combT.t.ap()[0:16, o:o + n], start=True, stop=True), r=[self.esel, self.combT], w=[pc])
            P.op("act", lambda: nc.scalar.copy(out=self.combB.t.ap()[:, 0:n], in_=pc.t.ap()[:, 0:n]), r=[pc], w=[self.combB])
        if gu == 0:
            held[0] = ps
            return
        pg = held[0]
        P.op("act", lambda: nc.scalar.activation(out=self.hsil.t.ap()[:, 0:n], in_=pg.t.ap()[:, 0:n], func=AF.Silu), r=[pg], w=[self.hsil])
        P.op("dve", lambda: nc.vector.tensor_tensor(out=self.hsil.t.ap()[:, 0:n], in0=self.hsil.t.ap()[:, 0:n], in1=ps.t.ap()[:, 0:n], op=ALU.mult),
             r=[self.hsil, ps], w=[self.hsil])
        hb = self.hbf[self.mi % 2]
        self.mi += 1
        P.op("dve", lambda: nc.vector.tensor_tensor(out=hb.t.ap()[:, 0:n], in0=self.hsil.t.ap()[:, 0:n], in1=self.combB.t.ap()[:, 0:n], op=ALU.mult),
             r=[self.hsil, self.combB], w=[hb])
        row = e * 512 + f * 128
        P.dma(self.hidT.t.ap()[row:row + 128, o:o + n], hb.t.ap()[:, 0:n], r=[hb], w=[self.hidT], q="act")
    self.gemm(self.hT, KD, _LayerView(self.w_gu, l), 128, cfg.tiles, evac)
    P.barrier()
    self.resid_gemm(l, self.hidT, 64, self.w_dn, 5)


K.moe = moe


def final_norm(self):
    P, nc, cfg = self.P, self.nc, self.cfg
    P.barrier()
    gf = self.gvec
    P.dma(gf.t.ap()[:, 0:KD], self.g_final.t.ap(), r=[self.g_final], w=[gf])
    for ti, (o, n, j) in enumerate(cfg.tiles256):
        if j == 1:
            continue
        xt, sq = self.big[0], self.big[1]
        xv = xt.t.ap()[:, 0:KD * n].rearrange("p (kc t) -> p kc t", kc=KD)
        P.dma(xv, self.xT.t.ap().rearrange("(kc p) t -> p kc t", p=128)[:, :, o:o + n], r=[self.xT], w=[xt])
        P.op("act", lambda: nc.scalar.activation(out=sq.t.ap()[:, 0:KD * n], in_=xt.t.ap()[:, 0:KD * n], func=AF.Square), r=[xt], w=[sq])
        ps = self.psum[4 + (ti % 2)]
        for kc in range(KD):
            P.op("pe", lambda: nc.tensor.matmul(out=ps.t.ap()[:, 0:n], lhsT=self.ones_f.t.ap(), rhs=sq.t.ap()[:, kc * n:(kc + 1) * n],
                                                 start=(kc == 0), stop=(kc == KD - 1)), r=[self.ones_f, sq], w=[ps], inc=(kc == KD - 1))
        rs = self.stage()
        P.op("dve", lambda: nc.vector.tensor_scalar(out=rs.t.ap()[:, 0:n], in0=ps.t.ap()[:, 0:n], scalar1=1.0 / D, scalar2=EPS, op0=ALU.mult, op1=ALU.add),
             r=[ps], w=[rs])
        P.op("act", lambda: nc.scalar.activation(out=rs.t.ap()[:, 0:n], in_=rs.t.ap()[:, 0:n], func=AF.Sqrt), r=[rs], w=[rs])
        P.op("dve", lambda: nc.vector.reciprocal(out=rs.t.ap()[:, 0:n], in_=rs.t.ap()[:, 0:n]), r=[rs], w=[rs])
        P.op("dve", lambda: nc.vector.tensor_tensor(out=sq.t.ap()[:, 0:KD * n].rearrange("p (kc t) -> p kc t", kc=KD), in0=xv,
                                                    in1=rs.t.ap()[:, 0:n].unsqueeze(1).to_broadcast([128, KD, n]), op=ALU.mult), r=[xt, rs], w=[sq])
        for kc in range(KD):
            P.op("act", lambda: nc.scalar.activation(out=xt.t.ap()[:, kc * n:(kc + 1) * n], in_=sq.t.ap()[:, kc * n:(kc + 1) * n], func=AF.Identity,
                                                     scale=gf.t.ap()[:, kc:kc + 1]), r=[sq, gf], w=[xt])
        P.dma(self.o_out.t.ap().rearrange("(kc p) t -> p kc t", p=128)[:, :, o - cfg.CTX:o - cfg.CTX + n], xv, r=[xt], w=[self.o_out])


K.final_norm = final_norm


def build_all(cfg):
    k = K(cfg)
    k.setup(); k.setup_mix(); k.setup_ssd(); k.setup_ffn()
    P = k.P
    P.dma(k.xT.t.ap()[:, :], k.x_in.t.ap()[:, :], r=[k.x_in], w=[k.xT])
    P.barrier()
    for l in range(cfg.DEPTH):
        k.adaln(l)
        k.norm_mod(k.A1, 0, k.xT, k.hT)
        P.barrier()
        k.p1(l)
        k.attention(l)
        k.rglru(l)
        k.ssd(l)
        k.merge(l)
        k.resid_gemm(l, k.mT, KD, k.w_o, 2)
        P.barrier()
        k.norm_mod(k.A2, 3, k.xT, k.hT)
        k.router(l)
        k.moe(l)
        P.barrier()
    k.final_norm()
    P.finish([k.o_out])
    return k


def rope_tables(SEQ):
    GRID_W, NF = 64, 32
    t = np.arange(SEQ)
    row = (t // GRID_W).astype(np.float32); col = (t % GRID_W).astype(np.float32)
    inv = (np.float32(10000.0) ** (-np.arange(NF, dtype=np.float32) / NF)).astype(np.float32)
    ang = np.stack([row[:, None] * inv, col[:, None] * inv], 1)
    cos = np.cos(ang).astype(np.float32); sin = np.sin(ang).astype(np.float32)
    cosT = np.zeros((128, SEQ), np.float32); sinT = np.zeros((128, SEQ), np.float32)
    for d in range(128):
        ax, f = d // 64, d % 32
        cosT[d] = cos[:, ax, f]; sinT[d] = sin[:, ax, f]
    return np.stack([cosT, sinT])

def consts():
    c = np.zeros((128, 256), np.float32)
    c[:, :128] = np.eye(128, dtype=np.float32)
    R = np.zeros((128, 128), np.float32)
    for m in range(128):
        if (m % 64) < 32: R[m + 32, m] = -1.0
        else: R[m - 32, m] = 1.0
    c[:, 128:] = R
    return c

def prep(I, b, L, cfg):
    m = {}
    f32 = lambda a: np.ascontiguousarray(a, dtype=np.float32)
    m["x_in"] = f32(np.concatenate([I["ctx"][b], I["x"][b]], 0).T)
    cond = np.stack([I["c"][b], I["c_ctx"]], -1)
    m["cond"] = f32(cond.reshape(KD, 128, 2).transpose(1, 0, 2).reshape(128, KD * 2))
    m["w_mod_a"] = f32(np.stack([I["w_mod_a"][l].reshape(KD, 128, 256).transpose(1, 0, 2).reshape(128, KD * 256) for l in range(L)]))
    m["w_mod_b"] = f32(np.stack([np.stack([I["w_mod_b"][l][:, kk * D:(kk + 1) * D].reshape(2, 128, D).transpose(1, 0, 2).reshape(128, 2 * D) for kk in range(6)]) for l in range(L)]))
    m["b_mod"] = f32(np.stack([I["b_mod"][l].reshape(6, KD, 128).transpose(2, 0, 1).reshape(128, 6 * KD) for l in range(L)]))
    m["g_mix"] = f32(np.stack([vlay(I["g_mix"][l]) for l in range(L)]))
    m["g_ffn"] = f32(np.stack([vlay(I["g_ffn"][l]) for l in range(L)]))
    ws = []
    for l in range(L):
        W = I["w_in"][l]
        W2 = np.concatenate([W[:, :6144], W[:, MIXC:], W[:, 6144:MIXC], np.zeros((D, 96), np.float32)], 1)
        ws.append(wlay(W2))
    m["w_in"] = f32(np.stack(ws))
    m["consts"] = consts()
    m["qkn"] = f32(np.stack([np.stack([I["q_norm"][l], I["k_norm"][l]], -1) for l in range(L)]))
    m["rope"] = rope_tables(cfg.SEQ)
    rv = np.zeros((L, 128, 8, 11), np.float32)
    rw = np.zeros((L, 128, 8, 512), np.float32)
    for l in range(L):
        for c in range(8):
            sl = slice(c * 128, (c + 1) * 128)
            rv[l, :, c, 0:4] = I["rnn_conv_w"][l][:, sl].T
            rv[l, :, c, 4] = I["rnn_conv_b"][l][sl]
            for d in range(2):
                rv[l, :, c, 5 + d] = I["rnn_lambda"][l][d][sl]
                rv[l, :, c, 7 + d] = I["rnn_b_r"][l][d][sl]
                rv[l, :, c, 9 + d] = I["rnn_b_i"][l][d][sl]
                rw[l, :, c, (0 * 2 + d) * 128:(0 * 2 + d + 1) * 128] = I["rnn_w_r"][l][d][c]
                rw[l, :, c, (1 * 2 + d) * 128:(1 * 2 + d + 1) * 128] = I["rnn_w_i"][l][d][c]
    m["rnn_vec"] = rv; m["rnn_w"] = rw
    return m

def ssd_consts():
    c = np.zeros((128, 512), np.float32)
    j = np.arange(128)[:, None]; i = np.arange(128)[None, :]
    c[:, 0:128] = (j <= i); c[:, 128:256] = (j >= i)
    c[:, 256:384] = np.where(j <= i, 0.0, -30000.0); c[:, 384:512] = np.where(j >= i, 0.0, -30000.0)
    return c

def prep_ssd(I, L):
    m = {}
    m["ssd_consts"] = ssd_consts()
    hv = np.zeros((L, 16, 64, 8), np.float32); gv = np.zeros((L, 2, 128, 10), np.float32); dv = np.zeros((L, 32, 2), np.float32)
    for l in range(L):
        cw, cb = I["ssd_conv_w"][l], I["ssd_conv_b"][l]
        for h in range(16):
            sl = slice(h * 64, (h + 1) * 64)
            hv[l, h, :, 0:4] = cw[:, sl].T; hv[l, h, :, 4] = cb[sl]
            hv[l, h, :, 5] = I["ssd_d"][l][h]; hv[l, h, :, 6] = I["ssd_norm"][l][sl]
        for g in range(2):
            for bi in range(2):
                sl = slice(1024 + bi * 256 + g * 128, 1024 + bi * 256 + (g + 1) * 128)
                gv[l, g, :, bi * 5:bi * 5 + 4] = cw[:, sl].T; gv[l, g, :, bi * 5 + 4] = cb[sl]
        for d in range(2):
            dv[l, d * 16:(d + 1) * 16, 0] = I["ssd_dt_bias"][l][d]; dv[l, d * 16:(d + 1) * 16, 1] = I["ssd_a_log"][l][d]
    m["ssd_hvec"] = hv; m["ssd_gvec"] = gv; m["ssd_dvec"] = dv
    return m

def prep_ffn(I, L):
    m = {}
    f32 = lambda a: np.ascontiguousarray(a, dtype=np.float32)
    m["w_up"] = f32(np.stack([wlay(I["w_up"][l].reshape(3 * 1024, D)) for l in range(L)]))
    m["w_o"] = f32(np.stack([wlay(I["w_o"][l]) for l in range(L)]))
    gus = []
    for l in range(L):
        wg, wu = I["moe_w_gate"][l], I["moe_w_up"][l]
        cols = []
        for e in range(16):
            for f in range(4):
                cols.append(wg[e][:, f * 128:(f + 1) * 128]); cols.append(wu[e][:, f * 128:(f + 1) * 128])
        gus.append(wlay(np.concatenate(cols, 1)))
    m["w_gu"] = f32(np.stack(gus))
    m["w_dn"] = f32(np.stack([wlay(I["moe_w_down"][l].reshape(16 * 512, D)) for l in range(L)]))
    m["router_w"] = f32(I["router_w"].reshape(KD, 128, 16).transpose(1, 0, 2).reshape(128, KD * 16))
    m["router_b"] = f32(I["router_b"].reshape(1, 16))
    es = np.zeros((16, 16 * 128), np.float32)
    for e in range(16): es[e, e * 128:(e + 1) * 128] = 1.0
    m["esel"] = es
    m["g_final"] = f32(vlay(I["g_final"]))
    return m


PER_LAYER = ("w_mod_a", "w_mod_b", "b_mod", "g_mix", "g_ffn", "w_in", "w_up", "w_o", "q_norm", "k_norm",
             "rnn_conv_w", "rnn_conv_b", "rnn_lambda", "rnn_w_r", "rnn_b_r", "rnn_w_i", "rnn_b_i",
             "ssd_conv_w", "ssd_conv_b", "ssd_dt_bias", "ssd_a_log", "ssd_d", "ssd_norm",
             "moe_w_gate", "moe_w_up", "moe_w_down")


def build_layer(cfg):
    k = K(cfg)
    k.setup(); k.setup_mix(); k.setup_ssd(); k.setup_ffn()
    P = k.P
    P.dma(k.xT.t.ap()[:, :], k.x_in.t.ap()[:, :], r=[k.x_in], w=[k.xT])
    P.barrier()
    k.adaln(0)
    k.norm_mod(k.A1, 0, k.xT, k.hT)
    P.barrier()
    k.p1(0)
    k.attention(0)
    k.rglru(0)
    k.ssd(0)
    k.merge(0)
    k.resid_gemm(0, k.mT, KD, k.w_o, 2)
    P.barrier()
    k.norm_mod(k.A2, 3, k.xT, k.hT)
    k.router(0)
    k.moe(0)
    P.barrier()
    o_x = k.outp("o_x", [D, cfg.T])
    P.dma(o_x.t.ap()[:, :], k.xT.t.ap()[:, :], r=[k.xT], w=[o_x])
    k.final_norm()
    P.finish([k.o_out, o_x])
    return k


def kernel(**inputs):
    I = {k_: np.asarray(v) for k_, v in inputs.items()}
    B, SEQ, _ = I["x"].shape
    CTX = I["ctx"].shape[1]
    L = I["w_in"].shape[0]
    cfg = Cfg(CTX, SEQ, 1)
    k = build_layer(cfg)
    xs = [np.ascontiguousarray(np.concatenate([I["ctx"][b], I["x"][b]], 0).T, dtype=np.float32) for b in range(B)]
    conds = []
    for b in range(B):
        cond = np.stack([I["c"][b], I["c_ctx"]], -1)
        conds.append(np.ascontiguousarray(cond.reshape(KD, 128, 2).transpose(1, 0, 2).reshape(128, KD * 2), dtype=np.float32))
    res = None
    for l in range(L):
        Il = {k_: (v[l:l + 1] if k_ in PER_LAYER else v) for k_, v in I.items()}
        shared = prep(Il, 0, 1, cfg)
        shared.update(prep_ssd(Il, 1))
        shared.update(prep_ffn(Il, 1))
        in_maps = []
        for b in range(B):
            m = dict(shared)
            m["x_in"] = xs[b]
            m["cond"] = conds[b]
            in_maps.append({k_: v for k_, v in m.items() if k_ in k.inputs})
        res = run_bass_kernel_spmd(k.nc, in_maps, core_ids=list(range(B)))
        xs = [np.ascontiguousarray(res.results[b]["o_x"], dtype=np.float32) for b in range(B)]
        del shared, in_maps
    out = np.stack([np.ascontiguousarray(res.results[b]["o_out"].T) for b in range(B)]).astype(np.float32)
    return out
```

```python
import numpy as np
import concourse.bass as bass
import concourse.mybir as mybir
from concourse.bass_utils import run_bass_kernel_spmd

F32 = mybir.dt.float32
BF16 = mybir.dt.bfloat16
I32 = mybir.dt.int32
AF = mybir.ActivationFunctionType
ALU = mybir.AluOpType
AX = mybir.AxisListType


class Buf:
    __slots__ = ("t", "w", "r", "name", "nt")

    def __init__(self, t, name=""):
        self.t = t
        self.w = None
        self.r = {}
        self.name = name
        self.nt = False

    def __getitem__(self, idx):
        return self.t[idx]


class Prog:
    ENG = ("pe", "dve", "act", "pool", "sp")

    def __init__(self, nc, n_dma_sems=48):
        self.nc = nc
        self.e = {"pe": nc.tensor, "dve": nc.vector, "act": nc.scalar, "pool": nc.gpsimd, "sp": nc.sync}
        self.sem = {}
        self.cnt = {}
        for k in self.ENG:
            self.sem[k] = nc.alloc_semaphore("s_" + k)
            self.cnt[k] = 0
        self.dsem = [nc.alloc_semaphore("d%d" % i) for i in range(n_dma_sems)]
        self.dcnt = [0] * n_dma_sems
        self.dnext = 0
        for i in range(n_dma_sems):
            self.sem[("d", i)] = self.dsem[i]
        self.seen = {k: {} for k in self.ENG}
        self.pend = {k: [] for k in self.ENG}
        self.ninst = 0

    def sb(self, name, shape, dt=F32):
        return Buf(self.nc.alloc_sbuf_tensor(name, list(shape), dt), name)

    def ps(self, name, shape, dt=F32):
        return Buf(self.nc.alloc_psum_tensor(name, list(shape), dt), name)

    def dram(self, name, shape, dt=F32, kind="Internal"):
        return Buf(self.nc.dram_tensor(name, list(shape), dt, kind=kind), name)

    def _wait(self, eng, ev):
        if ev is None:
            return
        key, val = ev
        if self.seen[eng].get(key, 0) >= val:
            return
        if key == eng and eng == "pe":
            return
        self.e[eng].wait_ge(self.sem[key], val)
        self.seen[eng][key] = val

    def _waitw(self, eng, w):
        if w is None:
            return
        if isinstance(w, list):
            for ev in w:
                self._wait(eng, ev)
        else:
            self._wait(eng, w)

    def _deps(self, eng, reads, writes):
        for b in reads:
            self._waitw(eng, b.w)
        for b in writes:
            self._waitw(eng, b.w)
            for k, v in b.r.items():
                self._wait(eng, (k, v))

    def _mark(self, ev, reads, writes):
        for b in reads:
            if not b.nt:
                b.r[ev[0]] = ev[1]
        for b in writes:
            if not b.nt:
                b.w = ev
                b.r = {}

    def op(self, eng, fn, r=(), w=(), inc=True):
        self._deps(eng, r, w)
        inst = fn()
        self.ninst += 1
        if not inc:
            self.pend[eng].append((r, w))
            return None
        self.cnt[eng] += 1
        inst.then_inc(self.sem[eng], 1)
        ev = (eng, self.cnt[eng])
        for pr, pw in self.pend[eng]:
            self._mark(ev, pr, pw)
        self.pend[eng] = []
        self._mark(ev, r, w)
        return ev

    def dma(self, out, in_, r=(), w=(), q="sp", **kw):
        i = self.dnext
        self.dnext = (self.dnext + 1) % len(self.dsem)
        key = ("d", i)
        if self.dcnt[i] > 0:
            self._wait(q, (key, self.dcnt[i]))
        self._deps(q, r, w)
        inst = self.e[q].dma_start(out=out, in_=in_, **kw)
        self.dcnt[i] += 16
        inst.then_inc(self.dsem[i], 16)
        ev = (key, self.dcnt[i])
        self._mark(ev, r, w)
        self.ninst += 1
        return ev

    def dma_multi(self, pairs, r=(), w=(), q="sp", **kw):
        self._deps(q, r, w)
        evs = []
        for (out, in_) in pairs:
            i = self.dnext
            self.dnext = (self.dnext + 1) % len(self.dsem)
            key = ("d", i)
            if self.dcnt[i] > 0:
                self._wait(q, (key, self.dcnt[i]))
            inst = self.e[q].dma_start(out=out, in_=in_, **kw)
            self.dcnt[i] += 16
            inst.then_inc(self.dsem[i], 16)
            evs.append((key, self.dcnt[i]))
            self.ninst += 1
        for b in r:
            if not b.nt:
                for ev in evs:
                    b.r[ev[0]] = ev[1]
        for b in w:
            if not b.nt:
                b.w = list(evs)
                b.r = {}
        return evs

    def barrier(self):
        for eng in self.ENG:
            for k in self.ENG:
                if k != eng and self.cnt[k] > 0:
                    self._wait(eng, (k, self.cnt[k]))
            for i in range(len(self.dsem)):
                if self.dcnt[i] > 0:
                    self._wait(eng, (("d", i), self.dcnt[i]))

    def finish(self, bufs):
        for b in bufs:
            self._waitw("sp", b.w)


import math

D = 4096
KD = 32
NB = 3
BW = 1024
MIXC = 6176
NCH_IN = 145
CH_Q, CH_K, CH_V, CH_RX, CH_RG, CH_SZ, CH_SX, CH_SB, CH_SC, CH_G, CH_DT = 0, 8, 10, 12, 20, 28, 36, 44, 46, 48, 144
EPS = 1e-6
WCAP = 16384
ACAP = 16384


class Cfg:
    def __init__(self, CTX, SEQ, DEPTH):
        self.CTX, self.SEQ, self.DEPTH = CTX, SEQ, DEPTH
        self.T = CTX + SEQ
        self.tiles = []
        for o in range(0, CTX, 512):
            self.tiles.append((o, min(512, CTX - o), 1))
        for o in range(0, SEQ, 512):
            self.tiles.append((CTX + o, min(512, SEQ - o), 0))
        self.tiles256 = []
        for (o, n, j) in self.tiles:
            for oo in range(0, n, 256):
                self.tiles256.append((o + oo, min(256, n - oo), j))


def wlay(W):
    K, N = W.shape
    KC, NC = K // 128, N // 128
    return np.ascontiguousarray(W.reshape(KC, 128, NC, 128).transpose(2, 1, 0, 3).reshape(NC, 128, KC * 128))


def vlay(v):
    return np.ascontiguousarray(v.reshape(-1, 128).T)


class K:
    def __init__(self, cfg, debug=()):
        self.cfg = cfg
        self.debug = set(debug)
        nc = bass.Bass("TRN2", target_bir_lowering=False)
        self.nc = nc
        self.P = Prog(nc)
        self.inputs = {}
        self.outputs = {}

    def inp(self, name, shape, dt=F32):
        b = self.P.dram(name, shape, dt, kind="ExternalInput")
        self.inputs[name] = b
        return b

    def outp(self, name, shape, dt=F32):
        b = self.P.dram(name, shape, dt, kind="ExternalOutput")
        self.outputs[name] = b
        return b

    def gemm(self, act, KC, wd, nch, tiles, evac, c0=0, tok_cap=512, split=1):
        P, nc = self.P, self.nc
        G = max(1, min(nch, WCAP // (KC * 128)))
        ntmax = min(tok_cap, ACAP // KC)
        tl = []
        for (o, n, j) in tiles:
            for oo in range(0, n, ntmax):
                tl.append((o + oo, min(ntmax, n - oo), j))
        actv = act.t.ap().rearrange("(kc p) t -> p kc t", p=128)
        gi = 0
        for g0 in range(0, nch, G):
            gn = min(G, nch - g0)
            wb = self.wbuf[gi % 2]
            gi += 1
            P.dma_multi([(wb.t.ap()[:, c * KC * 128:(c + 1) * KC * 128], wd.t.ap()[c0 + g0 + c, :, :]) for c in range(gn)],
                        r=[wd], w=[wb], q="pool", max_dma_last_dim=8192)
            for (o, n, j) in tl:
                ab = self.abuf[self.ai % 2]
                self.ai += 1
                adst = ab.t.ap()[:, 0:KC * n].rearrange("p (kc t) -> p kc t", kc=KC)
                P.dma(adst, actv[:, :, o:o + n], r=[act], w=[ab], q="sp")
                for c in range(gn):
                    if split > 1:
                        kg = KC // split
                        pss = [self.psum[(self.pi % 2) * 3 + s_] for s_ in range(split)]
                        self.pi += 1
                        for kc in range(KC):
                            ps = pss[kc // kg]
                            P.op("pe", lambda: nc.tensor.matmul(
                                out=ps.t.ap()[:, 0:n],
                                lhsT=wb.t.ap()[:, (c * KC + kc) * 128:(c * KC + kc + 1) * 128],
                                rhs=ab.t.ap()[:, kc * n:(kc + 1) * n],
                                start=(kc % kg == 0), stop=(kc % kg == kg - 1)),
                                r=[wb, ab], w=[ps], inc=(kc % kg == kg - 1))
                        evac(g0 + c, (o, n, j), pss)
                        continue
                    ps = self.psum[self.pi % 4]
                    self.pi += 1
                    for kc in range(KC):
                        P.op("pe", lambda: nc.tensor.matmul(
                            out=ps.t.ap()[:, 0:n],
                            lhsT=wb.t.ap()[:, (c * KC + kc) * 128:(c * KC + kc + 1) * 128],
                            rhs=ab.t.ap()[:, kc * n:(kc + 1) * n],
                            start=(kc == 0), stop=(kc == KC - 1)),
                            r=[wb, ab], w=[ps], inc=(kc == KC - 1))
                    evac(g0 + c, (o, n, j), ps)

    def setup(self):
        P, nc, cfg = self.P, self.nc, self.cfg
        self.wbuf = [P.sb("wbuf%d" % i, [128, WCAP], BF16) for i in range(2)]
        self.abuf = [P.sb("abuf%d" % i, [128, ACAP], BF16) for i in range(2)]
        self.psum = [P.ps("ps%d" % i, [128, 512], F32) for i in range(8)]
        self.ai = 0
        self.pi = 0
        self.stg = [P.sb("stg%d" % i, [128, 512], F32) for i in range(4)]
        self.si = 0
        self.ones_f = P.sb("ones_f", [128, 128], F32)
        P.op("dve", lambda: nc.vector.memset(self.ones_f.t.ap(), 1.0), w=[self.ones_f])
        self.ones_b = P.sb("ones_b", [128, 128], BF16)
        P.op("dve", lambda: nc.vector.memset(self.ones_b.t.ap(), 1.0), w=[self.ones_b])
        T = cfg.T
        self.xT = P.dram("xT", [D, T], F32)
        self.hT = P.dram("hT", [D, T], BF16)
        self.plA = P.dram("plA", [49 * 128, T], F32)
        self.plG = P.dram("plG", [96 * 128, T], F32)
        for b_ in (self.xT, self.hT, self.plA, self.plG):
            b_.nt = True
        self.x_in = self.inp("x_in", [D, T])
        self.cond = self.inp("cond", [128, KD * 2])
        L = cfg.DEPTH
        self.w_mod_a = self.inp("w_mod_a", [L, 128, KD * 256])
        self.w_mod_b = self.inp("w_mod_b", [L, 6, 128, 2 * D])
        self.b_mod = self.inp("b_mod", [L, 128, 6 * KD])
        self.g_mix = self.inp("g_mix", [L, 128, KD])
        self.g_ffn = self.inp("g_ffn", [L, 128, KD])
        self.w_in = self.inp("w_in", [L, NCH_IN, 128, D])
        self.modT = P.sb("modT", [128, 6 * KD * 2], F32)
        self.A1 = P.sb("A1", [128, KD * 2], F32)
        self.A2 = P.sb("A2", [128, KD * 2], F32)
        self.sc = P.sb("sc", [128, KD * 2], F32)
        self.t1 = P.sb("t1", [128, 4], F32)
        self.gvec = P.sb("gvec", [128, 2 * KD], F32)
        self.bmod = P.sb("bmod", [128, 6 * KD], F32)
        self.big = [P.sb("big%d" % i, [128, 8192], F32) for i in range(2)]

    def plv(self, r0, r1):
        c = r0 // 128
        if c < 48:
            return self.plA.t.ap()[r0:r1, :]
        if c == 144:
            return self.plA.t.ap()[r0 - 96 * 128:r1 - 96 * 128, :]
        return self.plG.t.ap()[r0 - 48 * 128:r1 - 48 * 128, :]

    def stage(self):
        b = self.stg[self.si % 4]
        self.si += 1
        return b

    def adaln(self, l):
        P, nc = self.P, self.nc
        ct = self.stage()
        P.dma(ct.t.ap()[:, 0:KD * 2], self.cond.t.ap()[:, :], r=[self.cond], w=[ct])
        P.op("act", lambda: nc.scalar.activation(out=self.sc.t.ap(), in_=ct.t.ap()[:, 0:KD * 2], func=AF.Silu),
             r=[ct], w=[self.sc])
        wa = self.big[0]
        P.dma(wa.t.ap()[:, 0:KD * 256], self.w_mod_a.t.ap()[l, :, :], r=[self.w_mod_a], w=[wa])
        ps = self.psum[4]
        for rc in range(2):
            for kc in range(KD):
                P.op("pe", lambda: nc.tensor.matmul(
                    out=ps.t.ap()[:, rc * 2:rc * 2 + 2],
                    lhsT=wa.t.ap()[:, kc * 256 + rc * 128: kc * 256 + rc * 128 + 128],
                    rhs=self.sc.t.ap()[:, kc * 2:kc * 2 + 2],
                    start=(kc == 0), stop=(kc == KD - 1)), r=[wa, self.sc], w=[ps], inc=(kc == KD - 1 and rc == 1))
        P.op("dve", lambda: nc.vector.tensor_copy(out=self.t1.t.ap(), in_=ps.t.ap()[:, 0:4]), r=[ps], w=[self.t1])
        P.dma(self.bmod.t.ap(), self.b_mod.t.ap()[l, :, :], r=[self.b_mod], w=[self.bmod])
        P.dma(self.gvec.t.ap()[:, 0:KD], self.g_mix.t.ap()[l, :, :], r=[self.g_mix], w=[self.gvec])
        P.dma(self.gvec.t.ap()[:, KD:2 * KD], self.g_ffn.t.ap()[l, :, :], r=[self.g_ffn], w=[self.gvec])
        for k in range(6):
            wbm = self.big[k % 2]
            P.dma(wbm.t.ap(), self.w_mod_b.t.ap()[l, k, :, :], r=[self.w_mod_b], w=[wbm])
            ps = self.psum[5 + (k % 2)]
            for dc in range(KD):
                for rc in range(2):
                    P.op("pe", lambda: nc.tensor.matmul(
                        out=ps.t.ap()[:, dc * 2:dc * 2 + 2],
                        lhsT=wbm.t.ap()[:, rc * D + dc * 128: rc * D + dc * 128 + 128],
                        rhs=self.t1.t.ap()[:, rc * 2:rc * 2 + 2],
                        start=(rc == 0), stop=(rc == 1)), r=[wbm, self.t1], w=[ps], inc=(rc == 1 and dc == KD - 1))
            P.op("dve", lambda: nc.vector.tensor_tensor(
                out=self.modT.t.ap()[:, k * 64:(k + 1) * 64].rearrange("p (d j) -> p d j", j=2),
                in0=ps.t.ap()[:, 0:64].rearrange("p (d j) -> p d j", j=2),
                in1=self.bmod.t.ap()[:, k * KD:(k + 1) * KD].unsqueeze(2).to_broadcast([128, KD, 2]),
                op=ALU.add), r=[ps, self.bmod], w=[self.modT])
        for (A, kk, go) in ((self.A1, 1, 0), (self.A2, 4, KD)):
            P.op("dve", lambda: nc.vector.scalar_tensor_tensor(
                out=A.t.ap().rearrange("p (d j) -> p d j", j=2),
                in0=self.modT.t.ap()[:, kk * 64:(kk + 1) * 64].rearrange("p (d j) -> p d j", j=2),
                scalar=1.0,
                in1=self.gvec.t.ap()[:, go:go + KD].unsqueeze(2).to_broadcast([128, KD, 2]),
                op0=ALU.add, op1=ALU.mult), r=[self.modT, self.gvec], w=[A])

    def mod(self, k, dc, j):
        return self.modT.t.ap()[:, k * 64 + dc * 2 + j: k * 64 + dc * 2 + j + 1]

    def norm_mod(self, A, kshift, src, dst):
        P, nc, cfg = self.P, self.nc, self.cfg
        for ti, (o, n, j) in enumerate(cfg.tiles256):
            xt = self.big[0]
            xv = xt.t.ap()[:, 0:KD * n].rearrange("p (kc t) -> p kc t", kc=KD)
            P.dma(xv, src.t.ap().rearrange("(kc p) t -> p kc t", p=128)[:, :, o:o + n], r=[src], w=[xt])
            sq = self.big[1]
            sqb = sq.t.ap().bitcast(BF16)
            P.op("act", lambda: nc.scalar.activation(out=sqb[:, 0:KD * n], in_=xt.t.ap()[:, 0:KD * n], func=AF.Square),
                 r=[xt], w=[sq])
            ps = self.psum[4 + (ti % 2)]
            for kc in range(KD):
                P.op("pe", lambda: nc.tensor.matmul(out=ps.t.ap()[:, 0:n], lhsT=self.ones_b.t.ap(),
                                                     rhs=sqb[:, kc * n:(kc + 1) * n],
                                                     start=(kc == 0), stop=(kc == KD - 1)),
                     r=[self.ones_b, sq], w=[ps], inc=(kc == KD - 1))
            rs = self.stage()
            P.op("dve", lambda: nc.vector.tensor_scalar(out=rs.t.ap()[:, 0:n], in0=ps.t.ap()[:, 0:n],
                                                        scalar1=1.0 / D, scalar2=EPS, op0=ALU.mult, op1=ALU.add),
                 r=[ps], w=[rs])
            P.op("act", lambda: nc.scalar.activation(out=rs.t.ap()[:, 0:n], in_=rs.t.ap()[:, 0:n], func=AF.Sqrt),
                 r=[rs], w=[rs])
            P.op("dve", lambda: nc.vector.reciprocal(out=rs.t.ap()[:, 0:n], in_=rs.t.ap()[:, 0:n]), r=[rs], w=[rs])
            P.op("dve", lambda: nc.vector.tensor_tensor(
                out=xv, in0=xv,
                in1=rs.t.ap()[:, 0:n].unsqueeze(1).to_broadcast([128, KD, n]), op=ALU.mult),
                r=[xt, rs], w=[xt])
            hb = self.abuf[self.ai % 2]
            self.ai += 1
            for kc in range(KD):
                P.op("act", lambda: nc.scalar.activation(
                    out=hb.t.ap()[:, kc * n:(kc + 1) * n], in_=xt.t.ap()[:, kc * n:(kc + 1) * n], func=AF.Identity,
                    bias=self.mod(kshift, kc, j), scale=A.t.ap()[:, kc * 2 + j:kc * 2 + j + 1]),
                    r=[xt, self.modT, A], w=[hb])
            P.dma(dst.t.ap().rearrange("(kc p) t -> p kc t", p=128)[:, :, o:o + n],
                  hb.t.ap()[:, 0:KD * n].rearrange("p (kc t) -> p kc t", kc=KD), r=[hb], w=[dst])

    def p1(self, l):
        P, nc, cfg = self.P, self.nc, self.cfg
        wd = Buf(self.w_in.t.ap()[l], "w_in_l")
        wd.t = self.w_in.t
        def evac(c, tile, ps):
            o, n, j = tile
            st = self.stage()
            P.op("act", lambda: nc.scalar.copy(out=st.t.ap()[:, 0:n], in_=ps.t.ap()[:, 0:n]), r=[ps], w=[st])
            P.dma(self.plv(c * 128, (c + 1) * 128)[:, o:o + n], st.t.ap()[:, 0:n], r=[st], w=[self.plA, self.plG], q="act")
        wl = Buf(self.w_in.t.ap()[l], "w_in_layer")
        self.gemm(self.hT, KD, _LayerView(self.w_in, l), NCH_IN, cfg.tiles, evac)


class _LayerView:
    def __init__(self, buf, l):
        self.buf = buf
        self.l = l
        self.w, self.r = None, {}
        self.nt = True

    @property
    def t(self):
        return _TV(self.buf.t.ap()[self.l])


class _TV:
    def __init__(self, ap):
        self._ap = ap

    def ap(self):
        return self._ap


class _V:
    def __init__(self, ap):
        self._ap = ap

    def ap(self):
        return self._ap


def carve_buf(parent, e0, e1, dt=None, name=""):
    ap = parent.t.ap()[:, e0:e1]
    if dt is not None:
        ap = ap.bitcast(dt)
    return Buf(_V(ap), name)


def _attn_setup(self):
    T = self.cfg.T
    a = {}
    a["kT"] = carve_buf(self.wbuf[0], 0, T, None, "kT")
    a["vtok"] = carve_buf(self.wbuf[0], 8192, 8192 + T, None, "vtok")
    a["qT"] = [carve_buf(self.wbuf[1], i * 8192, i * 8192 + T, None, "qT%d" % i) for i in range(2)]
    a["PT"] = [carve_buf(self.abuf[0], i * 512, (i + 1) * 512, None, "PT%d" % i) for i in range(6)]
    a["yo"] = [carve_buf(self.abuf[0], 4096 + i * 512, 4096 + (i + 1) * 512, None, "yo%d" % i) for i in range(2)]
    f = lambda i, nm: carve_buf(self.big[0], i * 512, (i + 1) * 512, None, nm)
    a["raw"] = [f(0, "raw0"), f(1, "raw1")]
    a["sq"] = f(2, "sq")
    a["rs"] = f(3, "rs")
    a["xn"] = f(4, "xn")
    a["t1"] = f(5, "t1")
    a["t2"] = f(6, "t2")
    a["cos"] = [f(7, "cos0"), f(8, "cos1")]
    a["sin"] = [f(9, "sin0"), f(10, "sin1")]
    a["rz"] = f(11, "rz")
    return a


def attention(self, l):
    P, nc, cfg = self.P, self.nc, self.cfg
    T, CTX = cfg.T, cfg.CTX
    P.barrier()
    a = _attn_setup(self)
    ident = self.consts.t.ap()[:, 0:128]
    Rm = self.consts.t.ap()[:, 128:256]
    qk = self.stage()
    P.dma(qk.t.ap()[:, 0:2], self.qkn.t.ap()[l, :, :], r=[self.qkn], w=[qk])
    ri = [0]

    def prep(chunk_row, gcol, dst):
        for (o, n, j) in cfg.tiles:
            raw = a["raw"][ri[0] % 2]
            ri[0] += 1
            P.dma(raw.t.ap()[:, 0:n], self.plv(chunk_row * 128, (chunk_row + 1) * 128)[:, o:o + n], r=[self.plA, self.plG], w=[raw])
            P.op("act", lambda: nc.scalar.activation(out=a["sq"].t.ap()[:, 0:n], in_=raw.t.ap()[:, 0:n], func=AF.Square),
                 r=[raw], w=[a["sq"]])
            ps = self.psum[self.pi % 4]
            self.pi += 1
            P.op("pe", lambda: nc.tensor.matmul(out=ps.t.ap()[:, 0:n], lhsT=self.ones_f.t.ap(), rhs=a["sq"].t.ap()[:, 0:n],
                                                 start=True, stop=True), r=[self.ones_f, a["sq"]], w=[ps])
            rs = a["rs"]
            P.op("dve", lambda: nc.vector.tensor_scalar(out=rs.t.ap()[:, 0:n], in0=ps.t.ap()[:, 0:n], scalar1=1.0 / 128,
                                                        scalar2=EPS, op0=ALU.mult, op1=ALU.add), r=[ps], w=[rs])
            P.op("act", lambda: nc.scalar.activation(out=rs.t.ap()[:, 0:n], in_=rs.t.ap()[:, 0:n], func=AF.Sqrt), r=[rs], w=[rs])
            P.op("dve", lambda: nc.vector.reciprocal(out=rs.t.ap()[:, 0:n], in_=rs.t.ap()[:, 0:n]), r=[rs], w=[rs])
            if j == 1:
                P.op("dve", lambda: nc.vector.scalar_tensor_tensor(
                    out=dst.t.ap()[:, o:o + n], in0=raw.t.ap()[:, 0:n], scalar=qk.t.ap()[:, gcol:gcol + 1],
                    in1=rs.t.ap()[:, 0:n], op0=ALU.mult, op1=ALU.mult), r=[raw, qk, rs], w=[dst])
                continue
            xn = a["xn"]
            P.op("dve", lambda: nc.vector.scalar_tensor_tensor(
                out=xn.t.ap()[:, 0:n], in0=raw.t.ap()[:, 0:n], scalar=qk.t.ap()[:, gcol:gcol + 1],
                in1=rs.t.ap()[:, 0:n], op0=ALU.mult, op1=ALU.mult), r=[raw, qk, rs], w=[xn])
            ps2 = self.psum[self.pi % 4]
            self.pi += 1
            P.op("pe", lambda: nc.tensor.matmul(out=ps2.t.ap()[:, 0:n], lhsT=Rm, rhs=xn.t.ap()[:, 0:n], start=True, stop=True),
                 r=[self.consts, xn], w=[ps2])
            cs, sn = a["cos"][ri[0] % 2], a["sin"][ri[0] % 2]
            P.dma(cs.t.ap()[:, 0:n], self.rope.t.ap()[0, :, o - CTX:o - CTX + n], r=[self.rope], w=[cs])
            P.dma(sn.t.ap()[:, 0:n], self.rope.t.ap()[1, :, o - CTX:o - CTX + n], r=[self.rope], w=[sn])
            P.op("dve", lambda: nc.vector.tensor_tensor(out=a["t1"].t.ap()[:, 0:n], in0=xn.t.ap()[:, 0:n], in1=cs.t.ap()[:, 0:n],
                                                        op=ALU.mult), r=[xn, cs], w=[a["t1"]])
            P.op("dve", lambda: nc.vector.tensor_tensor(out=a["t2"].t.ap()[:, 0:n], in0=ps2.t.ap()[:, 0:n], in1=sn.t.ap()[:, 0:n],
                                                        op=ALU.mult), r=[ps2, sn], w=[a["t2"]])
            P.op("dve", lambda: nc.vector.tensor_tensor(out=dst.t.ap()[:, o:o + n], in0=a["t1"].t.ap()[:, 0:n],
                                                        in1=a["t2"].t.ap()[:, 0:n], op=ALU.add), r=[a["t1"], a["t2"]], w=[dst])

    scale = 128 ** -0.5
    for kv in range(2):
        prep(CH_K + kv, 1, a["kT"])
        for (o, n, j) in cfg.tiles:
            raw = a["raw"][ri[0] % 2]
            ri[0] += 1
            P.dma(raw.t.ap()[:, 0:n], self.plv((CH_V + kv) * 128, (CH_V + kv + 1) * 128)[:, o:o + n], r=[self.plA, self.plG], w=[raw])
            for s in range(n // 128):
                ps = self.psum[self.pi % 4]
                self.pi += 1
                P.op("pe", lambda: nc.tensor.transpose(out=ps.t.ap()[:, 0:128], in_=raw.t.ap()[:, s * 128:(s + 1) * 128], identity=ident),
                     r=[raw, self.consts], w=[ps])
                P.op("act", lambda: nc.scalar.copy(out=a["vtok"].t.ap()[:, o + s * 128:o + (s + 1) * 128], in_=ps.t.ap()[:, 0:128]),
                     r=[ps], w=[a["vtok"]])
        for hh in range(4):
            h = kv * 4 + hh
            qT = a["qT"][h % 2]
            prep(CH_Q + h, 0, qT)
            for (o, n, j) in cfg.tiles:
                kchunks = list(range(0, CTX // 128)) if j == 1 else list(range(0, T // 128))
                psO, psZ = self.psum[6], self.psum[7]
                nk = len(kchunks)

                def s_mm(ix):
                    kc = kchunks[ix]
                    psS = self.psum[4 + (ix % 2)]
                    P.op("pe", lambda: nc.tensor.matmul(out=psS.t.ap()[:, 0:n], lhsT=a["kT"].t.ap()[:, kc * 128:(kc + 1) * 128],
                                                         rhs=qT.t.ap()[:, o:o + n], start=True, stop=True),
                         r=[a["kT"], qT], w=[psS])
                s_mm(0)
                for ix in range(nk):
                    if ix + 1 < nk:
                        s_mm(ix + 1)
                    kc = kchunks[ix]
                    psS = self.psum[4 + (ix % 2)]
                    pt = a["PT"][ix % 6]
                    P.op("act", lambda: nc.scalar.activation(out=pt.t.ap()[:, 0:n], in_=psS.t.ap()[:, 0:n], func=AF.Exp, scale=scale),
                         r=[psS], w=[pt])
                    last = (ix == nk - 1)
                    P.op("pe", lambda: nc.tensor.matmul(out=psO.t.ap()[:, 0:n], lhsT=a["vtok"].t.ap()[:, kc * 128:(kc + 1) * 128],
                                                         rhs=pt.t.ap()[:, 0:n], start=(ix == 0), stop=last),
                         r=[a["vtok"], pt], w=[psO], inc=last)
                    P.op("pe", lambda: nc.tensor.matmul(out=psZ.t.ap()[:, 0:n], lhsT=self.ones_b.t.ap(),
                                                         rhs=pt.t.ap()[:, 0:n], start=(ix == 0), stop=last),
                         r=[self.ones_b, pt], w=[psZ], inc=last)
                rz = a["rz"]
                P.op("dve", lambda: nc.vector.reciprocal(out=rz.t.ap()[:, 0:n], in_=psZ.t.ap()[:, 0:n]), r=[psZ], w=[rz])
                yo = a["yo"][self.ai % 2]
                self.ai += 1
                P.op("dve", lambda: nc.vector.tensor_tensor(out=yo.t.ap()[:, 0:n], in0=psO.t.ap()[:, 0:n], in1=rz.t.ap()[:, 0:n],
                                                            op=ALU.mult), r=[psO, rz], w=[yo])
                P.dma(self.yT.t.ap()[h * 128:(h + 1) * 128, o:o + n], yo.t.ap()[:, 0:n], r=[yo], w=[self.yT])
    P.barrier()


K.attention = attention


def rglru(self, l):
    P, nc, cfg = self.P, self.nc, self.cfg
    T, CTX = cfg.T, cfg.CTX
    P.barrier()
    B = [carve_buf(self.wbuf[0], 0, 2 * T, F32, "rB0"), carve_buf(self.wbuf[1], 0, 2 * T, F32, "rB1"),
         carve_buf(self.abuf[0], 0, 2 * T, F32, "rB2"), carve_buf(self.abuf[1], 0, 2 * T, F32, "rB3"),
         carve_buf(self.big[0], 0, T, None, "rB4"), carve_buf(self.big[1], 0, T, None, "rB5")]
    yb = carve_buf(self.wbuf[0], 2 * T, 3 * T, None, "ryb")
    wm = carve_buf(self.wbuf[1], 2 * T, 2 * T + 1024, F32, "rwm")
    vec = carve_buf(self.abuf[0], 2 * T, 2 * T + 64, F32, "rvec")
    segs = [(0, CTX), (CTX, T)]
    for c in range(8):
        rx, rg, xc, Br, Bi, hs = B
        P.dma(rx.t.ap(), self.plv((CH_RX + c) * 128, (CH_RX + c + 1) * 128)[:, :], r=[self.plA, self.plG], w=[rx])
        P.dma(rg.t.ap(), self.plv((CH_RG + c) * 128, (CH_RG + c + 1) * 128)[:, :], r=[self.plA, self.plG], w=[rg])
        P.dma(vec.t.ap()[:, 0:11], self.rnn_vec.t.ap()[l, :, c, :], r=[self.rnn_vec], w=[vec])
        P.dma(wm.t.ap(), self.rnn_w.t.ap()[l, :, c, :], r=[self.rnn_w], w=[wm])
        V = lambda i: vec.t.ap()[:, i:i + 1]
        for d in range(2):
            P.op("act", lambda: nc.scalar.activation(out=V(15 + d), in_=V(5 + d), func=AF.Exp, scale=-1.0), r=[vec], w=[vec])
            P.op("act", lambda: nc.scalar.activation(out=V(15 + d), in_=V(15 + d), func=AF.Ln, bias=1.0, scale=1.0), r=[vec], w=[vec])
            P.op("dve", lambda: nc.vector.tensor_scalar(out=V(11 + d), in0=V(15 + d), scalar1=-8.0, scalar2=None, op0=ALU.mult), r=[vec], w=[vec])
            P.op("dve", lambda: nc.vector.tensor_scalar(out=V(13 + d), in0=V(15 + d), scalar1=-16.0, scalar2=None, op0=ALU.mult), r=[vec], w=[vec])
        for (s0, s1) in segs:
            X = lambda a0, a1: rx.t.ap()[:, a0:a1]
            Y = lambda a0, a1: xc.t.ap()[:, a0:a1]
            P.op("act", lambda: nc.scalar.activation(out=Y(s0, s1), in_=X(s0, s1), func=AF.Identity, bias=V(4), scale=V(1)),
                 r=[rx, vec], w=[xc])
            P.op("dve", lambda: nc.vector.scalar_tensor_tensor(out=Y(s0 + 1, s1), in0=X(s0, s1 - 1), scalar=V(0), in1=Y(s0 + 1, s1),
                                                               op0=ALU.mult, op1=ALU.add), r=[rx, vec, xc], w=[xc])
            P.op("dve", lambda: nc.vector.scalar_tensor_tensor(out=Y(s0, s1 - 1), in0=X(s0 + 1, s1), scalar=V(2), in1=Y(s0, s1 - 1),
                                                               op0=ALU.mult, op1=ALU.add), r=[rx, vec, xc], w=[xc])
            P.op("dve", lambda: nc.vector.scalar_tensor_tensor(out=Y(s0, s1 - 2), in0=X(s0 + 2, s1), scalar=V(3), in1=Y(s0, s1 - 2),
                                                               op0=ALU.mult, op1=ALU.add), r=[rx, vec, xc], w=[xc])
        for d in range(2):
            for (o, n, j) in cfg.tiles:
                for gi, (dstb, bcol) in enumerate(((Br, 7 + d), (Bi, 9 + d))):
                    ps = self.psum[self.pi % 4]
                    self.pi += 1
                    wsl = wm.t.ap()[:, (gi * 2 + d) * 128:(gi * 2 + d + 1) * 128]
                    P.op("pe", lambda: nc.tensor.matmul(out=ps.t.ap()[:, 0:n], lhsT=wsl, rhs=xc.t.ap()[:, o:o + n], start=True, stop=True),
                         r=[wm, xc], w=[ps])
                    P.op("act", lambda: nc.scalar.activation(out=dstb.t.ap()[:, o:o + n], in_=ps.t.ap()[:, 0:n], func=AF.Sigmoid,
                                                             bias=V(bcol), scale=1.0), r=[ps, vec], w=[dstb])
            tmp = rx
            P.op("act", lambda: nc.scalar.activation(out=tmp.t.ap(), in_=Br.t.ap(), func=AF.Exp, scale=V(13 + d)), r=[Br, vec], w=[tmp])
            P.op("act", lambda: nc.scalar.activation(out=tmp.t.ap(), in_=tmp.t.ap(), func=AF.Sqrt, bias=1.0, scale=-1.0), r=[tmp], w=[tmp])
            P.op("act", lambda: nc.scalar.activation(out=Br.t.ap(), in_=Br.t.ap(), func=AF.Exp, scale=V(11 + d)), r=[Br, vec], w=[Br])
            P.op("dve", lambda: nc.vector.tensor_tensor(out=Bi.t.ap(), in0=Bi.t.ap(), in1=tmp.t.ap(), op=ALU.mult), r=[Bi, tmp], w=[Bi])
            P.op("dve", lambda: nc.vector.tensor_tensor(out=Bi.t.ap(), in0=Bi.t.ap(), in1=xc.t.ap(), op=ALU.mult), r=[Bi, xc], w=[Bi])
            hd = hs if d == 0 else tmp
            if d == 0:
                P.op("dve", lambda: nc.vector.tensor_tensor_scan(out=hd.t.ap()[:, 0:CTX], data0=Br.t.ap()[:, 0:CTX], data1=Bi.t.ap()[:, 0:CTX],
                                                                 initial=0.0, op0=ALU.mult, op1=ALU.add), r=[Br, Bi], w=[hd])
                P.op("dve", lambda: nc.vector.tensor_tensor_scan(out=hd.t.ap()[:, CTX:T], data0=Br.t.ap()[:, CTX:T], data1=Bi.t.ap()[:, CTX:T],
                                                                 initial=hd.t.ap()[:, CTX - 1:CTX], op0=ALU.mult, op1=ALU.add), r=[Br, Bi, hd], w=[hd])
            else:
                rev = lambda b_, a0, a1: (b_.t.ap()[:, a1 - 1::-1] if a0 == 0 else b_.t.ap()[:, a1 - 1:a0 - 1:-1])
                P.op("dve", lambda: nc.vector.tensor_tensor_scan(out=rev(hd, 0, CTX), data0=rev(Br, 0, CTX), data1=rev(Bi, 0, CTX),
                                                                 initial=0.0, op0=ALU.mult, op1=ALU.add), r=[Br, Bi], w=[hd])
                P.op("dve", lambda: nc.vector.tensor_tensor_scan(out=rev(hd, CTX, T), data0=rev(Br, CTX, T), data1=rev(Bi, CTX, T),
                                                                 initial=hd.t.ap()[:, 0:1], op0=ALU.mult, op1=ALU.add), r=[Br, Bi, hd], w=[hd])
                P.op("dve", lambda: nc.vector.tensor_tensor(out=hs.t.ap(), in0=hs.t.ap(), in1=hd.t.ap(), op=ALU.add), r=[hs, hd], w=[hs])
        g1 = Br
        P.op("act", lambda: nc.scalar.activation(out=g1.t.ap(), in_=rg.t.ap(), func=AF.Square), r=[rg], w=[g1])
        P.op("dve", lambda: nc.vector.tensor_scalar(out=g1.t.ap(), in0=g1.t.ap(), scalar1=0.044715, scalar2=1.0, op0=ALU.mult, op1=ALU.add),
             r=[g1], w=[g1])
        P.op("dve", lambda: nc.vector.tensor_tensor(out=g1.t.ap(), in0=g1.t.ap(), in1=rg.t.ap(), op=ALU.mult), r=[g1, rg], w=[g1])
        P.op("act", lambda: nc.scalar.activation(out=g1.t.ap(), in_=g1.t.ap(), func=AF.Sigmoid, scale=2.0 * 0.7978845608028654), r=[g1], w=[g1])
        P.op("dve", lambda: nc.vector.tensor_tensor(out=g1.t.ap(), in0=g1.t.ap(), in1=rg.t.ap(), op=ALU.mult), r=[g1, rg], w=[g1])
        P.op("dve", lambda: nc.vector.tensor_tensor(out=yb.t.ap(), in0=g1.t.ap(), in1=hs.t.ap(), op=ALU.mult), r=[g1, hs], w=[yb])
        P.dma(self.yT.t.ap()[1024 + c * 128:1024 + (c + 1) * 128, :], yb.t.ap(), r=[yb], w=[self.yT])
    P.barrier()


K.rglru = rglru


def setup_mix(self):
    P, cfg = self.P, self.cfg
    L = cfg.DEPTH
    self.yT = P.dram("yT", [3 * BW, cfg.T], BF16)
    self.yT.nt = True
    self.consts_in = self.inp("consts", [128, 256])
    self.consts = P.sb("consts_sb", [128, 256], F32)
    P.dma(self.consts.t.ap(), self.consts_in.t.ap(), r=[self.consts_in], w=[self.consts])
    self.qkn = self.inp("qkn", [L, 128, 2])
    self.rope = self.inp("rope", [2, 128, cfg.SEQ])
    self.rnn_vec = self.inp("rnn_vec", [L, 128, 8, 11])
    self.rnn_w = self.inp("rnn_w", [L, 128, 8, 512])


K.setup_mix = setup_mix


def setup_ssd(self):
    P, cfg = self.P, self.cfg
    L = cfg.DEPTH
    self.gT = P.dram("gT", [BW, cfg.T], F32)
    self.gT.nt = True
    self.ssd_c_in = self.inp("ssd_consts", [128, 512])
    self.ssd_c = P.sb("ssd_c_sb", [128, 512], F32)
    P.dma(self.ssd_c.t.ap(), self.ssd_c_in.t.ap(), r=[self.ssd_c_in], w=[self.ssd_c])
    self.ssd_hvec = self.inp("ssd_hvec", [L, 16, 64, 8])
    self.ssd_gvec = self.inp("ssd_gvec", [L, 2, 128, 10])
    self.ssd_dvec = self.inp("ssd_dvec", [L, 32, 2])


K.setup_ssd = setup_ssd


def ssd(self, l):
    P, nc, cfg = self.P, self.nc, self.cfg
    T, CTX = cfg.T, cfg.CTX
    NC_, NCc = T // 128, CTX // 128
    P.barrier()
    ident = self.consts.t.ap()[:, 0:128]
    TRI = [self.ssd_c.t.ap()[:, 0:128], self.ssd_c.t.ap()[:, 128:256]]
    MNEG = [self.ssd_c.t.ap()[:, 256:384], self.ssd_c.t.ap()[:, 384:512]]
    segs = [(0, CTX), (CTX, T)]
    W = NC_ * 32
    f0 = lambda i, nm: carve_buf(self.big[0], i * W, (i + 1) * W, None, nm)
    dt_tok, a_tok, acum, atotB, Wt, EA = [f0(i, "s%d" % i) for i in range(6)]
    sm0 = 6 * W
    sm = lambda off, n, nm, dt=None: carve_buf(self.big[0], sm0 + off, sm0 + off + n, dt, nm)
    a_bc, Ex, LT = sm(0, 128, "a_bc"), sm(128, 128, "Ex"), sm(256, 128, "LT")
    Mb = sm(384, 64, "Mb", BF16)
    Csf = sm(448, 128, "Csf")
    Csb = sm(576, 64, "Csb", BF16)
    xw = sm(640, 32, "xw", BF16)
    S = sm(672, 64, "S")
    STb = sm(736, 32, "STb", BF16)
    dvec = sm(768, 4, "dvec")
    hvec = sm(772, 8, "hvec")
    gvec = sm(780, 10, "gvecs")
    dr = carve_buf(self.wbuf[0], 0, 2 * T, F32, "dr")
    d2 = carve_buf(self.wbuf[1], 0, 2 * T, F32, "d2")
    d3 = carve_buf(self.abuf[0], 0, 2 * T, F32, "d3")
    R32 = lambda b_: b_.t.ap()[0:32, :]
    P.dma(R32(dr), self.plv(CH_DT * 128, CH_DT * 128 + 32)[:, :], r=[self.plA, self.plG], w=[dr])
    P.dma(dvec.t.ap()[0:32, 0:2], self.ssd_dvec.t.ap()[l, :, :], r=[self.ssd_dvec], w=[dvec])
    DV = lambda i: dvec.t.ap()[0:32, i:i + 1]
    P.op("act", lambda: nc.scalar.activation(out=R32(dr), in_=R32(dr), func=AF.Identity, bias=DV(0), scale=1.0), r=[dr, dvec], w=[dr])
    P.op("act", lambda: nc.scalar.activation(out=R32(d2), in_=R32(dr), func=AF.Abs), r=[dr], w=[d2])
    P.op("act", lambda: nc.scalar.activation(out=R32(d2), in_=R32(d2), func=AF.Exp, scale=-1.0), r=[d2], w=[d2])
    P.op("act", lambda: nc.scalar.activation(out=R32(d2), in_=R32(d2), func=AF.Ln, bias=1.0, scale=1.0), r=[d2], w=[d2])
    P.op("dve", lambda: nc.vector.scalar_tensor_tensor(out=R32(dr), in0=R32(dr), scalar=0.0, in1=R32(d2), op0=ALU.max, op1=ALU.add),
         r=[dr, d2], w=[dr])
    P.op("act", lambda: nc.scalar.activation(out=DV(2), in_=DV(1), func=AF.Exp), r=[dvec], w=[dvec])
    P.op("dve", lambda: nc.vector.tensor_scalar(out=DV(3), in0=DV(2), scalar1=-1.0, scalar2=None, op0=ALU.mult), r=[dvec], w=[dvec])
    P.op("dve", lambda: nc.vector.tensor_scalar(out=R32(d3), in0=R32(dr), scalar1=DV(3), scalar2=None, op0=ALU.mult), r=[dr, dvec], w=[d3])
    for k in range(NC_):
        for (src, dst) in ((dr, dt_tok), (d3, a_tok)):
            ps = self.psum[self.pi % 4]
            self.pi += 1
            P.op("pe", lambda: nc.tensor.transpose(out=ps.t.ap()[:, 0:32], in_=src.t.ap()[0:32, k * 128:(k + 1) * 128], identity=ident[0:32, 0:32]),
                 r=[src, self.consts], w=[ps])
            P.op("act", lambda: nc.scalar.copy(out=dst.t.ap()[:, k * 32:(k + 1) * 32], in_=ps.t.ap()[:, 0:32]), r=[ps], w=[dst])
    for k in range(NC_):
        ps = self.psum[self.pi % 4]
        self.pi += 1
        for d in range(2):
            P.op("pe", lambda: nc.tensor.matmul(out=ps.t.ap()[:, d * 16:(d + 1) * 16], lhsT=TRI[d], rhs=a_tok.t.ap()[:, k * 32 + d * 16:k * 32 + (d + 1) * 16],
                                                 start=True, stop=True), r=[self.ssd_c, a_tok], w=[ps], inc=(d == 1))
        P.op("dve", lambda: nc.vector.tensor_copy(out=acum.t.ap()[:, k * 32:(k + 1) * 32], in_=ps.t.ap()[:, 0:32]), r=[ps], w=[acum])
        ps2 = self.psum[self.pi % 4]
        self.pi += 1
        P.op("pe", lambda: nc.tensor.matmul(out=ps2.t.ap()[:, 0:32], lhsT=self.ones_f.t.ap(), rhs=a_tok.t.ap()[:, k * 32:(k + 1) * 32],
                                             start=True, stop=True), r=[self.ones_f, a_tok], w=[ps2])
        P.op("act", lambda: nc.scalar.copy(out=atotB.t.ap()[:, k * 32:(k + 1) * 32], in_=ps2.t.ap()[:, 0:32]), r=[ps2], w=[atotB])
    P.op("dve", lambda: nc.vector.tensor_tensor(out=Wt.t.ap(), in0=atotB.t.ap(), in1=acum.t.ap(), op=ALU.subtract), r=[atotB, acum], w=[Wt])
    P.op("act", lambda: nc.scalar.activation(out=Wt.t.ap(), in_=Wt.t.ap(), func=AF.Exp), r=[Wt], w=[Wt])
    P.op("dve", lambda: nc.vector.tensor_tensor(out=Wt.t.ap(), in0=Wt.t.ap(), in1=dt_tok.t.ap(), op=ALU.mult), r=[Wt, dt_tok], w=[Wt])
    P.op("act", lambda: nc.scalar.activation(out=EA.t.ap(), in_=atotB.t.ap(), func=AF.Exp), r=[atotB], w=[EA])
    P.barrier()

    def conv_silu(src, dst, vec_ap, np_, tmp):
        Vv = lambda i: vec_ap[0:np_, i:i + 1]
        for (s0, s1) in segs:
            X = lambda a0, a1: src.t.ap()[0:np_, a0:a1]
            Y = lambda a0, a1: tmp.t.ap()[0:np_, a0:a1]
            P.op("act", lambda: nc.scalar.activation(out=Y(s0, s1), in_=X(s0, s1), func=AF.Identity, bias=Vv(4), scale=Vv(1)), r=[src], w=[tmp])
            P.op("dve", lambda: nc.vector.scalar_tensor_tensor(out=Y(s0 + 1, s1), in0=X(s0, s1 - 1), scalar=Vv(0), in1=Y(s0 + 1, s1), op0=ALU.mult, op1=ALU.add), r=[src, tmp], w=[tmp])
            P.op("dve", lambda: nc.vector.scalar_tensor_tensor(out=Y(s0, s1 - 1), in0=X(s0 + 1, s1), scalar=Vv(2), in1=Y(s0, s1 - 1), op0=ALU.mult, op1=ALU.add), r=[src, tmp], w=[tmp])
            P.op("dve", lambda: nc.vector.scalar_tensor_tensor(out=Y(s0, s1 - 2), in0=X(s0 + 2, s1), scalar=Vv(3), in1=Y(s0, s1 - 2), op0=ALU.mult, op1=ALU.add), r=[src, tmp], w=[tmp])
        P.op("act", lambda: nc.scalar.activation(out=dst.t.ap()[0:np_, :], in_=tmp.t.ap()[0:np_, :], func=AF.Silu), r=[tmp], w=[dst])

    for g in range(2):
        Bf = carve_buf(self.wbuf[0], 0, T, None, "Bf")
        Cf = carve_buf(self.wbuf[0], T, 2 * T, None, "Cf")
        Btok = carve_buf(self.wbuf[0], 2 * T, 3 * T, None, "Btok")
        CBT = carve_buf(self.wbuf[1], 0, 2 * T, F32, "CBT")
        xtok = carve_buf(self.wbuf[1], 2 * T, 2 * T + T // 2, None, "xtok")
        raw = carve_buf(self.abuf[0], 0, 2 * T, F32, "sraw")
        tsets = []
        tb = 2 * T
        for si_ in range(4):
            def cv_(n_f32, dt_, nm):
                nonlocal tb
                b_ = carve_buf(self.abuf[0], tb, tb + 2 * n_f32, dt_, "%s_%d" % (nm, si_))
                tb += 2 * n_f32
                return b_
            tsets.append((cv_(128, F32, "a_bc"), cv_(128, F32, "Ex"), cv_(128, F32, "LT"), cv_(64, None, "Mb"),
                          cv_(128, F32, "Csf"), cv_(64, None, "Csb"), cv_(32, None, "xw")))
        Sd, STd = [], []
        for d_ in range(2):
            Sd.append(carve_buf(self.abuf[0], tb, tb + 128, F32, "S%d" % d_)); tb += 128
            STd.append(carve_buf(self.abuf[0], tb, tb + 64, None, "STb%d" % d_)); tb += 64
        assert tb <= ACAP, tb
        cv = carve_buf(self.abuf[1], 0, 2 * T, F32, "scv")
        zt = carve_buf(self.big[1], 0, T, None, "zt")
        cvt = carve_buf(self.big[1], T, T + 2048, None, "cvt")
        P.dma(gvec.t.ap(), self.ssd_gvec.t.ap()[l, g, :, :], r=[self.ssd_gvec], w=[gvec])
        for bi, (chrow, dstb) in enumerate(((CH_SB + g, Bf), (CH_SC + g, Cf))):
            P.dma(raw.t.ap(), self.plv(chrow * 128, (chrow + 1) * 128)[:, :], r=[self.plA, self.plG], w=[raw])
            conv_silu(raw, zt, gvec.t.ap()[:, bi * 5:(bi + 1) * 5], 128, cv)
            P.op("dve", lambda: nc.vector.tensor_copy(out=dstb.t.ap(), in_=zt.t.ap()), r=[zt], w=[dstb])
            if bi == 0:
                for k in range(NC_):
                    ps = self.psum[self.pi % 4]
                    self.pi += 1
                    P.op("pe", lambda: nc.tensor.transpose(out=ps.t.ap()[:, 0:128], in_=zt.t.ap()[:, k * 128:(k + 1) * 128], identity=ident),
                         r=[zt, self.consts], w=[ps])
                    P.op("act", lambda: nc.scalar.copy(out=Btok.t.ap()[:, k * 128:(k + 1) * 128], in_=ps.t.ap()[:, 0:128]), r=[ps], w=[Btok])
        for k in range(NC_):
            ps = self.psum[self.pi % 4]
            self.pi += 1
            P.op("pe", lambda: nc.tensor.matmul(out=ps.t.ap()[:, 0:128], lhsT=Bf.t.ap()[:, k * 128:(k + 1) * 128], rhs=Cf.t.ap()[:, k * 128:(k + 1) * 128],
                                                 start=True, stop=True), r=[Bf, Cf], w=[ps])
            P.op("act", lambda: nc.scalar.copy(out=CBT.t.ap()[:, k * 128:(k + 1) * 128], in_=ps.t.ap()[:, 0:128]), r=[ps], w=[CBT])
        for hh in range(8):
            h = g * 8 + hh
            xs, ysum = raw, cv
            P.dma(hvec.t.ap()[0:64, :], self.ssd_hvec.t.ap()[l, h, :, :], r=[self.ssd_hvec], w=[hvec])
            HV = lambda i: hvec.t.ap()[0:64, i:i + 1]
            P.dma(zt.t.ap()[0:64, :], self.plv(CH_SX * 128 + h * 64, CH_SX * 128 + (h + 1) * 64)[:, :], r=[self.plA, self.plG], w=[zt])
            conv_silu(zt, xs, hvec.t.ap()[:, 0:5], 64, ysum)
            for k in range(NC_):
                ps = self.psum[self.pi % 4]
                self.pi += 1
                P.op("pe", lambda: nc.tensor.transpose(out=ps.t.ap()[:, 0:64], in_=xs.t.ap()[0:64, k * 128:(k + 1) * 128], identity=ident[0:64, 0:64]),
                     r=[xs, self.consts], w=[ps])
                P.op("act", lambda: nc.scalar.copy(out=xtok.t.ap()[:, k * 64:(k + 1) * 64], in_=ps.t.ap()[:, 0:64]), r=[ps], w=[xtok])
            P.op("dve", lambda: nc.vector.tensor_scalar(out=ysum.t.ap()[0:64, :], in0=xs.t.ap()[0:64, :], scalar1=HV(5), scalar2=None, op0=ALU.mult),
                 r=[xs, hvec], w=[ysum])
            orders = [list(range(NC_)), list(range(NCc - 1, -1, -1)) + list(range(NC_ - 1, NCc - 1, -1))]
            for d in range(2):
                P.op("dve", lambda: nc.vector.memset(Sd[d].t.ap(), 0.0), w=[Sd[d]])
                P.op("dve", lambda: nc.vector.memset(STd[d].t.ap(), 0.0), w=[STd[d]])
            for i in range(NC_):
                for d in range(2):
                    col = d * 16 + h
                    k = orders[d][i]
                    a_bc, Ex, LT, Mb, Csf, Csb, xw = tsets[d * 2 + (i % 2)]
                    S, STb = Sd[d], STd[d]
                    cc = k * 32 + col
                    P.op("dve", lambda: nc.vector.tensor_copy(out=a_bc.t.ap(), in_=a_tok.t.ap()[:, cc:cc + 1].to_broadcast([128, 128])), r=[a_tok], w=[a_bc])
                    psA = self.psum[4 + d]
                    P.op("pe", lambda: nc.tensor.matmul(out=psA.t.ap()[:, 0:128], lhsT=a_bc.t.ap(), rhs=TRI[d], start=True, stop=True),
                         r=[a_bc, self.ssd_c], w=[psA])
                    P.op("dve", lambda: nc.vector.scalar_tensor_tensor(out=Ex.t.ap(), in0=psA.t.ap()[:, 0:128], scalar=acum.t.ap()[:, cc:cc + 1],
                                                                       in1=MNEG[d], op0=ALU.subtract, op1=ALU.add), r=[psA, acum, self.ssd_c], w=[Ex])
                    P.op("act", lambda: nc.scalar.activation(out=LT.t.ap(), in_=Ex.t.ap(), func=AF.Exp), r=[Ex], w=[LT])
                    P.op("dve", lambda: nc.vector.scalar_tensor_tensor(out=Mb.t.ap(), in0=LT.t.ap(), scalar=dt_tok.t.ap()[:, cc:cc + 1],
                                                                       in1=CBT.t.ap()[:, k * 128:(k + 1) * 128], op0=ALU.mult, op1=ALU.mult),
                         r=[LT, dt_tok, CBT], w=[Mb])
                    P.op("act", lambda: nc.scalar.activation(out=Csf.t.ap(), in_=psA.t.ap()[:, 0:128], func=AF.Exp), r=[psA], w=[Csf])
                    P.op("dve", lambda: nc.vector.tensor_tensor(out=Csb.t.ap(), in0=Csf.t.ap(), in1=Cf.t.ap()[:, k * 128:(k + 1) * 128], op=ALU.mult),
                         r=[Csf, Cf], w=[Csb])
                    P.op("dve", lambda: nc.vector.tensor_scalar(out=xw.t.ap(), in0=xtok.t.ap()[:, k * 64:(k + 1) * 64], scalar1=Wt.t.ap()[:, cc:cc + 1],
                                                                scalar2=None, op0=ALU.mult), r=[xtok, Wt], w=[xw])
                    psS = self.psum[self.pi % 4]
                    self.pi += 1
                    P.op("pe", lambda: nc.tensor.matmul(out=psS.t.ap()[:, 0:64], lhsT=Btok.t.ap()[:, k * 128:(k + 1) * 128], rhs=xw.t.ap(),
                                                         start=True, stop=True), r=[Btok, xw], w=[psS])
                    psY = self.psum[6 + d]
                    P.op("pe", lambda: nc.tensor.matmul(out=psY.t.ap()[0:64, 0:128], lhsT=xtok.t.ap()[:, k * 64:(k + 1) * 64], rhs=Mb.t.ap(),
                                                         start=True, stop=False), r=[xtok, Mb], w=[psY], inc=False)
                    P.op("pe", lambda: nc.tensor.matmul(out=psY.t.ap()[0:64, 0:128], lhsT=STb.t.ap(), rhs=Csb.t.ap(),
                                                         start=False, stop=True), r=[STb, Csb], w=[psY])
                    P.op("dve", lambda: nc.vector.scalar_tensor_tensor(out=S.t.ap(), in0=S.t.ap(), scalar=EA.t.ap()[:, cc:cc + 1], in1=psS.t.ap()[:, 0:64],
                                                                       op0=ALU.mult, op1=ALU.add), r=[S, EA, psS], w=[S])
                    P.op("act", lambda: nc.scalar.copy(out=STb.t.ap(), in_=S.t.ap()), r=[S], w=[STb])
                    P.op("dve", lambda: nc.vector.tensor_tensor(out=ysum.t.ap()[0:64, k * 128:(k + 1) * 128], in0=ysum.t.ap()[0:64, k * 128:(k + 1) * 128],
                                                                in1=psY.t.ap()[0:64, 0:128], op=ALU.add), r=[ysum, psY], w=[ysum])
            P.dma(zt.t.ap()[0:64, :], self.plv(CH_SZ * 128 + h * 64, CH_SZ * 128 + (h + 1) * 64)[:, :], r=[self.plA, self.plG], w=[zt])
            P.op("act", lambda: nc.scalar.activation(out=zt.t.ap()[0:64, :], in_=zt.t.ap()[0:64, :], func=AF.Silu), r=[zt], w=[zt])
            P.op("dve", lambda: nc.vector.tensor_tensor(out=ysum.t.ap()[0:64, :], in0=ysum.t.ap()[0:64, :], in1=zt.t.ap()[0:64, :], op=ALU.mult),
                 r=[ysum, zt], w=[ysum])
            P.dma(self.gT.t.ap()[h * 64:(h + 1) * 64, :], ysum.t.ap()[0:64, :], r=[ysum], w=[self.gT])
    P.barrier()
    gt = [carve_buf(self.big[1], i * 1024, (i + 1) * 1024, None, "gt%d" % i) for i in range(8)]
    sqs = [carve_buf(self.big[0], 2048 + i * 512, 2048 + (i + 1) * 512, None, "gsq%d" % i) for i in range(4)]
    rs = carve_buf(self.big[0], 512, 1024, None, "grs")
    nv = carve_buf(self.big[0], 1024, 1024 + 128, None, "gnv")
    yb = [carve_buf(self.abuf[0], i * 512, (i + 1) * 512, None, "gyb%d" % i) for i in range(4)]
    P.dma(nv.t.ap()[0:64, :].rearrange("p (h e) -> p h e", e=8), self.ssd_hvec.t.ap()[l].rearrange("h p e -> p h e"), r=[self.ssd_hvec], w=[nv])
    yi = 0
    for g in range(2):
        for (o, n, j) in cfg.tiles:
            ps = self.psum[self.pi % 4]
            self.pi += 1
            for hh in range(8):
                h = g * 8 + hh
                sq = sqs[hh % 4]
                P.dma(gt[hh].t.ap()[0:64, 0:n], self.gT.t.ap()[h * 64:(h + 1) * 64, o:o + n], r=[self.gT], w=[gt[hh]])
                P.op("act", lambda: nc.scalar.activation(out=sq.t.ap()[0:64, 0:n], in_=gt[hh].t.ap()[0:64, 0:n], func=AF.Square), r=[gt[hh]], w=[sq])
                P.op("pe", lambda: nc.tensor.matmul(out=ps.t.ap()[:, 0:n], lhsT=self.ones_f.t.ap()[0:64, :], rhs=sq.t.ap()[0:64, 0:n],
                                                     start=(hh == 0), stop=(hh == 7)), r=[self.ones_f, sq], w=[ps])
            P.op("dve", lambda: nc.vector.tensor_scalar(out=rs.t.ap()[:, 0:n], in0=ps.t.ap()[:, 0:n], scalar1=1.0 / 512, scalar2=EPS, op0=ALU.mult, op1=ALU.add),
                 r=[ps], w=[rs])
            P.op("act", lambda: nc.scalar.activation(out=rs.t.ap()[:, 0:n], in_=rs.t.ap()[:, 0:n], func=AF.Sqrt), r=[rs], w=[rs])
            P.op("dve", lambda: nc.vector.reciprocal(out=rs.t.ap()[:, 0:n], in_=rs.t.ap()[:, 0:n]), r=[rs], w=[rs])
            for hh in range(8):
                h = g * 8 + hh
                y_ = yb[yi % 4]
                yi += 1
                P.op("dve", lambda: nc.vector.scalar_tensor_tensor(out=y_.t.ap()[0:64, 0:n], in0=gt[hh].t.ap()[0:64, 0:n], scalar=nv.t.ap()[0:64, h * 8 + 6:h * 8 + 7],
                                                                   in1=rs.t.ap()[0:64, 0:n], op0=ALU.mult, op1=ALU.mult), r=[gt[hh], nv, rs], w=[y_])
                P.dma(self.yT.t.ap()[2048 + h * 64:2048 + (h + 1) * 64, o:o + n], y_.t.ap()[0:64, 0:n], r=[y_], w=[self.yT])
    P.barrier()


K.ssd = ssd


def setup_ffn(self):
    P, cfg = self.P, self.cfg
    L, T = cfg.DEPTH, cfg.T
    self.mT = P.dram("mT", [D, T], BF16)
    self.hidT = P.dram("hidT", [2 * D, T], BF16)
    self.mT.nt = True
    self.hidT.nt = True
    self.w_up = self.inp("w_up", [L, 32, 128, 3 * BW])
    self.w_o = self.inp("w_o", [L, 32, 128, D])
    self.w_gu = self.inp("w_gu", [L, 128, 128, D])
    self.w_dn = self.inp("w_dn", [L, 32, 128, 2 * D])
    self.router_w = self.inp("router_w", [128, KD * 16])
    self.router_b = self.inp("router_b", [1, 16])
    self.esel_in = self.inp("esel", [16, 16 * 128])
    self.g_final = self.inp("g_final", [128, KD])
    self.o_out = self.outp("o_out", [D, cfg.SEQ])
    self.mg = [carve_buf(self.big[0], i * 512, (i + 1) * 512, None, "mg%d" % i) for i in range(3)]
    self.mbf = [carve_buf(self.big[0], 1536 + i * 256, 1536 + (i + 1) * 256, BF16, "mbf%d" % i) for i in range(2)]
    self.mi = 0


K.setup_ffn = setup_ffn


def merge(self, l):
    P, nc, cfg = self.P, self.nc, self.cfg

    def evac(dc, tile, pss):
        o, n, j = tile
        for nb in range(3):
            gt = self.mg[nb]
            row = (CH_G + nb * 32 + dc) * 128
            P.dma(gt.t.ap()[:, 0:n], self.plv(row, row + 128)[:, o:o + n], r=[self.plA, self.plG], w=[gt])
            P.op("act", lambda: nc.scalar.activation(out=gt.t.ap()[:, 0:n], in_=gt.t.ap()[:, 0:n], func=AF.Sigmoid), r=[gt], w=[gt])
            P.op("dve", lambda: nc.vector.tensor_tensor(out=gt.t.ap()[:, 0:n], in0=gt.t.ap()[:, 0:n], in1=pss[nb].t.ap()[:, 0:n], op=ALU.mult),
                 r=[gt, pss[nb]], w=[gt])
        P.op("dve", lambda: nc.vector.tensor_tensor(out=self.mg[0].t.ap()[:, 0:n], in0=self.mg[0].t.ap()[:, 0:n], in1=self.mg[1].t.ap()[:, 0:n], op=ALU.add),
             r=[self.mg[0], self.mg[1]], w=[self.mg[0]])
        mb = self.mbf[self.mi % 2]
        self.mi += 1
        P.op("dve", lambda: nc.vector.tensor_tensor(out=mb.t.ap()[:, 0:n], in0=self.mg[0].t.ap()[:, 0:n], in1=self.mg[2].t.ap()[:, 0:n], op=ALU.add),
             r=[self.mg[0], self.mg[2]], w=[mb])
        P.dma(self.mT.t.ap()[dc * 128:(dc + 1) * 128, o:o + n], mb.t.ap()[:, 0:n], r=[mb], w=[self.mT], q="act")
    P.barrier()
    self.gemm(self.yT, 24, _LayerView(self.w_up, l), 32, cfg.tiles, evac, split=3)
    P.barrier()


K.merge = merge


def resid_gemm(self, l, act, KC, wbuf_in, kmod):
    P, nc, cfg = self.P, self.nc, self.cfg

    def evac(dc, tile, ps):
        o, n, j = tile
        xt = self.stage()
        P.dma(xt.t.ap()[:, 0:n], self.xT.t.ap()[dc * 128:(dc + 1) * 128, o:o + n], r=[self.xT], w=[xt])
        P.op("dve", lambda: nc.vector.scalar_tensor_tensor(out=xt.t.ap()[:, 0:n], in0=ps.t.ap()[:, 0:n], scalar=self.mod(kmod, dc, j),
                                                           in1=xt.t.ap()[:, 0:n], op0=ALU.mult, op1=ALU.add), r=[ps, self.modT, xt], w=[xt])
        P.dma(self.xT.t.ap()[dc * 128:(dc + 1) * 128, o:o + n], xt.t.ap()[:, 0:n], r=[xt], w=[self.xT], q="act")
    self.gemm(act, KC, _LayerView(wbuf_in, l), 32, cfg.tiles, evac)


K.resid_gemm = resid_gemm


def router(self, l):
    P, nc, cfg = self.P, self.nc, self.cfg
    T = cfg.T
    NC_ = T // 128
    P.barrier()
    W16 = NC_ * 16
    self.combT = carve_buf(self.big[1], 0, T, None, "combT")
    self.esel = carve_buf(self.big[1], T, T + 2048, None, "esel")
    self.combB = carve_buf(self.big[1], T + 2048, T + 2048 + 512, None, "combB")
    self.hsil = carve_buf(self.big[1], T + 2560, T + 2560 + 512, None, "hsil")
    self.hbf = [carve_buf(self.big[1], T + 3072 + i * 256, T + 3072 + (i + 1) * 256, BF16, "hbf%d" % i) for i in range(2)]
    P.dma(self.esel.t.ap()[0:16, :], self.esel_in.t.ap(), r=[self.esel_in], w=[self.esel])
    f = lambda i, nm: carve_buf(self.big[0], i * W16, (i + 1) * W16, None, nm)
    score, sel, msk, oh, tmp = f(0, "score"), f(1, "sel"), f(2, "msk"), f(3, "oh"), f(4, "rtmp")
    o2 = 5 * W16
    pairs = carve_buf(self.big[0], o2, o2 + NC_ * 24, None, "pairs")
    o2 += NC_ * 24
    gs = carve_buf(self.big[0], o2, o2 + NC_ * 4, None, "gs"); o2 += NC_ * 4
    ing = carve_buf(self.big[0], o2, o2 + NC_ * 4, None, "ing"); o2 += NC_ * 4
    v1 = carve_buf(self.big[0], o2, o2 + NC_, None, "v1"); o2 += NC_
    rb = carve_buf(self.big[0], o2, o2 + 16, None, "rb"); o2 += 16
    rw = carve_buf(self.big[0], o2, o2 + 256, BF16, "rw"); o2 += 256
    P.dma(rw.t.ap(), self.router_w.t.ap(), r=[self.router_w], w=[rw], q="pool")
    P.dma(rb.t.ap(), self.router_b.t.ap().partition_broadcast(128), r=[self.router_b], w=[rb])
    actv = self.hT.t.ap().rearrange("(kc p) t -> p kc t", p=128)
    for (o, n, j) in cfg.tiles:
        ab = self.abuf[self.ai % 2]
        self.ai += 1
        P.dma(ab.t.ap()[:, 0:KD * n].rearrange("p (kc t) -> p kc t", kc=KD), actv[:, :, o:o + n], r=[self.hT], w=[ab])
        for s_ in range(n // 128):
            ps = self.psum[self.pi % 4]
            self.pi += 1
            for kc in range(KD):
                P.op("pe", lambda: nc.tensor.matmul(out=ps.t.ap()[:, 0:16], lhsT=ab.t.ap()[:, kc * n + s_ * 128:kc * n + (s_ + 1) * 128],
                                                     rhs=rw.t.ap()[:, kc * 16:(kc + 1) * 16], start=(kc == 0), stop=(kc == KD - 1)),
                     r=[ab, rw], w=[ps], inc=(kc == KD - 1))
            ck = (o + s_ * 128) // 128
            P.op("act", lambda: nc.scalar.activation(out=score.t.ap()[:, ck * 16:(ck + 1) * 16], in_=ps.t.ap()[:, 0:16], func=AF.Sigmoid), r=[ps], w=[score])
    v3 = lambda b_, e: b_.t.ap().rearrange("p (c e) -> p c e", e=e)
    P.op("dve", lambda: nc.vector.tensor_tensor(out=v3(sel, 16), in0=v3(score, 16), in1=rb.t.ap().unsqueeze(1).to_broadcast([128, NC_, 16]), op=ALU.add),
         r=[score, rb], w=[sel])
    s4 = sel.t.ap().rearrange("p (c e) -> p c e", e=4)
    p6 = pairs.t.ap().rearrange("p (c e) -> p c e", e=6)
    for (po, pn, a0, b0) in ((0, 3, 0, 1), (3, 2, 0, 2), (5, 1, 0, 3)):
        P.op("dve", lambda: nc.vector.tensor_tensor(out=p6[:, :, po:po + pn], in0=s4[:, :, a0:a0 + pn], in1=s4[:, :, b0:b0 + pn], op=ALU.add),
             r=[sel], w=[pairs])
    P.op("dve", lambda: nc.vector.tensor_reduce(out=gs.t.ap(), in_=p6, axis=AX.X, op=ALU.max), r=[pairs], w=[gs])
    P.op("dve", lambda: nc.vector.tensor_reduce(out=v1.t.ap(), in_=v3(gs, 4), axis=AX.X, op=ALU.max), r=[gs], w=[v1])
    P.op("dve", lambda: nc.vector.tensor_tensor(out=v3(ing, 4), in0=v3(gs, 4), in1=v1.t.ap().unsqueeze(2).to_broadcast([128, NC_, 4]), op=ALU.is_equal),
         r=[gs, v1], w=[ing])
    P.op("dve", lambda: nc.vector.tensor_scalar(out=ing.t.ap(), in0=ing.t.ap(), scalar1=1e9, scalar2=-1e9, op0=ALU.mult, op1=ALU.add), r=[ing], w=[ing])
    P.op("dve", lambda: nc.vector.tensor_tensor(out=msk.t.ap().rearrange("p (c e) -> p c e", e=4), in0=s4,
                                                in1=ing.t.ap().unsqueeze(2).to_broadcast([128, NC_ * 4, 4]), op=ALU.add), r=[sel, ing], w=[msk])
    P.op("dve", lambda: nc.vector.tensor_reduce(out=v1.t.ap(), in_=v3(msk, 16), axis=AX.X, op=ALU.max), r=[msk], w=[v1])
    P.op("dve", lambda: nc.vector.tensor_tensor(out=v3(oh, 16), in0=v3(msk, 16), in1=v1.t.ap().unsqueeze(2).to_broadcast([128, NC_, 16]), op=ALU.is_equal),
         r=[msk, v1], w=[oh])
    P.op("dve", lambda: nc.vector.scalar_tensor_tensor(out=msk.t.ap(), in0=oh.t.ap(), scalar=-1e9, in1=msk.t.ap(), op0=ALU.mult, op1=ALU.add),
         r=[oh, msk], w=[msk])
    P.op("dve", lambda: nc.vector.tensor_reduce(out=v1.t.ap(), in_=v3(msk, 16), axis=AX.X, op=ALU.max), r=[msk], w=[v1])
    P.op("dve", lambda: nc.vector.tensor_tensor(out=v3(tmp, 16), in0=v3(msk, 16), in1=v1.t.ap().unsqueeze(2).to_broadcast([128, NC_, 16]), op=ALU.is_equal),
         r=[msk, v1], w=[tmp])
    P.op("dve", lambda: nc.vector.tensor_tensor(out=oh.t.ap(), in0=oh.t.ap(), in1=tmp.t.ap(), op=ALU.add), r=[oh, tmp], w=[oh])
    P.op("dve", lambda: nc.vector.tensor_tensor(out=oh.t.ap(), in0=oh.t.ap(), in1=score.t.ap(), op=ALU.mult), r=[oh, score], w=[oh])
    P.op("dve", lambda: nc.vector.tensor_reduce(out=v1.t.ap(), in_=v3(oh, 16), axis=AX.X, op=ALU.add), r=[oh], w=[v1])
    P.op("dve", lambda: nc.vector.reciprocal(out=v1.t.ap(), in_=v1.t.ap()), r=[v1], w=[v1])
    P.op("dve", lambda: nc.vector.tensor_tensor(out=v3(oh, 16), in0=v3(oh, 16), in1=v1.t.ap().unsqueeze(2).to_broadcast([128, NC_, 16]), op=ALU.mult),
         r=[oh, v1], w=[oh])
    ident = self.consts.t.ap()[:, 0:128]
    for ck in range(NC_):
        ps = self.psum[self.pi % 4]
        self.pi += 1
        P.op("pe", lambda: nc.tensor.transpose(out=ps.t.ap()[0:16, 0:128], in_=oh.t.ap()[:, ck * 16:(ck + 1) * 16], identity=ident),
             r=[oh, self.consts], w=[ps])
        P.op("act", lambda: nc.scalar.copy(out=self.combT.t.ap()[0:16, ck * 128:(ck + 1) * 128], in_=ps.t.ap()[0:16, 0:128]), r=[ps], w=[self.combT])
    P.barrier()


K.router = router


def moe(self, l):
    P, nc, cfg = self.P, self.nc, self.cfg
    held = [None]

    def evac(c, tile, ps):
        o, n, j = tile
        e, f, gu = c // 8, (c % 8) // 2, c % 2
        if c % 4 == 0:
            pc = self.psum[7]
            P.op("pe", lambda: nc.tensor.matmul(out=pc.t.ap()[:, 0:n], lhsT=self.esel.t.ap()[0:16, e * 128:(e + 1) * 128],
                                                 rhs=self.combT.t.ap()[0:16, o:o + n], start=True, stop=True), r=[self.esel, self.combT], w=[pc])
            P.op("act", lambda: nc.scalar.copy(out=self.combB.t.ap()[:, 0:n], in_=pc.t.ap()[:, 0:n]), r=[pc], w=[self.combB])
        if gu == 0:
            held[0] = ps
            return
        pg = held[0]
        P.op("act", lambda: nc.scalar.activation(out=self.hsil.t.ap()[:, 0:n], in_=pg.t.ap()[:, 0:n], func=AF.Silu), r=[pg], w=[self.hsil])
        P.op("dve", lambda: nc.vector.tensor_tensor(out=self.hsil.t.ap()[:, 0:n], in0=self.hsil.t.ap()[:, 0:n], in1=ps.t.ap()[:, 0:n], op=ALU.mult),
             r=[self.hsil, ps], w=[self.hsil])
        hb = self.hbf[self.mi % 2]
        self.mi += 1
        P.op("dve", lambda: nc.vector.tensor_tensor(out=hb.t.ap()[:, 0:n], in0=self.hsil.t.ap()[:, 0:n], in1=self.combB.t.ap()[:, 0:n], op=ALU.mult),
             r=[self.hsil, self.combB], w=[hb])
        row = e * 512 + f * 128
        P.dma(self.hidT.t.ap()[row:row + 128, o:o + n], hb.t.ap()[:, 0:n], r=[hb], w=[self.hidT], q="act")
    self.gemm(self.hT, KD, _LayerView(self.w_gu, l), 128, cfg.tiles, evac)
    P.barrier()
    self.resid_gemm(l, self.hidT, 64, self.w_dn, 5)


K.moe = moe


def final_norm(self):
    P, nc, cfg = self.P, self.nc, self.cfg
    P.barrier()
    gf = self.gvec
    P.dma(gf.t.ap()[:, 0:KD], self.g_final.t.ap(), r=[self.g_final], w=[gf])
    for ti, (o, n, j) in enumerate(cfg.tiles256):
        if j == 1:
            continue
        xt, sq = self.big[0], self.big[1]
        xv = xt.t.ap()[:, 0:KD * n].rearrange("p (kc t) -> p kc t", kc=KD)
        P.dma(xv, self.xT.t.ap().rearrange("(kc p) t -> p kc t", p=128)[:, :, o:o + n], r=[self.xT], w=[xt])
        P.op("act", lambda: nc.scalar.activation(out=sq.t.ap()[:, 0:KD * n], in_=xt.t.ap()[:, 0:KD * n], func=AF.Square), r=[xt], w=[sq])
        ps = self.psum[4 + (ti % 2)]
        for kc in range(KD):
            P.op("pe", lambda: nc.tensor.matmul(out=ps.t.ap()[:, 0:n], lhsT=self.ones_f.t.ap(), rhs=sq.t.ap()[:, kc * n:(kc + 1) * n],
                                                 start=(kc == 0), stop=(kc == KD - 1)), r=[self.ones_f, sq], w=[ps], inc=(kc == KD - 1))
        rs = self.stage()
        P.op("dve", lambda: nc.vector.tensor_scalar(out=rs.t.ap()[:, 0:n], in0=ps.t.ap()[:, 0:n], scalar1=1.0 / D, scalar2=EPS, op0=ALU.mult, op1=ALU.add),
             r=[ps], w=[rs])
        P.op("act", lambda: nc.scalar.activation(out=rs.t.ap()[:, 0:n], in_=rs.t.ap()[:, 0:n], func=AF.Sqrt), r=[rs], w=[rs])
        P.op("dve", lambda: nc.vector.reciprocal(out=rs.t.ap()[:, 0:n], in_=rs.t.ap()[:, 0:n]), r=[rs], w=[rs])
        P.op("dve", lambda: nc.vector.tensor_tensor(out=sq.t.ap()[:, 0:KD * n].rearrange("p (kc t) -> p kc t", kc=KD), in0=xv,
                                                    in1=rs.t.ap()[:, 0:n].unsqueeze(1).to_broadcast([128, KD, n]), op=ALU.mult), r=[xt, rs], w=[sq])
        for kc in range(KD):
            P.op("act", lambda: nc.scalar.activation(out=xt.t.ap()[:, kc * n:(kc + 1) * n], in_=sq.t.ap()[:, kc * n:(kc + 1) * n], func=AF.Identity,
                                                     scale=gf.t.ap()[:, kc:kc + 1]), r=[sq, gf], w=[xt])
        P.dma(self.o_out.t.ap().rearrange("(kc p) t -> p kc t", p=128)[:, :, o - cfg.CTX:o - cfg.CTX + n], xv, r=[xt], w=[self.o_out])


K.final_norm = final_norm


def build_all(cfg):
    k = K(cfg)
    k.setup(); k.setup_mix(); k.setup_ssd(); k.setup_ffn()
    P = k.P
    P.dma(k.xT.t.ap()[:, :], k.x_in.t.ap()[:, :], r=[k.x_in], w=[k.xT])
    P.barrier()
    for l in range(cfg.DEPTH):
        k.adaln(l)
        k.norm_mod(k.A1, 0, k.xT, k.hT)
        P.barrier()
        k.p1(l)
        k.attention(l)
        k.rglru(l)
        k.ssd(l)
        k.merge(l)
        k.resid_gemm(l, k.mT, KD, k.w_o, 2)
        P.barrier()
        k.norm_mod(k.A2, 3, k.xT, k.hT)
        k.router(l)
        k.moe(l)
        P.barrier()
    k.final_norm()
    P.finish([k.o_out])
    return k


def rope_tables(SEQ):
    GRID_W, NF = 64, 32
    t = np.arange(SEQ)
    row = (t // GRID_W).astype(np.float32); col = (t % GRID_W).astype(np.float32)
    inv = (np.float32(10000.0) ** (-np.arange(NF, dtype=np.float32) / NF)).astype(np.float32)
    ang = np.stack([row[:, None] * inv, col[:, None] * inv], 1)
    cos = np.cos(ang).astype(np.float32); sin = np.sin(ang).astype(np.float32)
    cosT = np.zeros((128, SEQ), np.float32); sinT = np.zeros((128, SEQ), np.float32)
    for d in range(128):
        ax, f = d // 64, d % 32
        cosT[d] = cos[:, ax, f]; sinT[d] = sin[:, ax, f]
    return np.stack([cosT, sinT])

def consts():
    c = np.zeros((128, 256), np.float32)
    c[:, :128] = np.eye(128, dtype=np.float32)
    R = np.zeros((128, 128), np.float32)
    for m in range(128):
        if (m % 64) < 32: R[m + 32, m] = -1.0
        else: R[m - 32, m] = 1.0
    c[:, 128:] = R
    return c

def prep(I, b, L, cfg):
    m = {}
    f32 = lambda a: np.ascontiguousarray(a, dtype=np.float32)
    m["x_in"] = f32(np.concatenate([I["ctx"][b], I["x"][b]], 0).T)
    cond = np.stack([I["c"][b], I["c_ctx"]], -1)
    m["cond"] = f32(cond.reshape(KD, 128, 2).transpose(1, 0, 2).reshape(128, KD * 2))
    m["w_mod_a"] = f32(np.stack([I["w_mod_a"][l].reshape(KD, 128, 256).transpose(1, 0, 2).reshape(128, KD * 256) for l in range(L)]))
    m["w_mod_b"] = f32(np.stack([np.stack([I["w_mod_b"][l][:, kk * D:(kk + 1) * D].reshape(2, 128, D).transpose(1, 0, 2).reshape(128, 2 * D) for kk in range(6)]) for l in range(L)]))
    m["b_mod"] = f32(np.stack([I["b_mod"][l].reshape(6, KD, 128).transpose(2, 0, 1).reshape(128, 6 * KD) for l in range(L)]))
    m["g_mix"] = f32(np.stack([vlay(I["g_mix"][l]) for l in range(L)]))
    m["g_ffn"] = f32(np.stack([vlay(I["g_ffn"][l]) for l in range(L)]))
    ws = []
    for l in range(L):
        W = I["w_in"][l]
        W2 = np.concatenate([W[:, :6144], W[:, MIXC:], W[:, 6144:MIXC], np.zeros((D, 96), np.float32)], 1)
        ws.append(wlay(W2))
    m["w_in"] = f32(np.stack(ws))
    m["consts"] = consts()
    m["qkn"] = f32(np.stack([np.stack([I["q_norm"][l], I["k_norm"][l]], -1) for l in range(L)]))
    m["rope"] = rope_tables(cfg.SEQ)
    rv = np.zeros((L, 128, 8, 11), np.float32)
    rw = np.zeros((L, 128, 8, 512), np.float32)
    for l in range(L):
        for c in range(8):
            sl = slice(c * 128, (c + 1) * 128)
            rv[l, :, c, 0:4] = I["rnn_conv_w"][l][:, sl].T
            rv[l, :, c, 4] = I["rnn_conv_b"][l][sl]
            for d in range(2):
                rv[l, :, c, 5 + d] = I["rnn_lambda"][l][d][sl]
                rv[l, :, c, 7 + d] = I["rnn_b_r"][l][d][sl]
                rv[l, :, c, 9 + d] = I["rnn_b_i"][l][d][sl]
                rw[l, :, c, (0 * 2 + d) * 128:(0 * 2 + d + 1) * 128] = I["rnn_w_r"][l][d][c]
                rw[l, :, c, (1 * 2 + d) * 128:(1 * 2 + d + 1) * 128] = I["rnn_w_i"][l][d][c]
    m["rnn_vec"] = rv; m["rnn_w"] = rw
    return m

def ssd_consts():
    c = np.zeros((128, 512), np.float32)
    j = np.arange(128)[:, None]; i = np.arange(128)[None, :]
    c[:, 0:128] = (j <= i); c[:, 128:256] = (j >= i)
    c[:, 256:384] = np.where(j <= i, 0.0, -30000.0); c[:, 384:512] = np.where(j >= i, 0.0, -30000.0)
    return c

def prep_ssd(I, L):
    m = {}
    m["ssd_consts"] = ssd_consts()
    hv = np.zeros((L, 16, 64, 8), np.float32); gv = np.zeros((L, 2, 128, 10), np.float32); dv = np.zeros((L, 32, 2), np.float32)
    for l in range(L):
        cw, cb = I["ssd_conv_w"][l], I["ssd_conv_b"][l]
        for h in range(16):
            sl = slice(h * 64, (h + 1) * 64)
            hv[l, h, :, 0:4] = cw[:, sl].T; hv[l, h, :, 4] = cb[sl]
            hv[l, h, :, 5] = I["ssd_d"][l][h]; hv[l, h, :, 6] = I["ssd_norm"][l][sl]
        for g in range(2):
            for bi in range(2):
                sl = slice(1024 + bi * 256 + g * 128, 1024 + bi * 256 + (g + 1) * 128)
                gv[l, g, :, bi * 5:bi * 5 + 4] = cw[:, sl].T; gv[l, g, :, bi * 5 + 4] = cb[sl]
        for d in range(2):
            dv[l, d * 16:(d + 1) * 16, 0] = I["ssd_dt_bias"][l][d]; dv[l, d * 16:(d + 1) * 16, 1] = I["ssd_a_log"][l][d]
    m["ssd_hvec"] = hv; m["ssd_gvec"] = gv; m["ssd_dvec"] = dv
    return m

def prep_ffn(I, L):
    m = {}
    f32 = lambda a: np.ascontiguousarray(a, dtype=np.float32)
    m["w_up"] = f32(np.stack([wlay(I["w_up"][l].reshape(3 * 1024, D)) for l in range(L)]))
    m["w_o"] = f32(np.stack([wlay(I["w_o"][l]) for l in range(L)]))
    gus = []
    for l in range(L):
        wg, wu = I["moe_w_gate"][l], I["moe_w_up"][l]
        cols = []
        for e in range(16):
            for f in range(4):
                cols.append(wg[e][:, f * 128:(f + 1) * 128]); cols.append(wu[e][:, f * 128:(f + 1) * 128])
        gus.append(wlay(np.concatenate(cols, 1)))
    m["w_gu"] = f32(np.stack(gus))
    m["w_dn"] = f32(np.stack([wlay(I["moe_w_down"][l].reshape(16 * 512, D)) for l in range(L)]))
    m["router_w"] = f32(I["router_w"].reshape(KD, 128, 16).transpose(1, 0, 2).reshape(128, KD * 16))
    m["router_b"] = f32(I["router_b"].reshape(1, 16))
    es = np.zeros((16, 16 * 128), np.float32)
    for e in range(16): es[e, e * 128:(e + 1) * 128] = 1.0
    m["esel"] = es
    m["g_final"] = f32(vlay(I["g_final"]))
    return m


PER_LAYER = ("w_mod_a", "w_mod_b", "b_mod", "g_mix", "g_ffn", "w_in", "w_up", "w_o", "q_norm", "k_norm",
             "rnn_conv_w", "rnn_conv_b", "rnn_lambda", "rnn_w_r", "rnn_b_r", "rnn_w_i", "rnn_b_i",
             "ssd_conv_w", "ssd_conv_b", "ssd_dt_bias", "ssd_a_log", "ssd_d", "ssd_norm",
             "moe_w_gate", "moe_w_up", "moe_w_down")


def build_layer(cfg):
    k = K(cfg)
    k.setup(); k.setup_mix(); k.setup_ssd(); k.setup_ffn()
    P = k.P
    P.dma(k.xT.t.ap()[:, :], k.x_in.t.ap()[:, :], r=[k.x_in], w=[k.xT])
    P.barrier()
    k.adaln(0)
    k.norm_mod(k.A1, 0, k.xT, k.hT)
    P.barrier()
    k.p1(0)
    k.attention(0)
    k.rglru(0)
    k.ssd(0)
    k.merge(0)
    k.resid_gemm(0, k.mT, KD, k.w_o, 2)
    P.barrier()
    k.norm_mod(k.A2, 3, k.xT, k.hT)
    k.router(0)
    k.moe(0)
    P.barrier()
    o_x = k.outp("o_x", [D, cfg.T])
    P.dma(o_x.t.ap()[:, :], k.xT.t.ap()[:, :], r=[k.xT], w=[o_x])
    k.final_norm()
    P.finish([k.o_out, o_x])
    return k


def kernel(**inputs):
    I = {k_: np.asarray(v) for k_, v in inputs.items()}
    B, SEQ, _ = I["x"].shape
    CTX = I["ctx"].shape[1]
    L = I["w_in"].shape[0]
    cfg = Cfg(CTX, SEQ, 1)
    k = build_layer(cfg)
    xs = [np.ascontiguousarray(np.concatenate([I["ctx"][b], I["x"][b]], 0).T, dtype=np.float32) for b in range(B)]
    conds = []
    for b in range(B):
        cond = np.stack([I["c"][b], I["c_ctx"]], -1)
        conds.append(np.ascontiguousarray(cond.reshape(KD, 128, 2).transpose(1, 0, 2).reshape(128, KD * 2), dtype=np.float32))
    res = None
    for l in range(L):
        Il = {k_: (v[l:l + 1] if k_ in PER_LAYER else v) for k_, v in I.items()}
        shared = prep(Il, 0, 1, cfg)
        shared.update(prep_ssd(Il, 1))
        shared.update(prep_ffn(Il, 1))
        in_maps = []
        for b in range(B):
            m = dict(shared)
            m["x_in"] = xs[b]
            m["cond"] = conds[b]
            in_maps.append({k_: v for k_, v in m.items() if k_ in k.inputs})
        res = run_bass_kernel_spmd(k.nc, in_maps, core_ids=list(range(B)))
        xs = [np.ascontiguousarray(res.results[b]["o_x"], dtype=np.float32) for b in range(B)]
        del shared, in_maps
    out = np.stack([np.ascontiguousarray(res.results[b]["o_out"].T) for b in range(B)]).astype(np.float32)
    return out
```
